# Optimizing a Trainium2 kernel written in Bass

```python
import jax
import jax.numpy as jnp
from jax import lax
import numpy as np

D_MODEL = 1024
BATCH = 8
SEQ = 2048
DEPTH = 4

N_MIXERS = 3
D_FF = 2816
NORM_EPS = 1e-6
N_SUBLAYER_NORMS = 6

SWA_HEADS = 16
SWA_KV_HEADS = 4
SWA_GROUP = SWA_HEADS // SWA_KV_HEADS
SWA_HEAD_DIM = 64
SWA_WINDOW = 128
SWA_BLOCK = 128

GLA_HEADS = 4
GLA_DK = D_MODEL // 2
GLA_DV = D_MODEL
GLA_DK_HEAD = GLA_DK // GLA_HEADS
GLA_DV_HEAD = GLA_DV // GLA_HEADS
GLA_GATE_RANK = 16
GLA_GATE_TEMP = 16.0
GLA_CHUNK = 64
GLA_NORM_EPS = 1e-5

RWKV_HEAD_SIZE = 64
RWKV_HEADS = D_MODEL // RWKV_HEAD_SIZE
RWKV_DECAY_LORA = 64
RWKV_AAA_LORA = 64
RWKV_GATE_LORA = 160
RWKV_LNX_EPS = 64e-5
RWKV_N_SHIFT = 6

N_SWA_LAYERS = (DEPTH + 2) // 3
N_GLA_LAYERS = (DEPTH + 1) // 3
N_RWKV_LAYERS = DEPTH // 3

kernel_name = 'hybrid_swa_gla_rwkv7_macaron'


def rmsnorm(x, g, eps=NORM_EPS):
    xf = x.astype(jnp.float32)
    y = xf * lax.rsqrt(jnp.mean(xf * xf, axis=-1, keepdims=True) + eps)
    return (y * g.astype(jnp.float32)).astype(x.dtype)


def swiglu(x, w_in, w_out):
    gate, up = jnp.split(x @ w_in, 2, axis=-1)
    return (jax.nn.silu(gate) * up) @ w_out


def swa_attention(x, w_qkv, b_qkv, sinks, w_o, b_o):
    bsz, seq, _ = x.shape
    nb = seq // SWA_BLOCK
    qkv = x @ w_qkv + b_qkv
    q, k, v = jnp.split(qkv, [SWA_HEADS * SWA_HEAD_DIM, (SWA_HEADS + SWA_KV_HEADS) * SWA_HEAD_DIM], axis=-1)
    q = q.reshape(bsz, nb, SWA_BLOCK, SWA_KV_HEADS, SWA_GROUP, SWA_HEAD_DIM)
    k = k.reshape(bsz, nb, SWA_BLOCK, SWA_KV_HEADS, SWA_HEAD_DIM)
    v = v.reshape(bsz, nb, SWA_BLOCK, SWA_KV_HEADS, SWA_HEAD_DIM)

    def with_prev(t):
        prev = jnp.concatenate([jnp.zeros_like(t[:, :1]), t[:, :-1]], axis=1)
        return jnp.concatenate([prev, t], axis=2)

    kb, vb = with_prev(k), with_prev(v)
    scores = jnp.einsum('bnqhgd,bnkhd->bnhgqk', q, kb).astype(jnp.float32) * (SWA_HEAD_DIM ** -0.5)
    q_rel = jnp.arange(SWA_BLOCK)[:, None] + SWA_BLOCK
    k_rel = jnp.arange(2 * SWA_BLOCK)[None, :]
    dist = q_rel - k_rel
    band = (dist >= 0) & (dist < SWA_WINDOW)
    has_prev = (jnp.arange(nb) > 0)[:, None, None] | (k_rel >= SWA_BLOCK)[None]
    valid = band[None] & has_prev
    scores = jnp.where(valid[None, :, None, None], scores, -jnp.inf)
    sink = sinks.astype(jnp.float32).reshape(SWA_KV_HEADS, SWA_GROUP)[None, None, :, :, None, None]
    sink = jnp.broadcast_to(sink, scores.shape[:-1] + (1,))
    probs = jax.nn.softmax(jnp.concatenate([scores, sink], axis=-1), axis=-1)[..., :-1]
    out = jnp.einsum('bnhgqk,bnkhd->bnqhgd', probs.astype(vb.dtype), vb)
    return out.reshape(bsz, seq, SWA_HEADS * SWA_HEAD_DIM) @ w_o + b_o


def gla_attention(x, w_in, w_gate2, b_gate, norm_g, w_o):
    bsz, seq, _ = x.shape
    nc = seq // GLA_CHUNK
    splits = [GLA_DK, 2 * GLA_DK, 2 * GLA_DK + GLA_DV, 2 * GLA_DK + 2 * GLA_DV]
    q, k, v, g_out, g_low = jnp.split(x @ w_in, splits, axis=-1)
    log_alpha = jax.nn.log_sigmoid((g_low @ w_gate2 + b_gate).astype(jnp.float32)) / GLA_GATE_TEMP

    def to_chunks(t, d):
        t = t.astype(jnp.float32).reshape(bsz, nc, GLA_CHUNK, GLA_HEADS, d)
        return t.transpose(1, 0, 3, 2, 4)

    qc = to_chunks(q, GLA_DK_HEAD) * (GLA_DK_HEAD ** -0.5)
    kc = to_chunks(k, GLA_DK_HEAD)
    vc = to_chunks(v, GLA_DV_HEAD)
    gc = to_chunks(log_alpha, GLA_DK_HEAD)
    causal = jnp.tril(jnp.ones((GLA_CHUNK, GLA_CHUNK), dtype=bool))[None, None, :, :, None]

    def chunk_step(state, inp):
        q_t, k_t, v_t, g_t = inp
        b = jnp.cumsum(g_t, axis=2)
        o_inter = jnp.einsum('bhik,bhkv->bhiv', q_t * jnp.exp(b), state)
        rel = jnp.where(causal, b[:, :, :, None, :] - b[:, :, None, :, :], -jnp.inf)
        scores = jnp.einsum('bhik,bhjk,bhijk->bhij', q_t, k_t, jnp.exp(rel))
        o_intra = jnp.einsum('bhij,bhjv->bhiv', scores, v_t)
        b_last = b[:, :, -1:, :]
        state = (state * jnp.exp(b_last[:, :, 0, :])[..., None]
                 + jnp.einsum('bhjk,bhjv->bhkv', k_t * jnp.exp(b_last - b), v_t))
        return state, o_inter + o_intra

    state0 = jnp.zeros((bsz, GLA_HEADS, GLA_DK_HEAD, GLA_DV_HEAD), jnp.float32)
    _, o = lax.scan(chunk_step, state0, (qc, kc, vc, gc))
    o = o.transpose(1, 0, 3, 2, 4).reshape(bsz, seq, GLA_HEADS, GLA_DV_HEAD)
    o = rmsnorm(o, norm_g, GLA_NORM_EPS).reshape(bsz, seq, GLA_DV)
    o = (o * jax.nn.silu(g_out.astype(jnp.float32))).astype(x.dtype)
    return o @ w_o


def rwkv7_time_mix(x, mu, w_rkv, w0, w1, w2, a0, a1, a2, g1, g2, k_k, k_a, r_k, lnx_g, lnx_b, w_o):
    bsz, seq, _ = x.shape
    H, N = RWKV_HEADS, RWKV_HEAD_SIZE
    f32 = jnp.float32
    xx = jnp.pad(x, ((0, 0), (1, 0), (0, 0)))[:, :-1] - x
    xr, xw, xk, xv, xa, xg = (x + xx * mu[i] for i in range(RWKV_N_SHIFT))
    r = xr @ w_rkv[0]
    k = xk @ w_rkv[1]
    v = xv @ w_rkv[2]
    w = -jax.nn.softplus(-(w0 + jnp.tanh(xw @ w1) @ w2).astype(f32)) - 0.5
    decay = jnp.exp(-jnp.exp(w))
    a = jax.nn.sigmoid((a0 + (xa @ a1) @ a2).astype(f32))
    g = jax.nn.sigmoid(xg @ g1) @ g2
    kk = (k * k_k).astype(f32).reshape(bsz, seq, H, N)
    kk = kk / jnp.maximum(jnp.sqrt(jnp.sum(kk * kk, axis=-1, keepdims=True)), 1e-12)
    k = k.astype(f32) * (1.0 + (a - 1.0) * k_a.astype(f32))

    def heads(t):
        return t.astype(f32).reshape(bsz, seq, H, N)

    r_h, k_h, v_h, a_h, w_h = heads(r), heads(k), heads(v), heads(a), heads(decay)
    tm = lambda t: t.transpose(1, 0, 2, 3)
    xs = (tm(r_h), tm(w_h), tm(k_h), tm(v_h), tm(-kk), tm(kk * a_h))

    def step(state, inp):
        r_t, w_t, k_t, v_t, a_t, b_t = inp
        sa = jnp.einsum('bhij,bhj->bhi', state, a_t)
        state = (state * w_t[:, :, None, :] + sa[..., None] * b_t[:, :, None, :]
                 + v_t[..., None] * k_t[:, :, None, :])
        return state, jnp.einsum('bhij,bhj->bhi', state, r_t)

    _, y = lax.scan(step, jnp.zeros((bsz, H, N, N), f32), xs)
    y = y.transpose(1, 0, 2, 3)
    mean = jnp.mean(y, axis=-1, keepdims=True)
    var = jnp.mean(jnp.square(y - mean), axis=-1, keepdims=True)
    y = ((y - mean) * lax.rsqrt(var + RWKV_LNX_EPS)).reshape(bsz, seq, D_MODEL)
    y = y * lnx_g.astype(f32) + lnx_b.astype(f32)
    bonus = jnp.sum(r_h * k_h * r_k.astype(f32), axis=-1, keepdims=True) * v_h
    y = y + bonus.reshape(bsz, seq, D_MODEL)
    return (y * g.astype(f32)).astype(x.dtype) @ w_o


def setup_inputs(seed: int = 0) -> dict:
    key = jax.random.key(seed)
    ks = iter(list(jax.random.split(key, 32)))

    def nrm(shape, scale):
        return jax.random.normal(next(ks), shape, jnp.float32) * scale

    nA, nB, nC = N_SWA_LAYERS, N_GLA_LAYERS, N_RWKV_LAYERS
    qkv_w = (SWA_HEADS + 2 * SWA_KV_HEADS) * SWA_HEAD_DIM
    gla_in_w = 2 * GLA_DK + 2 * GLA_DV + GLA_GATE_RANK
    D = D_MODEL
    return {
        'x': nrm((BATCH, SEQ, D), 1.0),
        'norm_g': 1.0 + nrm((DEPTH, N_SUBLAYER_NORMS, D), 0.02),
        'ffn_w_in': nrm((DEPTH, 2, D, 2 * D_FF), D ** -0.5),
        'ffn_w_out': nrm((DEPTH, 2, D_FF, D), D_FF ** -0.5),
        'swa_w_qkv': nrm((nA, D, qkv_w), D ** -0.5),
        'swa_b_qkv': nrm((nA, qkv_w), 0.02),
        'swa_sinks': nrm((nA, SWA_HEADS), 1.0),
        'swa_w_o': nrm((nA, SWA_HEADS * SWA_HEAD_DIM, D), (SWA_HEADS * SWA_HEAD_DIM) ** -0.5),
        'swa_b_o': nrm((nA, D), 0.02),
        'gla_w_in': nrm((nB, D, gla_in_w), D ** -0.5),
        'gla_w_gate2': nrm((nB, GLA_GATE_RANK, GLA_DK), GLA_GATE_RANK ** -0.5),
        'gla_b_gate': nrm((nB, GLA_DK), 0.1),
        'gla_norm_g': 1.0 + nrm((nB, GLA_DV_HEAD), 0.02),
        'gla_w_o': nrm((nB, GLA_DV, D), GLA_DV ** -0.5),
        'rwkv_mu': jax.random.uniform(next(ks), (nC, RWKV_N_SHIFT, D), jnp.float32),
        'rwkv_w_rkv': nrm((nC, 3, D, D), D ** -0.5),
        'rwkv_w0': jax.random.uniform(next(ks), (nC, D), jnp.float32, -6.0, -1.0),
        'rwkv_w1': nrm((nC, D, RWKV_DECAY_LORA), D ** -0.5),
        'rwkv_w2': nrm((nC, RWKV_DECAY_LORA, D), 0.1 * RWKV_DECAY_LORA ** -0.5),
        'rwkv_a0': nrm((nC, D), 0.1),
        'rwkv_a1': nrm((nC, D, RWKV_AAA_LORA), D ** -0.5),
        'rwkv_a2': nrm((nC, RWKV_AAA_LORA, D), 0.1 * RWKV_AAA_LORA ** -0.5),
        'rwkv_g1': nrm((nC, D, RWKV_GATE_LORA), D ** -0.5),
        'rwkv_g2': nrm((nC, RWKV_GATE_LORA, D), RWKV_GATE_LORA ** -0.5),
        'rwkv_k_k': 0.85 + nrm((nC, D), 0.02),
        'rwkv_k_a': 1.0 + nrm((nC, D), 0.02),
        'rwkv_r_k': nrm((nC, RWKV_HEADS, RWKV_HEAD_SIZE), 0.1),
        'rwkv_lnx_g': 1.0 + nrm((nC, D), 0.02),
        'rwkv_lnx_b': nrm((nC, D), 0.02),
        'rwkv_w_o': nrm((nC, D, D), D ** -0.5),
    }


def reference(x, norm_g, ffn_w_in, ffn_w_out,
              swa_w_qkv, swa_b_qkv, swa_sinks, swa_w_o, swa_b_o,
              gla_w_in, gla_w_gate2, gla_b_gate, gla_norm_g, gla_w_o,
              rwkv_mu, rwkv_w_rkv, rwkv_w0, rwkv_w1, rwkv_w2, rwkv_a0, rwkv_a1, rwkv_a2,
              rwkv_g1, rwkv_g2, rwkv_k_k, rwkv_k_a, rwkv_r_k, rwkv_lnx_g, rwkv_lnx_b, rwkv_w_o):
    h = x
    for layer in range(DEPTH):
        g = norm_g[layer]
        ff = swiglu(rmsnorm(h, g[0]), ffn_w_in[layer, 0], ffn_w_out[layer, 0])
        h = h + 0.5 * rmsnorm(ff, g[1])
        u = rmsnorm(h, g[2])
        kind = layer % N_MIXERS
        j = layer // N_MIXERS
        if kind == 0:
            m = swa_attention(u, swa_w_qkv[j], swa_b_qkv[j], swa_sinks[j], swa_w_o[j], swa_b_o[j])
        elif kind == 1:
            m = gla_attention(u, gla_w_in[j], gla_w_gate2[j], gla_b_gate[j], gla_norm_g[j], gla_w_o[j])
        else:
            m = rwkv7_time_mix(u, rwkv_mu[j], rwkv_w_rkv[j], rwkv_w0[j], rwkv_w1[j], rwkv_w2[j],
                               rwkv_a0[j], rwkv_a1[j], rwkv_a2[j], rwkv_g1[j], rwkv_g2[j],
                               rwkv_k_k[j], rwkv_k_a[j], rwkv_r_k[j], rwkv_lnx_g[j], rwkv_lnx_b[j],
                               rwkv_w_o[j])
        h = h + rmsnorm(m, g[3])
        ff = swiglu(rmsnorm(h, g[4]), ffn_w_in[layer, 1], ffn_w_out[layer, 1])
        h = h + 0.5 * rmsnorm(ff, g[5])
    return h
```

```python
import contextlib
import numpy as np
import ml_dtypes
import concourse.bass as bass
import concourse.mybir as mybir
from concourse.bass_utils import run_bass_kernel_spmd

F32 = mybir.dt.float32
BF16 = mybir.dt.bfloat16
AF = mybir.ActivationFunctionType
ALU = mybir.AluOpType

D = 1024
SEQ = 2048
DEPTH = 4
DFF = 2816
NFC = DFF // 128
NSLAB = NFC // 2
TB = 1024
NTT = TB // 128
EPS = 1e-6


class Buf:
    __slots__ = ("w", "rs")

    def __init__(self):
        self.w = None
        self.rs = []


class Inst:
    __slots__ = ("eng", "fn", "deps", "sig", "dma", "sem", "val", "prev_dma")

    def __init__(self, eng, fn, dma):
        self.eng = eng
        self.fn = fn
        self.deps = []
        self.sig = False
        self.dma = dma
        self.sem = None
        self.val = None
        self.prev_dma = None


class _Rec:
    def __init__(self):
        self.call = None

    def __getattr__(self, name):
        def f(*a, **k):
            self.call = (name, a, k)
            return self
        return f


class Prog:
    ENGS = ("pe", "act", "dve", "pool", "sp")
    NDMA = 8

    def __init__(self, nc, stack):
        self.nc = nc
        self.q = {e: [] for e in self.ENGS}
        self.esem = {e: stack.enter_context(nc.semaphore("s_" + e)) for e in self.ENGS}
        self.dsem = {e: [stack.enter_context(nc.semaphore("d_%s%d" % (e, i))) for i in range(self.NDMA)]
                     for e in ("act", "pool", "sp")}
        self.ndma = {e: [] for e in ("act", "pool", "sp")}
        self.bufs = {}

    def b(self, *key):
        bb = self.bufs.get(key)
        if bb is None:
            bb = self.bufs[key] = Buf()
        return bb

    def op(self, eng, fn, r=(), w=(), dma=False):
        rec = _Rec()
        fn(rec)
        inst = Inst(eng, rec.call, dma)

        def dep(o, war=False):
            if o is None or o is inst:
                return
            if not dma and not o.dma and o.eng == eng:
                if eng == "pe":
                    return
            if o not in inst.deps:
                inst.deps.append(o)
                o.sig = True

        for bb in r:
            dep(bb.w)
        for bb in w:
            dep(bb.w)
            for o in bb.rs:
                dep(o, war=True)
        for bb in r:
            bb.rs.append(inst)
        for bb in w:
            bb.w = inst
            bb.rs = []
        if dma:
            lst = self.ndma[eng]
            if len(lst) >= self.NDMA:
                inst.prev_dma = lst[len(lst) - self.NDMA]
            inst.sem = self.dsem[eng][len(lst) % self.NDMA]
            inst.val = 16 * (len(lst) // self.NDMA + 1)
            lst.append(inst)
        self.q[eng].append(inst)
        return inst

    def emit(self):
        nc = self.nc
        for e in self.ENGS:
            c = 0
            for inst in self.q[e]:
                if not inst.dma:
                    inst.sem = self.esem[e]
                    if inst.sig:
                        c += 1
                    inst.val = c if inst.sig else None
        engobj = {"pe": "tensor", "act": "scalar", "dve": "vector", "pool": "gpsimd", "sp": "sync"}

        def run(e, eng):
            known = {}
            for inst in self.q[e]:
                waits = {}
                ds = list(inst.deps)
                if inst.prev_dma is not None:
                    ds.append(inst.prev_dma)
                for o in ds:
                    k = id(o.sem)
                    if o.val > known.get(k, 0) and o.val > waits.get(k, (None, 0))[1]:
                        waits[k] = (o.sem, o.val)
                for k, (s, v) in waits.items():
                    eng.wait_ge(s, v)
                    known[k] = v
                name, a, k = inst.fn
                h = getattr(eng, name)(*a, **k)
                if inst.dma:
                    h.then_inc(inst.sem, 16)
                elif inst.sig:
                    h.then_inc(inst.sem, 1)

        with nc.Block() as block:
            for e in self.ENGS:
                if not self.q[e]:
                    continue
                getattr(block, engobj[e])(lambda eng, e=e: run(e, eng))


class K:
    def __init__(self, cfg):
        self.cfg = cfg
        nc = self.nc = bass.Bass("TRN2", target_bir_lowering=False)
        self.stack = contextlib.ExitStack()
        self.P = Prog(nc, self.stack)
        self.dram = {}
        self.gbi = 0
        self.pi = {}

    def din(self, name, shape, dt=F32):
        t = self.nc.dram_tensor(name, list(shape), dt, kind="ExternalInput")
        self.dram[name] = t
        return t

    def sb(self, name, shape, dt):
        return self.stack.enter_context(self.nc.sbuf_tensor(name, list(shape), dt))

    def ps(self, name, shape, dt):
        return self.stack.enter_context(self.nc.psum_tensor(name, list(shape), dt))

    def rot(self, key, n):
        i = self.pi.get(key, 0)
        self.pi[key] = i + 1
        return i % n

    def load_row_bc(self, row):
        P = self.P
        i = self.rot("gb", 2)
        src = self.dram["rows"].ap()[row:row + 1, :].partition_broadcast(128)
        P.op("sp", lambda e: e.dma_start(out=self.GB[i][:, :], in_=src), w=[P.b("GB", i)], dma=True)
        return i

    def rstd_batch(self, ss, n, scale, eps, half=False, key="rs"):
        P = self.P
        bs = P.b(key)
        P.op("act", lambda e: e.activation(out=ss[:, :n], in_=ss[:, :n], func=AF.Sqrt,
                                           bias=self.epsc[eps][:, 0:1], scale=scale), r=[bs, P.b("epsc")], w=[bs])
        P.op("dve", lambda e: e.reciprocal(out=ss[:, :n], in_=ss[:, :n]), r=[bs], w=[bs])
        if half:
            P.op("dve", lambda e: e.tensor_scalar(out=ss[:, :n], in0=ss[:, :n], scalar1=0.5, scalar2=None,
                                                  op0=ALU.mult), r=[bs], w=[bs])

    def _rstd_tile(self, ss, col, scale, eps, key):
        P = self.P
        P.op("act", lambda e: e.activation(out=ss[:, col:col + 1], in_=ss[:, col:col + 1], func=AF.Sqrt,
                                           bias=self.epsc[eps][:, 0:1], scale=scale), r=[key, P.b("epsc")], w=[key])
        P.op("dve", lambda e: e.reciprocal(out=ss[:, col:col + 1], in_=ss[:, col:col + 1]), r=[key], w=[key])

    def _norm_tile(self, tt, gi):
        P = self.P
        ss = self.SS
        key = P.b("rsn", tt)
        P.op("act", lambda e: e.activation(out=self.JUNK[:, :], in_=self.H[:, tt, :], func=AF.Square,
                                           accum_out=ss[:, tt:tt + 1]), r=[P.b("H", tt)], w=[key])
        self._rstd_tile(ss, tt, 1.0 / D, EPS, key)
        xi = self.rot("xs", 2)
        P.op("dve", lambda e: e.scalar_tensor_tensor(
            out=self.XS[xi][:, :], in0=self.H[:, tt, :], scalar=ss[:, tt:tt + 1], in1=self.GB[gi][:, :],
            op0=ALU.mult, op1=ALU.mult), r=[P.b("H", tt), key, P.b("GB", gi)], w=[P.b("XS", xi)])
        self.transpose_to_XT(self.XS[xi], P.b("XS", xi), tt)

    def norm_T(self, grow):
        if self.norm_done == grow:
            self.norm_done = None
            return
        gi = self.load_row_bc(grow)
        for tt in range(NTT):
            self._norm_tile(tt, gi)

    def post_norm(self, grow, half, src_key="ACC", bias_row=None):
        P = self.P
        gi = self.load_row_bc(grow)
        nrow = self.next_norm_row
        gn = self.load_row_bc(nrow) if nrow is not None else None
        ss = self.SS
        sc, ep = ((4.0 / D, 4 * EPS) if half else (1.0 / D, EPS))
        for tt in range(NTT):
            key = P.b("rsp", tt)
            P.op("act", lambda e: e.activation(out=self.JUNK[:, :], in_=self.ACC[:, tt, :], func=AF.Square,
                                               accum_out=ss[:, 8 + tt:9 + tt]),
                 r=[P.b("ACC", tt, 0), P.b("ACC", tt, 1)], w=[key])
            self._rstd_tile(ss, 8 + tt, sc, ep, key)
            P.op("dve", lambda e: e.scalar_tensor_tensor(
                out=self.ACC[:, tt, :], in0=self.ACC[:, tt, :], scalar=ss[:, 8 + tt:9 + tt], in1=self.GB[gi][:, :],
                op0=ALU.mult, op1=ALU.mult),
                r=[P.b("ACC", tt, 0), P.b("ACC", tt, 1), key, P.b("GB", gi)],
                w=[P.b("ACC", tt, 0), P.b("ACC", tt, 1)])
            P.op("dve" if tt % 2 == 0 else "pool", lambda e: e.tensor_tensor(
                out=self.H[:, tt, :], in0=self.H[:, tt, :], in1=self.ACC[:, tt, :], op=ALU.add),
                r=[P.b("H", tt), P.b("ACC", tt, 0), P.b("ACC", tt, 1)], w=[P.b("H", tt)])
            if gn is not None:
                self._norm_tile(tt, gn)
        if gn is not None:
            self.norm_done = nrow

    def ffn(self, l, a):
        P = self.P
        self.norm_T(l * 6 + (0 if a == 0 else 4))
        win_d = self.dram["ffn_win"].ap()
        wout_d = self.dram["ffn_wout"].ap()

        def load(s):
            wb = self.rot("wslab", 2)
            P.op("pool", lambda e: e.dma_start(out=self.WIN[wb][:, :], in_=win_d[l, a, s, :, :]),
                 w=[P.b("WIN", wb)], dma=True)
            P.op("pool", lambda e: e.dma_start(out=self.WOUT[wb][:, :], in_=wout_d[l, a, s, :, :]),
                 w=[P.b("WOUT", wb)], dma=True)
            return wb

        def phase1(s, wb):
            for tb in range(2):
                for fcl in range(2):
                    gi = self.rot("pgu", 2)
                    for gu, ps in ((0, self.PG[gi]), (1, self.PU[gi])):
                        for kc in range(8):
                            c0 = kc * 512 + fcl * 256 + gu * 128
                            P.op("pe", lambda e, ps=ps, c0=c0, kc=kc, tb=tb: e.matmul(
                                ps[:, :], lhsT=self.WIN[wb][:, c0:c0 + 128],
                                rhs=self.XT[:, kc, tb * 512:(tb + 1) * 512], start=(kc == 0), stop=(kc == 7)),
                                r=[P.b("WIN", wb)] + [P.b("XT", tb * 4 + i) for i in range(4)],
                                w=[P.b("PG" if gu == 0 else "PU", gi)])
                    P.op("act", lambda e, gi=gi: e.activation(out=self.SG[gi][:, :], in_=self.PG[gi][:, :],
                                                              func=AF.Silu),
                         r=[P.b("PG", gi)], w=[P.b("SG", gi)])
                    P.op("dve", lambda e, gi=gi, fcl=fcl, tb=tb: e.tensor_tensor(
                        out=self.ATS[wb][:, fcl, tb * 512:(tb + 1) * 512], in0=self.SG[gi][:, :],
                        in1=self.PU[gi][:, :], op=ALU.mult),
                        r=[P.b("SG", gi), P.b("PU", gi)], w=[P.b("ATS", wb, fcl, tb)])

        def phase2(s, wb):
            for tt in range(NTT):
                for nh in range(2):
                    oi = self.rot("po", 2)
                    for fcl in range(2):
                        P.op("pe", lambda e, oi=oi, fcl=fcl, tt=tt, nh=nh: e.matmul(
                            self.PO[oi][:, :], lhsT=self.ATS[wb][:, fcl, tt * 128:(tt + 1) * 128],
                            rhs=self.WOUT[wb][:, fcl * 1024 + nh * 512: fcl * 1024 + (nh + 1) * 512],
                            start=(fcl == 0), stop=(fcl == 1)),
                            r=[P.b("ATS", wb, fcl, tt // 4), P.b("WOUT", wb)], w=[P.b("PO", oi)])
                    if s == 0:
                        P.op("act", lambda e, oi=oi, tt=tt, nh=nh: e.activation(
                            out=self.ACC[:, tt, nh * 512:(nh + 1) * 512], in_=self.PO[oi][:, :], func=AF.Copy),
                            r=[P.b("PO", oi)], w=[P.b("ACC", tt, nh)])
                    else:
                        P.op("dve", lambda e, oi=oi, tt=tt, nh=nh: e.tensor_tensor(
                            out=self.ACC[:, tt, nh * 512:(nh + 1) * 512], in0=self.ACC[:, tt, nh * 512:(nh + 1) * 512],
                            in1=self.PO[oi][:, :], op=ALU.add),
                            r=[P.b("PO", oi), P.b("ACC", tt, nh)], w=[P.b("ACC", tt, nh)])

        wbs = {}
        wbs[0] = load(0)
        phase1(0, wbs[0])
        for s in range(NSLAB):
            if s + 1 < NSLAB:
                wbs[s + 1] = load(s + 1)
                phase1(s + 1, wbs[s + 1])
            phase2(s, wbs[s])
        self.post_norm(l * 6 + (1 if a == 0 else 5), half=True)


    def load_w(self, name, *idx):
        P = self.P
        wb = self.rot("wslab", 2)
        src = self.dram[name].ap()
        for i in idx:
            src = src[i]
        P.op("pool", lambda e: e.dma_start(out=self.WIN[wb][:, :], in_=src), w=[P.b("WIN", wb)], dma=True)
        return wb

    def proj_fm(self, wb, c0, M, tb, ps, pkey):
        P = self.P
        for kc in range(8):
            P.op("pe", lambda e, kc=kc: e.matmul(
                ps[:M, :], lhsT=self.WIN[wb][:, kc * 512 + c0: kc * 512 + c0 + M],
                rhs=self.XT[:, kc, tb * 512:(tb + 1) * 512], start=(kc == 0), stop=(kc == 7)),
                r=[P.b("WIN", wb)] + [P.b("XT", tb * 4 + i) for i in range(4)], w=[pkey])

    def proj_tm(self, wb, c0, N, tt, ps, pkey, xt=None, xkey="XT"):
        P = self.P
        xt = self.XT if xt is None else xt
        for kc in range(8):
            P.op("pe", lambda e, kc=kc: e.matmul(
                ps[:, :N], lhsT=xt[:, kc, tt * 128:(tt + 1) * 128],
                rhs=self.WIN[wb][:, kc * 512 + c0: kc * 512 + c0 + N], start=(kc == 0), stop=(kc == 7)),
                r=[P.b("WIN", wb), P.b(xkey, tt)], w=[pkey])

    def transpose_to_XT(self, src, skey, tt):
        P = self.P
        for kc in range(8):
            P.op("pe", lambda e, kc=kc: e.transpose(
                out=self.PT[:, kc * 128:(kc + 1) * 128], in_=src[:, kc * 128:(kc + 1) * 128],
                identity=self.IDB[:, :]), r=[skey, P.b("IDB")], w=[P.b("PT")])
        P.op("act", lambda e: e.activation(
            out=self.XT[:, :, tt * 128:(tt + 1) * 128],
            in_=self.PT[:, :].rearrange("p (k c) -> p k c", k=8), func=AF.Copy),
            r=[P.b("PT")], w=[P.b("XT", tt)])

    def out_proj(self, wname, widx, brow):
        P = self.P
        wbs = [self.load_w(wname, *widx, nh) for nh in range(2)]
        gi = self.load_row_bc(brow) if brow is not None else None
        for tt in range(NTT):
            for nh in range(2):
                oi = self.rot("po", 2)
                self.proj_tm(wbs[nh], 0, 512, tt, self.PO[oi], P.b("PO", oi))
                if gi is None:
                    P.op("act", lambda e, oi=oi, tt=tt, nh=nh: e.activation(
                        out=self.ACC[:, tt, nh * 512:(nh + 1) * 512], in_=self.PO[oi][:, :], func=AF.Copy),
                        r=[P.b("PO", oi)], w=[P.b("ACC", tt, nh)])
                else:
                    P.op("dve", lambda e, oi=oi, tt=tt, nh=nh: e.tensor_tensor(
                        out=self.ACC[:, tt, nh * 512:(nh + 1) * 512], in0=self.PO[oi][:, :],
                        in1=self.GB[gi][:, nh * 512:(nh + 1) * 512], op=ALU.add),
                        r=[P.b("PO", oi), P.b("GB", gi)], w=[P.b("ACC", tt, nh)])

    def swa(self, l):
        P = self.P
        j = l // 3
        half = self.half
        R0 = self.cfg["row_swa"] + j * 3
        C0 = self.cfg["col_swa"] + j * 12
        self.norm_T(l * 6 + 2)
        P.op("dve", lambda e: e.memset(self.VA[:, :, :], 1.0), w=[P.b("VA", i) for i in range(-1, 8)])
        gs = self.load_row_bc(R0 + 2)
        P.op("act", lambda e: e.activation(out=self.ESK[:, :], in_=self.GB[gs][:, 0:16], func=AF.Exp),
             r=[P.b("GB", gs)], w=[P.b("ESK")])
        P.op("dve", lambda e: e.tensor_scalar(out=self.BQ8[:, :], in0=self.COLS[:, C0:C0 + 8], scalar1=0.125,
                                              scalar2=None, op0=ALU.mult), r=[P.b("COLS")], w=[P.b("BQ8")])
        if half == 1:
            P.op("pool", lambda e: e.tensor_copy(out=self.KT[:, :, 0:128], in_=self.KC[j][:, :, :]),
                 r=[P.b("KC", j)], w=[P.b("KT", -1)])
            P.op("pool", lambda e: e.tensor_copy(out=self.VA[:, 0, :], in_=self.VC[j][:, :]),
                 r=[P.b("VC", j)], w=[P.b("VA", -1)])
        for blk in range(2):
            wb = self.load_w("swa_wq", j, blk)
            for ocl in range(4):
                oc = blk * 4 + ocl
                for tb in range(2):
                    gi = self.rot("pgu", 2)
                    self.proj_fm(wb, ocl * 128, 128, tb, self.PG[gi], P.b("PG", gi))
                    P.op("act", lambda e, gi=gi, oc=oc, tb=tb: e.activation(
                        out=self.QT[:, oc, tb * 512:(tb + 1) * 512], in_=self.PG[gi][:, :], func=AF.Identity,
                        bias=self.BQ8[:, oc:oc + 1], scale=0.125),
                        r=[P.b("PG", gi), P.b("BQ8")], w=[P.b("QT", oc, tb)])
        wb = self.load_w("swa_wk", j)
        for hk in range(4):
            for tb in range(2):
                gi = self.rot("pgu", 2)
                self.proj_fm(wb, hk * 128, 128, tb, self.PU[gi], P.b("PU", gi))
                P.op("act", lambda e, gi=gi, hk=hk, tb=tb: e.activation(
                    out=self.KT[:, hk, 128 + tb * 512: 128 + (tb + 1) * 512], in_=self.PU[gi][:, :], func=AF.Identity,
                    bias=self.COLS[:, C0 + 8 + hk:C0 + 9 + hk], scale=1.0),
                    r=[P.b("PU", gi), P.b("COLS")], w=[P.b("KT", tb * 4 + i) for i in range(4)])
        wb = self.load_w("swa_wv", j)
        gv = self.load_row_bc(R0 + 0)
        for tt in range(NTT):
            oi = self.rot("po", 2)
            self.proj_tm(wb, 0, 256, tt, self.PO[oi], P.b("PO", oi))
            P.op("dve", lambda e, oi=oi, tt=tt: e.tensor_tensor(
                out=self.VA[:, tt + 1, :].rearrange("p (h d) -> p h d", h=4)[:, :, 0:64],
                in0=self.PO[oi][:, 0:256].rearrange("p (h d) -> p h d", h=4),
                in1=self.GB[gv][:, 0:256].rearrange("p (h d) -> p h d", h=4), op=ALU.add),
                r=[P.b("PO", oi), P.b("GB", gv)], w=[P.b("VA", tt)])
        for n in range(NTT):
            has_prev = not (half == 0 and n == 0)
            xi = self.rot("xs", 2)
            for hk in range(4):
                srcs = [("cur", self.PG, "PG", 128 + n * 128, n)]
                if has_prev:
                    srcs.append(("prev", self.PU, "PU", n * 128, n - 1))
                pts = []
                for (kind, pss, pname, k0, kblk) in srcs:
                    si = self.rot("sg", 2)
                    for hp in range(2):
                        P.op("pe", lambda e: e.matmul(
                            pss[hp][:, 0:256], lhsT=self.KT[hp * 64:(hp + 1) * 64, hk, k0:k0 + 128],
                            rhs=self.QT[hp * 64:(hp + 1) * 64, 2 * hk:2 * hk + 2, n * 128:(n + 1) * 128],
                            start=True, stop=True),
                            r=[P.b("KT", kblk), P.b("QT", 2 * hk, n // 4), P.b("QT", 2 * hk + 1, n // 4)],
                            w=[P.b(pname, hp)])
                        P.op("act", lambda e: e.activation(out=self.SG[si][:, hp * 256:(hp + 1) * 256],
                                                           in_=pss[hp][:, 0:256], func=AF.Exp),
                             r=[P.b(pname, hp)], w=[P.b("SG", si)])
                    pi = self.rot("pts", 4)
                    mo = 0 if kind == "cur" else 512
                    P.op("dve", lambda e, si=si, pi=pi, mo=mo: e.tensor_tensor(
                        out=self.PTS[pi][:, :], in0=self.SG[si][:, :], in1=self.CMASK[:, mo:mo + 512], op=ALU.mult),
                        r=[P.b("SG", si), P.b("CMASK")], w=[P.b("PTS", pi)])
                    pts.append((pi, kblk))
                oi = self.rot("po", 2)
                for hh in range(4):
                    pos = (hh % 2) * 2 + hh // 2
                    for ii, (pi, kblk) in enumerate(pts):
                        P.op("pe", lambda e, oi=oi, hh=hh, pos=pos, pi=pi, kblk=kblk, ii=ii: e.matmul(
                            self.PO[oi][:, hh * 65:(hh + 1) * 65], lhsT=self.PTS[pi][:, pos * 128:(pos + 1) * 128],
                            rhs=self.VA[:, kblk + 1, hk * 65:(hk + 1) * 65], start=(ii == 0), stop=(ii == len(pts) - 1)),
                            r=[P.b("PTS", pi), P.b("VA", kblk)], w=[P.b("PO", oi)])
                ov = self.PO[oi][:, 0:260].rearrange("p (h d) -> p h d", h=4)
                P.op("dve", lambda e, ov=ov: e.tensor_tensor(
                    out=self.DEN[:, :], in0=ov[:, :, 64], in1=self.ESK[:, hk * 4:(hk + 1) * 4], op=ALU.add),
                    r=[P.b("PO", oi), P.b("ESK")], w=[P.b("DEN")])
                P.op("dve", lambda e: e.reciprocal(out=self.DEN[:, :], in_=self.DEN[:, :]),
                     r=[P.b("DEN")], w=[P.b("DEN")])
                P.op("dve", lambda e, ov=ov, xi=xi: e.tensor_tensor(
                    out=self.XS[xi][:, hk * 256:(hk + 1) * 256].rearrange("p (h d) -> p h d", h=4),
                    in0=ov[:, :, 0:64], in1=self.DEN[:, :].unsqueeze(2).to_broadcast([128, 4, 64]), op=ALU.mult),
                    r=[P.b("PO", oi), P.b("DEN")], w=[P.b("XS", xi)])
            self.transpose_to_XT(self.XS[xi], P.b("XS", xi), n)
        if half == 0:
            P.op("pool", lambda e: e.tensor_copy(out=self.KC[j][:, :, :], in_=self.KT[:, :, 1024:1152]),
                 r=[P.b("KT", 7)], w=[P.b("KC", j)])
            P.op("pool", lambda e: e.tensor_copy(out=self.VC[j][:, :], in_=self.VA[:, 8, :]),
                 r=[P.b("VA", 7)], w=[P.b("VC", j)])
        self.out_proj("swa_wo", (j,), R0 + 1)
        self.post_norm(l * 6 + 3, half=False)


    def gla(self, l):
        P = self.P
        half = self.half
        R0 = self.cfg["row_gla"]
        C0 = self.cfg["col_gla"]
        DK = 128
        LA = self.ATS[0][:, :, :].rearrange("p a b -> p (a b)").bitcast(F32)
        EB = self.ATS[1][:, :, :].rearrange("p a b -> p (a b)").bitcast(F32)
        ENB = self.WOUT[0][:, :].bitcast(F32)
        kLA, kEB, kENB = [P.b("ATS", 0, a, b) for a in range(2) for b in range(2)], \
            [P.b("ATS", 1, a, b) for a in range(2) for b in range(2)], [P.b("WOUT", 0)]
        ACCb = self.ACC[:, :, :].rearrange("p a b -> p (a b)").bitcast(BF16).rearrange("p (a b) -> p a b", a=NTT)
        self.norm_T(l * 6 + 2)
        if half == 0:
            P.op("dve", lambda e: e.memset(self.GS[:, :, :], 0.0), w=[P.b("GS", h) for h in range(4)])
            P.op("dve", lambda e: e.memset(self.GSB[:, :, :], 0.0), w=[P.b("GSB", h) for h in range(4)])
        P.op("dve", lambda e: e.tensor_scalar(out=self.NBG[:, :], in0=self.COLS[:, C0:C0 + 4], scalar1=-1.0,
                                              scalar2=None, op0=ALU.mult), r=[P.b("COLS")], w=[P.b("NBG")])
        P.op("pool", lambda e: e.dma_start(out=self.SG[0][0:16, :], in_=self.dram["gla_wg2"].ap()[:, :]),
             w=[P.b("SG", 0)], dma=True)
        wb = self.load_w("gla_win", 6)
        for tb in range(2):
            gi = self.rot("pgu", 2)
            self.proj_fm(wb, 0, 16, tb, self.PG[gi], P.b("PG", gi))
            P.op("act", lambda e: e.activation(out=self.XS[0][0:16, tb * 512:(tb + 1) * 512], in_=self.PG[gi][0:16, :],
                                               func=AF.Copy), r=[P.b("PG", gi)], w=[P.b("XS", 0)])
        wq = self.load_w("gla_win", 0)
        wk = self.load_w("gla_win", 1)
        for h in range(4):
            for tb in range(2):
                gi = self.rot("pgu", 2)
                P.op("pe", lambda e: e.matmul(self.PU[gi][:, :], lhsT=self.SG[0][0:16, h * 128:(h + 1) * 128],
                                              rhs=self.XS[0][0:16, tb * 512:(tb + 1) * 512], start=True, stop=True),
                     r=[P.b("SG", 0), P.b("XS", 0)], w=[P.b("PU", gi)])
                P.op("act", lambda e: e.activation(out=EB[:, tb * 512:(tb + 1) * 512], in_=self.PU[gi][:, :], func=AF.Exp,
                                                   bias=self.NBG[:, h:h + 1], scale=-1.0),
                     r=[P.b("PU", gi), P.b("NBG")], w=kEB)
                P.op("act", lambda e: e.activation(out=LA[:, tb * 512:(tb + 1) * 512], in_=EB[:, tb * 512:(tb + 1) * 512],
                                                   func=AF.Ln, bias=self.ONE1[:, 0:1], scale=1.0), r=kEB + [P.b("ONES")], w=kLA)
            for c in range(8):
                P.op("dve", lambda e: e.tensor_tensor_scan(
                    out=LA[:, c * 128:(c + 1) * 128], data0=self.ONES[:, :], data1=LA[:, c * 128:(c + 1) * 128],
                    initial=0.0, op0=ALU.mult, op1=ALU.add), r=kLA + [P.b("ONES")], w=kLA)
            P.op("act", lambda e: e.activation(out=EB[:, :], in_=LA[:, :], func=AF.Exp, scale=-1.0 / 16.0), r=kLA, w=kEB)
            P.op("act", lambda e: e.activation(out=ENB[:, :], in_=LA[:, :], func=AF.Exp, scale=1.0 / 16.0), r=kLA, w=kENB)
            P.op("dve", lambda e: e.tensor_copy(out=self.EBLS[:, h, :],
                                                in_=EB.rearrange("p (c t) -> p c t", c=8)[:, :, 127]),
                 r=kEB, w=[P.b("EBLS", h)])
            for tb in range(2):
                gi = self.rot("pgu", 2)
                self.proj_fm(wq, h * 128, 128, tb, self.PG[gi], P.b("PG", gi))
                P.op("dve", lambda e: e.scalar_tensor_tensor(
                    out=self.QT[:, h, tb * 512:(tb + 1) * 512], in0=self.PG[gi][:, :], scalar=DK ** -0.5,
                    in1=EB[:, tb * 512:(tb + 1) * 512], op0=ALU.mult, op1=ALU.mult),
                    r=[P.b("PG", gi)] + kEB, w=[P.b("QT", h, tb)])
                self.proj_fm(wk, h * 128, 128, tb, self.PU[gi], P.b("PU", gi))
                P.op("dve", lambda e: e.tensor_tensor(
                    out=self.QT[:, 4 + h, tb * 512:(tb + 1) * 512], in0=self.PU[gi][:, :],
                    in1=ENB[:, tb * 512:(tb + 1) * 512], op=ALU.mult),
                    r=[P.b("PU", gi)] + kENB, w=[P.b("QT", 4 + h, tb)])
            P.op("dve", lambda e: e.tensor_tensor(
                out=self.KT[:, h, 0:1024].rearrange("p (c t) -> p c t", c=8),
                in0=self.QT[:, 4 + h, :].rearrange("p (c t) -> p c t", c=8),
                in1=self.EBLS[:, h, :].unsqueeze(2).to_broadcast([128, 8, 128]), op=ALU.mult),
                r=[P.b("QT", 4 + h, 0), P.b("QT", 4 + h, 1), P.b("EBLS", h)], w=[P.b("KT", i) for i in range(-1, 8)])
        for blk in range(2):
            wb = self.load_w("gla_win", 2 + blk)
            for tt in range(NTT):
                oi = self.rot("po", 2)
                self.proj_tm(wb, 0, 512, tt, self.PO[oi], P.b("PO", oi))
                P.op("act", lambda e: e.activation(out=ACCb[:, tt, blk * 512:(blk + 1) * 512], in_=self.PO[oi][:, :],
                                                   func=AF.Copy), r=[P.b("PO", oi)], w=[P.b("ACC", tt, 0)])
        gn = self.load_row_bc(R0 + 0)
        for blk in range(2):
            wb = self.load_w("gla_win", 4 + blk)
            for tt in range(NTT):
                oi = self.rot("po", 2)
                self.proj_tm(wb, 0, 512, tt, self.PO[oi], P.b("PO", oi))
                si = self.rot("sg", 2)
                P.op("act", lambda e: e.activation(out=self.SG[si][:, :], in_=self.PO[oi][:, :], func=AF.Silu),
                     r=[P.b("PO", oi)], w=[P.b("SG", si)])
                P.op("dve", lambda e: e.tensor_tensor(
                    out=ACCb[:, tt, 1024 + blk * 512:1024 + (blk + 1) * 512], in0=self.SG[si][:, :],
                    in1=self.GB[gn][:, blk * 512:(blk + 1) * 512], op=ALU.mult),
                    r=[P.b("SG", si), P.b("GB", gn)], w=[P.b("ACC", tt, 1)])
        for c in range(NTT):
            cs = slice(c * 128, (c + 1) * 128)
            for h in range(4):
                gi = self.rot("pgu", 2)
                P.op("pe", lambda e: e.matmul(self.PG[gi][:, 0:128], lhsT=self.QT[:, 4 + h, cs], rhs=self.QT[:, h, cs],
                                              start=True, stop=True),
                     r=[P.b("QT", 4 + h, c // 4), P.b("QT", h, c // 4)], w=[P.b("PG", gi)])
                pa = self.rot("pts", 4)
                P.op("dve", lambda e: e.tensor_tensor(out=self.PTS[pa][:, 0:128], in0=self.PG[gi][:, 0:128],
                                                      in1=self.CMASK[:, 0:128], op=ALU.mult),
                     r=[P.b("PG", gi), P.b("CMASK")], w=[P.b("PTS", pa)])
                oi = self.rot("po", 2)
                P.op("pe", lambda e: e.matmul(self.PO[oi][:, 0:256], lhsT=self.PTS[pa][:, 0:128],
                                              rhs=ACCb[:, c, h * 256:(h + 1) * 256], start=True, stop=False),
                     r=[P.b("PTS", pa), P.b("ACC", c, 0)], w=[P.b("PO", oi)])
                P.op("pe", lambda e: e.matmul(self.PO[oi][:, 0:256], lhsT=self.QT[:, h, cs], rhs=self.GSB[:, h, :],
                                              start=False, stop=True),
                     r=[P.b("QT", h, c // 4), P.b("GSB", h)], w=[P.b("PO", oi)])
                P.op("pe", lambda e: e.transpose(out=self.PT[:, 0:128], in_=self.KT[:, h, cs], identity=self.IDB[:, :]),
                     r=[P.b("KT", c), P.b("IDB")], w=[P.b("PT")])
                pk = self.rot("pts", 4)
                P.op("act", lambda e: e.activation(out=self.PTS[pk][:, 0:128], in_=self.PT[:, 0:128], func=AF.Copy),
                     r=[P.b("PT")], w=[P.b("PTS", pk)])
                P.op("pe", lambda e: e.matmul(self.PU[gi][:, 0:256], lhsT=self.PTS[pk][:, 0:128],
                                              rhs=ACCb[:, c, h * 256:(h + 1) * 256], start=True, stop=True),
                     r=[P.b("PTS", pk), P.b("ACC", c, 0)], w=[P.b("PU", gi)])
                P.op("dve", lambda e: e.scalar_tensor_tensor(
                    out=self.GS[:, h, :], in0=self.GS[:, h, :], scalar=self.EBLS[:, h, c:c + 1], in1=self.PU[gi][:, 0:256],
                    op0=ALU.mult, op1=ALU.add), r=[P.b("GS", h), P.b("EBLS", h), P.b("PU", gi)], w=[P.b("GS", h)])
                P.op("act", lambda e: e.activation(out=self.GSB[:, h, :], in_=self.GS[:, h, :], func=AF.Copy),
                     r=[P.b("GS", h)], w=[P.b("GSB", h)])
                P.op("act", lambda e: e.activation(out=self.JUNK[:, 0:256], in_=self.PO[oi][:, 0:256], func=AF.Square,
                                                   accum_out=self.SSG[:, 0:1]), r=[P.b("PO", oi)], w=[P.b("SSG")])
                self.rstd_batch(self.SSG, 1, 1.0 / 256.0, 1e-5, key="SSG")
                P.op("dve", lambda e: e.scalar_tensor_tensor(
                    out=ACCb[:, c, 1024 + h * 256:1024 + (h + 1) * 256], in0=self.PO[oi][:, 0:256],
                    scalar=self.SSG[:, 0:1], in1=ACCb[:, c, 1024 + h * 256:1024 + (h + 1) * 256],
                    op0=ALU.mult, op1=ALU.mult), r=[P.b("PO", oi), P.b("SSG"), P.b("ACC", c, 1)], w=[P.b("ACC", c, 1)])
        for tt in range(NTT):
            self.transpose_to_XT(ACCb[:, tt, 1024:2048], P.b("ACC", tt, 1), tt)
        self.out_proj("gla_wo", (), None)
        self.post_norm(l * 6 + 3, half=False)


    def proj_mix_fm(self, wa, wb_, c0, M, tb, ps, pkey):
        P = self.P
        n = 0
        for (wbuf, xt) in ((wa, self.XT), (wb_, self.XTs)):
            for kc in range(8):
                P.op("pe", lambda e: e.matmul(
                    ps[:M, :], lhsT=self.WIN[wbuf][:, kc * 512 + c0: kc * 512 + c0 + M],
                    rhs=xt[:, kc, tb * 512:(tb + 1) * 512], start=(n == 0), stop=(n == 15)),
                    r=[P.b("WIN", wbuf), P.b("XTC")] + [P.b("XT", tb * 4 + i) for i in range(max(0, -1), 4)]
                    + ([P.b("XT", tb * 4 - 1)] if tb > 0 else []), w=[pkey])
                n += 1

    def load_mix(self, name, blk, mu0, ranges):
        P = self.P
        src = self.dram[name].ap()[blk]
        P.op("pool", lambda e: e.dma_start(out=self.WIN[0][:, :], in_=src), w=[P.b("WIN", 0)], dma=True)
        for (c0, c1, mi) in ranges:
            for kc in range(8):
                P.op("act", lambda e: e.activation(out=self.WIN[1][:, kc * 512 + c0: kc * 512 + c1],
                                                   in_=self.WIN[0][:, kc * 512 + c0: kc * 512 + c1], func=AF.Copy,
                                                   scale=self.COLS[:, mu0 + mi * 8 + kc: mu0 + mi * 8 + kc + 1]),
                     r=[P.b("WIN", 0), P.b("COLS")], w=[P.b("WIN", 1)])
            for kc in range(8):
                P.op("act", lambda e: e.activation(out=self.WIN[0][:, kc * 512 + c0: kc * 512 + c1],
                                                   in_=self.WIN[0][:, kc * 512 + c0: kc * 512 + c1], func=AF.Copy,
                                                   scale=self.OMMU[:, mi * 8 + kc: mi * 8 + kc + 1]),
                     r=[P.b("WIN", 0), P.b("OMMU")], w=[P.b("WIN", 0)])

    def rwkv(self, l):
        P = self.P
        half = self.half
        R0 = self.cfg["row_rwkv"]
        C0 = self.cfg["col_rwkv"]
        CMU, CW0, CA0, CKK, CKA, CRK = C0, C0 + 48, C0 + 56, C0 + 64, C0 + 72, C0 + 80
        c0e = float(np.exp(-0.5))
        f32v = lambda t: t.rearrange("p a b -> p (a b)").bitcast(F32) if len(t.shape) == 3 else t.bitcast(F32)
        T0 = f32v(self.ATS[0][:, :, :]); k0 = [P.b("ATS", 0, a, b) for a in range(2) for b in range(2)]
        T1 = f32v(self.ATS[1][:, :, :]); k1 = [P.b("ATS", 1, a, b) for a in range(2) for b in range(2)]
        T2 = f32v(self.WOUT[0][:, :]); k2 = [P.b("WOUT", 0)]
        T3 = f32v(self.WOUT[1][:, :]); k3 = [P.b("WOUT", 1)]
        KTf = f32v(self.KT[:, :, :])
        T4 = KTf[:, 0:1024]; k4 = [P.b("KT", i) for i in range(-1, 8)]
        T5 = KTf[:, 1024:2048]; k5 = [P.b("KTb")]
        T6 = f32v(self.VA[:, :, :])[:, 0:1024]; k6 = [P.b("VA", i) for i in range(-1, 8)]
        ACCb = self.ACC[:, :, :].rearrange("p a b -> p (a b)").bitcast(BF16).rearrange("p (a b) -> p a b", a=NTT)
        AR = self.QT[:, 0:2, :].rearrange("p a (c t) -> p (a c t)", t=128).rearrange("p (c a t) -> p c a t", a=2, t=128)
        u128 = lambda ap: ap.rearrange("p (u t) -> p u t", t=128)
        u64 = lambda ap: ap.rearrange("p (u t) -> p u t", t=64)
        W0k = self.WIN[0][:, :].rearrange("p (k u t) -> p k u t", k=2, t=128)
        W1k = self.WIN[1][:, :].rearrange("p (k u t) -> p k u t", k=2, t=128)
        LAK, MRK, PN, MRB = W0k[:, 0], W0k[:, 1], W1k[:, 0], W1k[:, 1]
        PTN = u128(self.ATS[0][:, :, :].rearrange("p a b -> p (a b)"))
        XX = u128(self.ATS[1][:, :, :].rearrange("p a b -> p (a b)"))
        ATOK, BHT = u64(self.WOUT[0][:, 0:1024]), u64(self.WOUT[0][:, 1024:2048])
        KHT, W0s = u64(self.WOUT[1][:, 0:1024]), u64(self.WOUT[1][:, 1024:2048])
        KTb = self.KT[:, :, :].rearrange("p a b -> p (a b)")
        U0s, AHs, MTB = u64(KTb[:, 0:1024]), u64(KTb[:, 1024:2048]), u128(KTb[:, 2048:4096])
        VAb = self.VA[:, :, :].rearrange("p a b -> p (a b)")
        DD, SALL = u64(VAb[:, 0:1024]), u64(VAb[:, 1024:1024 + 17 * 64])
        DG = u64(self.QT[:, 6, :])
        c3t = lambda t: t.rearrange("p (c t) -> p c t", t=128)
        kAR = [P.b("QT", 0, 0), P.b("QT", 0, 1), P.b("QT", 1, 0), P.b("QT", 1, 1)]
        KTL = self.QT[:, 2, :]; kKTL = [P.b("QT", 2, 0), P.b("QT", 2, 1)]
        BTL = self.QT[:, 3, :]; kBTL = [P.b("QT", 3, 0), P.b("QT", 3, 1)]
        KH = self.QT[:, 4, :]; kKH = [P.b("QT", 4, 0), P.b("QT", 4, 1)]
        BH = self.QT[:, 5, :]; kBH = [P.b("QT", 5, 0), P.b("QT", 5, 1)]
        PB = self.QT[:, 6, :]; kPB = [P.b("QT", 6, 0), P.b("QT", 6, 1)]
        TMPB = self.QT[:, 7, :]; kTMPB = [P.b("QT", 7, 0), P.b("QT", 7, 1)]
        c3 = lambda t: t.rearrange("p (c t) -> p c t", t=64)

        self.norm_T(l * 6 + 2)
        if half == 0:
            P.op("dve", lambda e: e.memset(self.XTfull[:, :, 7:8], 0.0), w=[P.b("XTC")])
            P.op("dve", lambda e: e.memset(self.STC[:, :, :], 0.0), w=[P.b("STC", i) for i in range(8)])

        else:
            P.op("dve", lambda e: e.tensor_copy(out=self.XTfull[:, :, 7:8], in_=self.XC[:, :].unsqueeze(2)),
                 r=[P.b("XC")], w=[P.b("XTC")])
        P.op("dve", lambda e: e.tensor_scalar(out=self.OMMU[:, :], in0=self.COLS[:, CMU:CMU + 48], scalar1=-1.0,
                                              scalar2=1.0, op0=ALU.mult, op1=ALU.add), r=[P.b("COLS")], w=[P.b("OMMU")])
        P.op("dve", lambda e: e.tensor_scalar(out=self.OMKA[:, :], in0=self.COLS[:, CKA:CKA + 8], scalar1=-1.0,
                                              scalar2=1.0, op0=ALU.mult, op1=ALU.add), r=[P.b("COLS")], w=[P.b("OMKA")])
        self.load_mix("rwkv_wl1", 0, CMU, [(0, 64, 1), (64, 128, 4), (128, 288, 5)])
        for tb in range(2):
            ts = slice(tb * 512, (tb + 1) * 512)
            for (c0_, M, fn, dst, dkey) in ((0, 64, AF.Tanh, self.LW1[0:64, ts], P.b("LW1", tb)),
                                           (64, 64, AF.Copy, self.LA1[0:64, ts], P.b("LA1", tb)),
                                           (128, 128, AF.Sigmoid, self.XS[0][:, ts], P.b("XS", 0)),
                                           (256, 32, AF.Sigmoid, self.XS[1][0:32, ts], P.b("XS", 1))):
                gi = self.rot("pgu", 2)
                self.proj_mix_fm(0, 1, c0_, M, tb, self.PG[gi], P.b("PG", gi))
                P.op("act", lambda e: e.activation(out=dst, in_=self.PG[gi][0:M, :], func=fn),
                     r=[P.b("PG", gi)], w=[dkey])
        P.op("pool", lambda e: e.dma_start(out=self.L2W[0:64, :], in_=self.dram["rwkv_w2"].ap()[:, :]), w=[P.b("L2W")], dma=True)
        P.op("pool", lambda e: e.dma_start(out=self.L2A[0:64, :], in_=self.dram["rwkv_a2"].ap()[:, :]), w=[P.b("L2A")], dma=True)
        for blk in range(2):
            self.load_mix("rwkv_wv", blk, CMU, [(0, 512, 3)])
            for tt in range(NTT):
                oi = self.rot("po", 2)
                n = 0
                for (wbuf, xt) in ((0, self.XT), (1, self.XTs)):
                    for kc in range(8):
                        P.op("pe", lambda e: e.matmul(
                            self.PO[oi][:, :], lhsT=xt[:, kc, tt * 128:(tt + 1) * 128],
                            rhs=self.WIN[wbuf][:, kc * 512:(kc + 1) * 512], start=(n == 0), stop=(n == 15)),
                            r=[P.b("WIN", wbuf), P.b("XT", tt), P.b("XTC")] + ([P.b("XT", tt - 1)] if tt > 0 else []),
                            w=[P.b("PO", oi)])
                        n += 1
                P.op("act", lambda e: e.activation(out=ACCb[:, tt, blk * 512:(blk + 1) * 512], in_=self.PO[oi][:, :],
                                                   func=AF.Copy), r=[P.b("PO", oi)], w=[P.b("ACC", tt, 0)])
        for kc in range(8):
            blk, cc = kc // 4, (kc % 4) * 128
            self.load_mix("rwkv_wr", blk, CMU, [(cc, cc + 128, 0)])
            for tb in range(2):
                gi = self.rot("pgu", 2)
                self.proj_mix_fm(0, 1, cc, 128, tb, self.PG[gi], P.b("PG", gi))
                P.op("act", lambda e: e.activation(out=T0[:, tb * 512:(tb + 1) * 512], in_=self.PG[gi][:, :], func=AF.Copy),
                     r=[P.b("PG", gi)], w=k0)
            self.load_mix("rwkv_wk", blk, CMU, [(cc, cc + 128, 2)])
            for tb in range(2):
                gi = self.rot("pgu", 2)
                self.proj_mix_fm(0, 1, cc, 128, tb, self.PG[gi], P.b("PG", gi))
                P.op("act", lambda e: e.activation(out=T1[:, tb * 512:(tb + 1) * 512], in_=self.PG[gi][:, :], func=AF.Copy),
                     r=[P.b("PG", gi)], w=k1)
            for tb in range(2):
                ts = slice(tb * 512, (tb + 1) * 512)
                gi = self.rot("pgu", 2)
                P.op("pe", lambda e: e.matmul(self.PU[gi][:, :], lhsT=self.L2W[0:64, kc * 128:(kc + 1) * 128],
                                              rhs=self.LW1[0:64, ts], start=True, stop=True),
                     r=[P.b("L2W"), P.b("LW1", tb)], w=[P.b("PU", gi)])
                P.op("act", lambda e: e.activation(out=T2[:, ts], in_=self.PU[gi][:, :], func=AF.Sigmoid,
                                                   bias=self.COLS[:, CW0 + kc:CW0 + kc + 1], scale=1.0),
                     r=[P.b("PU", gi), P.b("COLS")], w=k2)
            for c in range(16):
                cs = slice(c * 64, (c + 1) * 64)
                P.op("dve", lambda e: e.tensor_tensor_scan(out=T3[:, cs], data0=self.ONES[:, 0:64], data1=T2[:, cs],
                                                           initial=0.0, op0=ALU.mult, op1=ALU.add),
                     r=k2 + [P.b("ONES")], w=k3)
            P.op("dve", lambda e: e.tensor_tensor(out=T4[:, :], in0=T3[:, :], in1=T2[:, :], op=ALU.subtract),
                 r=k2 + k3, w=k4)
            P.op("act", lambda e: e.activation(out=T4[:, :], in_=T4[:, :], func=AF.Exp, scale=-c0e), r=k4, w=k4)
            P.op("act", lambda e: e.activation(out=T5[:, :], in_=T3[:, :], func=AF.Exp, scale=-c0e), r=k3, w=k5)
            P.op("act", lambda e: e.activation(out=T3[:, :], in_=T3[:, :], func=AF.Exp, scale=c0e), r=k3, w=k3)
            P.op("dve", lambda e: e.tensor_copy(out=self.GCC[:, :], in_=c3(T5)[:, :, 63]), r=k5, w=[P.b("GCC")])
            for tb in range(2):
                ts = slice(tb * 512, (tb + 1) * 512)
                gi = self.rot("pgu", 2)
                P.op("pe", lambda e: e.matmul(self.PU[gi][:, :], lhsT=self.L2A[0:64, kc * 128:(kc + 1) * 128],
                                              rhs=self.LA1[0:64, ts], start=True, stop=True),
                     r=[P.b("L2A"), P.b("LA1", tb)], w=[P.b("PU", gi)])
                P.op("act", lambda e: e.activation(out=T2[:, ts], in_=self.PU[gi][:, :], func=AF.Sigmoid,
                                                   bias=self.COLS[:, CA0 + kc:CA0 + kc + 1], scale=1.0),
                     r=[P.b("PU", gi), P.b("COLS")], w=k2)
            P.op("dve", lambda e: e.tensor_scalar(out=T6[:, :], in0=T1[:, :], scalar1=self.COLS[:, CKK + kc:CKK + kc + 1],
                                                  scalar2=None, op0=ALU.mult), r=k1 + [P.b("COLS")], w=k6)
            P.op("dve", lambda e: e.tensor_tensor(out=TMPB, in0=T6[:, :], in1=T6[:, :], op=ALU.mult), r=k6, w=kTMPB)
            for tb in range(2):
                ts = slice(tb * 512, (tb + 1) * 512)
                gi = self.rot("pgu", 2)
                P.op("pe", lambda e: e.matmul(self.PU[gi][:, :], lhsT=self.BLK[:, :], rhs=TMPB[:, ts], start=True, stop=True),
                     r=[P.b("BLK")] + kTMPB, w=[P.b("PU", gi)])
                P.op("act", lambda e: e.activation(out=self.GB[0][:, 0:512], in_=self.PU[gi][:, :], func=AF.Sqrt),
                     r=[P.b("PU", gi)], w=[P.b("GB", 0)])
                P.op("dve", lambda e: e.tensor_scalar(out=self.GB[0][:, 0:512], in0=self.GB[0][:, 0:512], scalar1=1e-12,
                                                      scalar2=None, op0=ALU.max), r=[P.b("GB", 0)], w=[P.b("GB", 0)])
                P.op("dve", lambda e: e.reciprocal(out=self.GB[0][:, 0:512], in_=self.GB[0][:, 0:512]),
                     r=[P.b("GB", 0)], w=[P.b("GB", 0)])
                P.op("dve", lambda e: e.tensor_tensor(out=T6[:, ts], in0=T6[:, ts], in1=self.GB[0][:, 0:512], op=ALU.mult),
                     r=k6 + [P.b("GB", 0)], w=k6)
            P.op("dve", lambda e: e.tensor_scalar(out=TMPB, in0=T2[:, :], scalar1=self.COLS[:, CKA + kc:CKA + kc + 1],
                                                  scalar2=self.OMKA[:, kc:kc + 1], op0=ALU.mult, op1=ALU.add),
                 r=k2 + [P.b("COLS"), P.b("OMKA")], w=kTMPB)
            P.op("dve", lambda e: e.tensor_tensor(out=T1[:, :], in0=T1[:, :], in1=TMPB, op=ALU.mult), r=k1 + kTMPB, w=k1)
            P.op("dve", lambda e: e.scalar_tensor_tensor(out=AR[:, :, 0, :], in0=c3t(T6), scalar=-1.0, in1=c3t(T4),
                                                         op0=ALU.mult, op1=ALU.mult), r=k6 + k4, w=kAR)
            P.op("dve", lambda e: e.tensor_tensor(out=AR[:, :, 1, :], in0=c3t(T0), in1=c3t(T5), op=ALU.mult), r=k0 + k5, w=kAR)
            P.op("dve", lambda e: e.tensor_tensor(out=KTL, in0=T1[:, :], in1=T3[:, :], op=ALU.mult), r=k1 + k3, w=kKTL)
            P.op("dve", lambda e: e.tensor_tensor(out=T6[:, :], in0=T6[:, :], in1=T2[:, :], op=ALU.mult), r=k6 + k2, w=k6)
            P.op("dve", lambda e: e.tensor_tensor(out=BTL, in0=T6[:, :], in1=T3[:, :], op=ALU.mult), r=k6 + k3, w=kBTL)
            gcb = self.GCC[:, :].unsqueeze(2).to_broadcast([128, 16, 64])
            P.op("dve", lambda e: e.tensor_tensor(out=c3(KH), in0=c3(KTL), in1=gcb, op=ALU.mult), r=kKTL + [P.b("GCC")], w=kKH)
            P.op("dve", lambda e: e.tensor_tensor(out=c3(BH), in0=c3(BTL), in1=gcb, op=ALU.mult), r=kBTL + [P.b("GCC")], w=kBH)
            P.op("dve", lambda e: e.scalar_tensor_tensor(out=PB, in0=T0[:, :], scalar=self.COLS[:, CRK + kc:CRK + kc + 1],
                                                         in1=T1[:, :], op0=ALU.mult, op1=ALU.mult),
                 r=k0 + k1 + [P.b("COLS")], w=kPB)
            gi = self.rot("pgu", 2)
            for tt in range(NTT):
                P.op("pe", lambda e: e.matmul(self.PU[gi][:, tt * 2:tt * 2 + 2], lhsT=PB[:, tt * 128:(tt + 1) * 128],
                                              rhs=self.HSEL[:, :], start=True, stop=True),
                     r=kPB + [P.b("HSEL")], w=[P.b("PU", gi)])
            P.op("act", lambda e: e.activation(out=self.BS[:, :, 2 * kc:2 * kc + 2],
                                               in_=self.PU[gi][:, 0:16].rearrange("p (t h) -> p t h", h=2), func=AF.Copy),
                 r=[P.b("PU", gi)], w=[P.b("BS", kc)])
            prs = [slice(0, 64), slice(64, 128)]
            KB = lambda kind, bt: P.b("RK", kind, bt)
            kall = lambda kind: [P.b("RK", kind, bt) for bt in range(4)]
            alias = k0 + k1 + k2 + k3 + k4 + k5 + k6 + kPB + [P.b("WIN", 0), P.b("WIN", 1)]
            rkk = [P.b("RK", kd, bt) for kd in ("LAK", "MRK", "PN", "MRB", "PTN", "XX", "ATOK", "BHT", "KHT", "W0", "U0", "AH", "MTB")
                   for bt in range(4)] + [P.b("DD"), P.b("DG")] + [P.b("SALL", i) for i in range(17)]
            P.op("pool", lambda e: e.memset(self.SEMT[:, 0:1], 0.0), w=alias + rkk + [P.b("SEMT")])
            P.op("pool", lambda e: e.memset(MTB[:, :, :], 0.0), w=kall("MTB"))
            P.op("pool", lambda e: e.tensor_tensor(out=DG, in0=self.ID2[:, :].unsqueeze(1).to_broadcast([128, 16, 64]),
                                                   in1=self.GCC[:, :].unsqueeze(2).to_broadcast([128, 16, 64]), op=ALU.mult),
                 r=[P.b("GCC"), P.b("RMASK")], w=[P.b("DG")])
            P.op("act", lambda e: e.activation(out=SALL[:, 0, :], in_=self.STC[:, kc, :], func=AF.Copy),
                 r=[P.b("STC", kc)], w=[P.b("SALL", 0)])
            for cp in range(8):
                tl = slice(cp * 128, (cp + 1) * 128)
                for hp in range(2):
                    pr = prs[hp]
                    u = cp * 2 + hp
                    bt = u // 4
                    for (src, off, key) in ((KTL, 0, kKTL), (BTL, 256, kBTL)):
                        P.op("pe", lambda e: e.matmul(self.PO[hp][:, off:off + 256], lhsT=src[pr, tl],
                                                      rhs=AR[pr, cp, :, :], start=True, stop=True),
                             r=key + kAR, w=[P.b("PO", hp)])
                    P.op("pe", lambda e: e.matmul(self.PU[hp][:, 0:128], lhsT=AR[pr, cp, 0, :], rhs=BTL[pr, tl],
                                                  start=True, stop=True), r=kAR + kBTL, w=[P.b("PU", hp)])
                    m2 = self.RMASK[:, 0:256].rearrange("p (a t) -> p a t", a=2)
                    P.op("dve", lambda e: e.tensor_tensor(out=W0k[:, :, u, :], in0=self.PO[hp][:, 0:256].rearrange("p (a t) -> p a t", a=2),
                                                          in1=m2, op=ALU.mult),
                         r=[P.b("PO", hp), P.b("RMASK")], w=[KB("LAK", bt), KB("MRK", bt)])
                    P.op("dve", lambda e: e.tensor_tensor(out=W1k[:, :, u, :], in0=self.PO[hp][:, 256:512].rearrange("p (a t) -> p a t", a=2),
                                                          in1=m2, op=ALU.mult),
                         r=[P.b("PO", hp), P.b("RMASK")], w=[KB("PN", bt), KB("MRB", bt)])
                    P.op("dve", lambda e: e.tensor_tensor(out=PTN[:, u, :], in0=self.PU[hp][:, 0:128], in1=self.RMASK[:, 256:384], op=ALU.mult),
                         r=[P.b("PU", hp), P.b("RMASK")], w=[KB("PTN", bt)])
            for bt in range(4):
                P.op("pool", lambda e: e.tensor_tensor(out=XX[:, bt * 4:(bt + 1) * 4, :], in0=PN[:, bt * 4:(bt + 1) * 4, :],
                                                       in1=self.IDB[:, :].unsqueeze(1).to_broadcast([128, 4, 128]), op=ALU.add),
                     r=[KB("PN", bt), P.b("IDB")], w=[KB("XX", bt)])
            for (dst, dkind, srcf, skey) in ((ATOK, "ATOK", lambda cp, pr: AR[pr, cp, 0, :], kAR),
                                             (BHT, "BHT", lambda cp, pr: BH[pr, cp * 128:(cp + 1) * 128], kBH),
                                             (KHT, "KHT", lambda cp, pr: KH[pr, cp * 128:(cp + 1) * 128], kKH)):
                for u in range(16):
                    cp, hp = u // 2, u % 2
                    pt = self.PT if hp == 0 else self.PT2
                    P.op("pe", lambda e: e.transpose(out=pt[:, cp * 64:(cp + 1) * 64], in_=srcf(cp, prs[hp]),
                                                     identity=self.IDB[prs[hp], hp * 64:(hp + 1) * 64]),
                         r=skey + [P.b("IDB")], w=[P.b("PT" if hp == 0 else "PT2")])
                for hp in range(2):
                    pt = self.PT if hp == 0 else self.PT2
                    P.op("act" if hp == 0 else "dve", lambda e: (e.activation(
                        out=dst[:, hp:16:2, :], in_=pt[:, 0:512].rearrange("p (u t) -> p u t", t=64), func=AF.Copy) if hp == 0 else
                        e.tensor_copy(out=dst[:, hp:16:2, :], in_=pt[:, 0:512].rearrange("p (u t) -> p u t", t=64))),
                        r=[P.b("PT" if hp == 0 else "PT2")], w=kall(dkind))
            for m in range(5):
                for bt in range(4):
                    us = range(bt * 4, bt * 4 + 4)
                    pb = bt % 2
                    for i, u in enumerate(us):
                        P.op("pe", lambda e: e.matmul(self.PG[pb][:, i * 128:(i + 1) * 128], lhsT=PN[:, u, :], rhs=PTN[:, u, :],
                                                      start=True, stop=True), r=[KB("PN", bt), KB("PTN", bt)], w=[P.b("PG", pb)])
                    if m < 4:
                        for i, u in enumerate(us):
                            P.op("pe", lambda e: e.matmul(self.PU[pb][:, i * 128:(i + 1) * 128], lhsT=PTN[:, u, :], rhs=PN[:, u, :],
                                                          start=True, stop=True), r=[KB("PN", bt), KB("PTN", bt)], w=[P.b("PU", pb)])
                    P.op("act", lambda e: e.activation(out=PTN[:, bt * 4:(bt + 1) * 4, :],
                                                       in_=self.PG[pb][:, :].rearrange("p (u t) -> p u t", t=128), func=AF.Copy),
                         r=[P.b("PG", pb)], w=[KB("PTN", bt)])
                    if m < 4:
                        P.op("dve", lambda e: e.tensor_copy(out=PN[:, bt * 4:(bt + 1) * 4, :],
                                                            in_=self.PU[pb][:, :].rearrange("p (u t) -> p u t", t=128)),
                             r=[P.b("PU", pb)], w=[KB("PN", bt)])
                    for i, u in enumerate(us):
                        P.op("pe", lambda e: e.matmul(self.PO[pb][:, i * 128:(i + 1) * 128], lhsT=PTN[:, u, :], rhs=XX[:, u, :],
                                                      start=True, stop=True), r=[KB("PTN", bt), KB("XX", bt)], w=[P.b("PO", pb)])
                    P.op("dve", lambda e: e.tensor_tensor(out=XX[:, bt * 4:(bt + 1) * 4, :], in0=XX[:, bt * 4:(bt + 1) * 4, :],
                                                          in1=self.PO[pb][:, :].rearrange("p (u t) -> p u t", t=128), op=ALU.add),
                         r=[KB("XX", bt), P.b("PO", pb)], w=[KB("XX", bt)])
            vcol = lambda u: ACCb[:, u // 2, (2 * kc + u % 2) * 64:(2 * kc + u % 2 + 1) * 64]
            for b8 in range(2):
                for i in range(8):
                    u = b8 * 8 + i
                    P.op("pe", lambda e: e.matmul(self.PG[b8][:, i * 64:(i + 1) * 64], lhsT=LAK[:, u, :], rhs=vcol(u), start=True, stop=True),
                         r=[KB("LAK", u // 4), P.b("ACC", u // 2, 0)], w=[P.b("PG", b8)])
                P.op("act", lambda e: e.activation(out=W0s[:, b8 * 8:(b8 + 1) * 8, :], in_=self.PG[b8][:, :].rearrange("p (u t) -> p u t", t=64),
                                                   func=AF.Copy), r=[P.b("PG", b8)], w=[KB("W0", 2 * b8), KB("W0", 2 * b8 + 1)])
            for b8 in range(2):
                for i in range(8):
                    u = b8 * 8 + i
                    P.op("pe", lambda e: e.matmul(self.PO[b8][:, i * 64:(i + 1) * 64], lhsT=XX[:, u, :], rhs=ATOK[:, u, :], start=True, stop=True),
                         r=[KB("XX", u // 4), KB("ATOK", u // 4)], w=[P.b("PO", b8)])
                P.op("dve", lambda e: e.tensor_copy(out=AHs[:, b8 * 8:(b8 + 1) * 8, :], in_=self.PO[b8][:, :].rearrange("p (u t) -> p u t", t=64)),
                     r=[P.b("PO", b8)], w=[KB("AH", 2 * b8), KB("AH", 2 * b8 + 1)])
            for b8 in range(2):
                for i in range(8):
                    u = b8 * 8 + i
                    P.op("pe", lambda e: e.matmul(self.PU[b8][:, i * 64:(i + 1) * 64], lhsT=XX[:, u, :], rhs=W0s[:, u, :], start=True, stop=True),
                         r=[KB("XX", u // 4), KB("W0", u // 4)], w=[P.b("PU", b8)])
                P.op("act", lambda e: e.activation(out=U0s[:, b8 * 8:(b8 + 1) * 8, :], in_=self.PU[b8][:, :].rearrange("p (u t) -> p u t", t=64),
                                                   func=AF.Copy), r=[P.b("PU", b8)], w=[KB("U0", 2 * b8), KB("U0", 2 * b8 + 1)])
            for cpar in range(2):
                tp = prs[cpar]
                for u in range(16):
                    cp, hp = u // 2, u % 2
                    P.op("pe", lambda e: e.matmul(self.PG[cpar][prs[hp], cp * 64:(cp + 1) * 64], lhsT=AHs[tp, u, :], rhs=BHT[tp, u, :],
                                                  start=True, stop=True), r=[KB("AH", u // 4), KB("BHT", u // 4)], w=[P.b("PG", cpar)])
                for hp in range(2):
                    P.op("dve", lambda e: e.tensor_tensor(
                        out=MTB[prs[hp], cpar:16:2, hp * 64:(hp + 1) * 64],
                        in0=self.PG[cpar][prs[hp], :].rearrange("p (c t) -> p c t", t=64),
                        in1=DG[prs[hp], cpar:16:2, :], op=ALU.add),
                        r=[P.b("PG", cpar), P.b("DG")], w=kall("MTB"))
                for u in range(16):
                    cp, hp = u // 2, u % 2
                    P.op("pe", lambda e: e.matmul(self.PU[cpar][prs[hp], cp * 64:(cp + 1) * 64], lhsT=BHT[tp, u, :], rhs=U0s[tp, u, :],
                                                  start=True, stop=False), r=[KB("BHT", u // 4), KB("U0", u // 4)], w=[P.b("PU", cpar)])
                    P.op("pe", lambda e: e.matmul(self.PU[cpar][prs[hp], cp * 64:(cp + 1) * 64], lhsT=KHT[tp, u, :],
                                                  rhs=ACCb[tp, cp, (2 * kc + hp) * 64:(2 * kc + hp + 1) * 64],
                                                  start=False, stop=True), r=[KB("KHT", u // 4), P.b("ACC", cp, 0)], w=[P.b("PU", cpar)])
                P.op("act", lambda e: e.activation(out=DD[:, cpar:16:2, :], in_=self.PU[cpar][:, :].rearrange("p (c t) -> p c t", t=64),
                                                   func=AF.Copy), r=[P.b("PU", cpar)], w=[P.b("DD")])
            for b4 in range(2):
                for i in range(4):
                    cp = b4 * 4 + i
                    for hp in range(2):
                        u = cp * 2 + hp
                        P.op("pe", lambda e: e.matmul(self.PO[b4][prs[hp], i * 128:(i + 1) * 128], lhsT=AHs[:, u, :], rhs=MRB[:, u, :],
                                                      start=True, stop=True), r=[KB("AH", u // 4), KB("MRB", u // 4)], w=[P.b("PO", b4)])
                P.op("dve", lambda e: e.tensor_tensor(out=AR[:, b4 * 4:(b4 + 1) * 4, 1, :], in0=AR[:, b4 * 4:(b4 + 1) * 4, 1, :],
                                                      in1=self.PO[b4][:, :].rearrange("p (c t) -> p c t", t=128), op=ALU.add),
                     r=kAR + [P.b("PO", b4)], w=kAR)
            for c in range(16):
                pb = c % 2
                P.op("pe", lambda e: e.matmul(self.PG[pb][:, 0:64], lhsT=MTB[:, c, :], rhs=SALL[:, c, :], start=True, stop=True),
                     r=kall("MTB") + [P.b("SALL", c)], w=[P.b("PG", pb)])
                P.op("dve", lambda e: e.tensor_tensor(out=SALL[:, c + 1, :], in0=self.PG[pb][:, 0:64], in1=DD[:, c, :], op=ALU.add),
                     r=[P.b("PG", pb), P.b("DD")], w=[P.b("SALL", c + 1)])
            P.op("act", lambda e: e.activation(out=self.STC[:, kc, :], in_=SALL[:, 16, :], func=AF.Copy),
                 r=[P.b("SALL", 16)], w=[P.b("STC", kc)])
            for hp in range(2):
                pr = prs[hp]
                for cp in range(8):
                    u = cp * 2 + hp
                    oc = slice(cp * 64, (cp + 1) * 64)
                    P.op("pe", lambda e: e.matmul(self.PO[hp][:, oc], lhsT=MRB[:, u, :], rhs=U0s[:, u, :], start=True, stop=False),
                         r=[KB("MRB", u // 4), KB("U0", u // 4)], w=[P.b("PO", hp)])
                    P.op("pe", lambda e: e.matmul(self.PO[hp][:, oc], lhsT=MRK[:, u, :], rhs=vcol(u), start=False, stop=False),
                         r=[KB("MRK", u // 4), P.b("ACC", cp, 0)], w=[P.b("PO", hp)])
                    for cpar in range(2):
                        c = 2 * cp + cpar
                        P.op("pe", lambda e: e.matmul(self.PO[hp][prs[cpar], oc], lhsT=AR[pr, cp, 1, cpar * 64:(cpar + 1) * 64],
                                                      rhs=SALL[pr, c, :], start=False, stop=True),
                             r=kAR + [P.b("SALL", c)], w=[P.b("PO", hp)])
                hd = 2 * kc + hp
                P.op("act", lambda e: e.activation(out=ACCb[:, :, 1024 + hd * 64:1024 + (hd + 1) * 64],
                                                   in_=self.PO[hp][:, :].rearrange("p (c t) -> p c t", t=64), func=AF.Copy),
                     r=[P.b("PO", hp)], w=[P.b("ACC", tt, 1) for tt in range(8)])
            P.op("pool", lambda e: e.memset(self.SEMT[:, 0:1], 0.0), w=alias + rkk + [P.b("SEMT")])
        if half == 0:
            P.op("dve", lambda e: e.tensor_copy(out=self.XC[:, :].unsqueeze(2), in_=self.XTfull[:, :, 8 + TB - 1:8 + TB]),
                 r=[P.b("XT", 7)], w=[P.b("XC")])
        g1 = self.load_row_bc(R0 + 0)
        g2 = self.load_row_bc(R0 + 1)
        h3 = lambda t: t.rearrange("p (h d) -> p h d", d=64)
        for c in range(NTT):
            yb = ACCb[:, c, 1024:2048]
            ky = [P.b("ACC", c, 1)]
            P.op("dve", lambda e: e.tensor_reduce(out=self.S1[:, :], in_=h3(yb), axis=mybir.AxisListType.X, op=ALU.add),
                 r=ky, w=[P.b("S1")])
            P.op("dve", lambda e: e.tensor_tensor(out=T0[:, :], in0=yb, in1=yb, op=ALU.mult), r=ky, w=k0)
            P.op("dve", lambda e: e.tensor_reduce(out=self.S2[:, :], in_=h3(T0[:, :]), axis=mybir.AxisListType.X, op=ALU.add),
                 r=k0, w=[P.b("S2")])
            P.op("dve", lambda e: e.tensor_scalar(out=self.S1[:, :], in0=self.S1[:, :], scalar1=1.0 / 64, scalar2=None,
                                                  op0=ALU.mult), r=[P.b("S1")], w=[P.b("S1")])
            P.op("dve", lambda e: e.tensor_tensor(out=self.S3[:, :], in0=self.S1[:, :], in1=self.S1[:, :], op=ALU.mult),
                 r=[P.b("S1")], w=[P.b("S3")])
            P.op("dve", lambda e: e.scalar_tensor_tensor(out=self.S2[:, :], in0=self.S2[:, :], scalar=1.0 / 64, in1=self.S3[:, :],
                                                         op0=ALU.mult, op1=ALU.subtract), r=[P.b("S2"), P.b("S3")], w=[P.b("S2")])
            self.rstd_batch(self.S2, 16, 1.0, 64e-5, key="S2")
            P.op("dve", lambda e: e.tensor_tensor(out=h3(T0[:, :]), in0=h3(yb), in1=self.S1[:, :].unsqueeze(2).to_broadcast([128, 16, 64]),
                                                  op=ALU.subtract), r=ky + [P.b("S1")], w=k0)
            P.op("dve", lambda e: e.tensor_tensor(out=h3(T0[:, :]), in0=h3(T0[:, :]), in1=self.S2[:, :].unsqueeze(2).to_broadcast([128, 16, 64]),
                                                  op=ALU.mult), r=k0 + [P.b("S2")], w=k0)
            P.op("dve", lambda e: e.tensor_tensor(out=T0[:, :], in0=T0[:, :], in1=self.GB[g1][:, :], op=ALU.mult),
                 r=k0 + [P.b("GB", g1)], w=k0)
            P.op("dve", lambda e: e.tensor_tensor(out=T0[:, :], in0=T0[:, :], in1=self.GB[g2][:, :], op=ALU.add),
                 r=k0 + [P.b("GB", g2)], w=k0)
            P.op("dve", lambda e: e.tensor_tensor(out=h3(T1[:, :]), in0=h3(ACCb[:, c, 0:1024]),
                                                  in1=self.BS[:, c, :].unsqueeze(2).to_broadcast([128, 16, 64]), op=ALU.mult),
                 r=[P.b("ACC", c, 0)] + [P.b("BS", i) for i in range(8)], w=k1)
            P.op("dve", lambda e: e.tensor_tensor(out=yb, in0=T0[:, :], in1=T1[:, :], op=ALU.add), r=k0 + k1, w=ky)
        P.op("pool", lambda e: e.dma_start(out=self.L2W[:, :], in_=self.dram["rwkv_g2"].ap()[0:128, :]), w=[P.b("L2W")], dma=True)
        P.op("pool", lambda e: e.dma_start(out=self.L2A[0:32, :], in_=self.dram["rwkv_g2"].ap()[128:160, :]), w=[P.b("L2A")], dma=True)
        for tt in range(NTT):
            tsl = slice(tt * 128, (tt + 1) * 128)
            for kc in range(8):
                P.op("pe", lambda e: e.transpose(out=self.PT[:, kc * 128:(kc + 1) * 128], in_=ACCb[:, tt, 1024 + kc * 128:1024 + (kc + 1) * 128],
                                                 identity=self.IDB[:, :]), r=[P.b("ACC", tt, 1), P.b("IDB")], w=[P.b("PT")])
            for kc in range(8):
                pg = self.PG[kc // 4]
                P.op("pe", lambda e: e.matmul(pg[:, (kc % 4) * 128:(kc % 4 + 1) * 128], lhsT=self.L2W[:, kc * 128:(kc + 1) * 128],
                                              rhs=self.XS[0][:, tsl], start=True, stop=False),
                     r=[P.b("L2W"), P.b("XS", 0)], w=[P.b("PG", kc // 4)])
                P.op("pe", lambda e: e.matmul(pg[:, (kc % 4) * 128:(kc % 4 + 1) * 128], lhsT=self.L2A[0:32, kc * 128:(kc + 1) * 128],
                                              rhs=self.XS[1][0:32, tsl], start=False, stop=True),
                     r=[P.b("L2A"), P.b("XS", 1)], w=[P.b("PG", kc // 4)])
            for hh in range(2):
                P.op("act", lambda e: e.activation(out=TMPB[:, hh * 512:(hh + 1) * 512], in_=self.PG[hh][:, :], func=AF.Copy),
                     r=[P.b("PG", hh)], w=kTMPB)
            P.op("dve", lambda e: e.tensor_tensor(out=self.XT[:, :, tsl], in0=self.PT[:, :].rearrange("p (k c) -> p k c", k=8),
                                                  in1=TMPB.rearrange("p (k c) -> p k c", k=8), op=ALU.mult),
                 r=[P.b("PT")] + kTMPB, w=[P.b("XT", tt)])
        self.out_proj("rwkv_wo", (), None)
        self.post_norm(l * 6 + 3, half=False)

    def build(self):
        nc, P, cfg = self.nc, self.P, self.cfg
        x_d = self.din("x", [SEQ, D])
        self.din("rows", [cfg["nrows"], D])
        self.din("ffn_win", [DEPTH, 2, NSLAB, 128, 8 * 512])
        self.din("ffn_wout", [DEPTH, 2, NSLAB, 128, 2 * 1024])
        self.din("idb", [128, 128], BF16)
        self.din("cmask", [128, 1024], BF16)
        self.din("cols", [128, cfg["ncols"]])
        self.din("swa_wq", [2, 2, 128, 4096])
        self.din("gla_win", [7, 128, 4096])
        self.din("rwkv_wl1", [1, 128, 4096])
        self.din("rwkv_wr", [2, 128, 4096])
        self.din("rwkv_wk", [2, 128, 4096])
        self.din("rwkv_wv", [2, 128, 4096])
        self.din("rwkv_wo", [2, 128, 4096])
        self.din("rwkv_w2", [64, 1024])
        self.din("rwkv_a2", [64, 1024])
        self.din("rwkv_g2", [160, 1024])
        self.din("rconst", [128, 384 + 128 + 2 + 64], BF16)
        self.din("gla_wo", [2, 128, 4096])
        self.din("gla_wg2", [16, 512])
        self.din("swa_wk", [2, 128, 4096])
        self.din("swa_wv", [2, 128, 4096])
        self.din("swa_wo", [2, 2, 128, 4096])
        y_d = self.nc.dram_tensor("y", [SEQ, D], F32, kind="ExternalOutput")

        self.H = self.sb("H", [128, NTT, D], F32)
        self.XTfull = self.sb("XTfull", [128, 8, TB + 8], BF16)
        self.XT = self.XTfull[:, :, 8:8 + TB]
        self.XTs = self.XTfull[:, :, 7:7 + TB]
        self.RCONST = self.sb("RCONST", [128, 384 + 128 + 2 + 64], BF16)
        self.RMASK = self.RCONST[:, 0:384]
        self.BLK = self.RCONST[:, 384:512]
        self.HSEL = self.RCONST[:, 512:514]
        self.ID2 = self.RCONST[:, 514:578]
        self.STC = self.sb("STC", [128, 8, 64], BF16)
        self.SEMT = self.sb("SEMT", [128, 4], F32)
        self.XC = self.sb("XC", [128, 8], BF16)
        self.OMMU = self.sb("OMMU", [128, 48], F32)
        self.OMKA = self.sb("OMKA", [128, 8], F32)
        self.GCC = self.sb("GCC", [128, 16], F32)
        self.BS = self.sb("BS", [128, 8, 16], F32)
        self.S1 = self.sb("S1", [128, 16], F32)
        self.S2 = self.sb("S2", [128, 16], F32)
        self.S3 = self.sb("S3", [128, 16], F32)
        self.LW1 = self.sb("LW1", [64, TB], BF16)
        self.LA1 = self.sb("LA1", [64, TB], BF16)
        self.L2W = self.sb("L2W", [128, D], BF16)
        self.L2A = self.sb("L2A", [64, D], BF16)
        self.ACC = self.sb("ACC", [128, NTT, D], F32)
        self.ATS = [self.sb("ATS%d" % i, [128, 2, TB], BF16) for i in range(2)]
        self.WIN = [self.sb("WIN%d" % i, [128, 8 * 512], BF16) for i in range(2)]
        self.WOUT = [self.sb("WOUT%d" % i, [128, 2 * 1024], BF16) for i in range(2)]
        self.GB = [self.sb("GB%d" % i, [128, D], F32) for i in range(2)]
        self.XS = [self.sb("XS%d" % i, [128, D], BF16) for i in range(2)]
        self.SG = [self.sb("SG%d" % i, [128, 512], BF16) for i in range(2)]
        self.JUNK = self.sb("JUNK", [128, D], BF16)
        self.SS = self.sb("SS", [128, 16], F32)
        self.IDB = self.sb("IDB", [128, 128], BF16)
        self.epsc = {EPS: self.sb("eps0", [128, 1], F32), 4 * EPS: self.sb("eps4", [128, 1], F32), 1e-5: self.sb("eps1", [128, 1], F32),
                     64e-5: self.sb("eps2", [128, 1], F32)}
        self.GS = self.sb("GS", [128, 4, 256], F32)
        self.GSB = self.sb("GSB", [128, 4, 256], BF16)
        self.EBLS = self.sb("EBLS", [128, 4, 8], F32)
        self.ONES = self.sb("ONES", [128, 128], F32)
        self.ONE1 = self.ONES
        self.NBG = self.sb("NBG", [128, 4], F32)
        self.SSG = self.sb("SSG", [128, 4], F32)
        self.CMASK = self.sb("CMASK", [128, 1024], BF16)
        self.COLS = self.sb("COLS", [128, cfg["ncols"]], F32)
        self.QT = self.sb("QT", [128, 8, TB], BF16)
        self.KT = self.sb("KT", [128, 4, TB + 128], BF16)
        self.VA = self.sb("VA", [128, 9, 260], BF16)
        self.KC = [self.sb("KC%d" % i, [128, 4, 128], BF16) for i in range(2)]
        self.VC = [self.sb("VC%d" % i, [128, 260], BF16) for i in range(2)]
        self.PTS = [self.sb("PTS%d" % i, [128, 512], BF16) for i in range(4)]
        self.ESK = self.sb("ESK", [128, 16], F32)
        self.BQ8 = self.sb("BQ8", [128, 8], F32)
        self.DEN = self.sb("DEN", [128, 4], F32)

        self.PG = [self.ps("PG%d" % i, [128, 512], F32) for i in range(2)]
        self.PU = [self.ps("PU%d" % i, [128, 512], F32) for i in range(2)]
        self.PO = [self.ps("PO%d" % i, [128, 512], F32) for i in range(2)]
        self.PT = self.ps("PT", [128, 1024], BF16)
        self.PT2 = self.ps("PT2", [128, 1024], BF16)

        P.op("sp", lambda e: e.dma_start(out=self.IDB[:, :], in_=self.dram["idb"].ap()[:, :]), w=[P.b("IDB")], dma=True)
        P.op("sp", lambda e: e.dma_start(out=self.CMASK[:, :], in_=self.dram["cmask"].ap()[:, :]), w=[P.b("CMASK")], dma=True)
        P.op("sp", lambda e: e.dma_start(out=self.COLS[:, :], in_=self.dram["cols"].ap()[:, :]), w=[P.b("COLS")], dma=True)
        P.op("dve", lambda e: e.memset(self.VA[:, :, :], 1.0), w=[P.b("VA", i) for i in range(-1, 8)])
        P.op("dve", lambda e: e.memset(self.ONES[:, :], 1.0), w=[P.b("ONES")])
        P.op("sp", lambda e: e.dma_start(out=self.RCONST[:, :], in_=self.dram["rconst"].ap()[:, :]),
             w=[P.b("RMASK"), P.b("BLK"), P.b("HSEL")], dma=True)
        for eps, t in self.epsc.items():
            P.op("dve", lambda e, t=t, eps=eps: e.memset(t[:, :], eps), w=[P.b("epsc")])

        xv = x_d.ap().rearrange("(n p) d -> p n d", p=128)
        yv = y_d.ap().rearrange("(n p) d -> p n d", p=128)
        stages = cfg["stages"]
        for half in range(2):
            self.half = half
            for tt in range(NTT):
                P.op("sp", lambda e, tt=tt, half=half: e.dma_start(out=self.H[:, tt, :], in_=xv[:, half * NTT + tt, :]),
                     w=[P.b("H", tt)], dma=True)
            self.norm_done = None
            for si, (l, what) in enumerate(stages):
                nxt = stages[si + 1] if si + 1 < len(stages) else None
                self.next_norm_row = None if nxt is None else nxt[0] * 6 + {"a": 0, "m": 2, "b": 4}[nxt[1]]
                if not hasattr(self, "marks"):
                    self.marks = []
                self.marks.append(("h%d L%d %s" % (half, l, what), len(P.q["pe"])))
                if what == "a":
                    self.ffn(l, 0)
                elif what == "b":
                    self.ffn(l, 1)
                elif what == "m" and l % 3 == 0:
                    self.swa(l)
                elif what == "m" and l % 3 == 1:
                    self.gla(l)
                elif what == "m" and l % 3 == 2:
                    self.rwkv(l)
            outs = []
            for tt in range(NTT):
                outs.append(P.op("sp", lambda e, tt=tt, half=half: e.dma_start(out=yv[:, half * NTT + tt, :], in_=self.H[:, tt, :]),
                                 r=[P.b("H", tt)], dma=True))
        fin = P.op("sp", lambda e: e.nop(), r=[])
        for lst in (P.ndma["sp"][-Prog.NDMA:],):
            for o in lst:
                fin.deps.append(o)
        P.emit()
        return nc


ALL_STAGES = [(l, w) for l in range(DEPTH) for w in ("a", "m", "b")]


def host_layout(inputs):
    f = np.float32
    win = inputs["ffn_w_in"]
    L = win.shape[0]
    w = win.reshape(L, 2, 8, 128, 2, NSLAB, 2, 128).transpose(0, 1, 5, 3, 2, 6, 4, 7)
    ffn_win = np.ascontiguousarray(w).reshape(L, 2, NSLAB, 128, 8 * 512).astype(f, copy=False)
    wout = inputs["ffn_w_out"]
    w = wout.reshape(L, 2, NSLAB, 2, 128, D).transpose(0, 1, 2, 4, 3, 5)
    ffn_wout = np.ascontiguousarray(w).reshape(L, 2, NSLAB, 128, 2 * 1024).astype(f, copy=False)
    rows = [inputs["norm_g"].reshape(DEPTH * 6, D)]
    cols = []
    lay = {}

    def blk(wm):
        n = wm.shape[1] // 512
        return np.ascontiguousarray(wm.reshape(8, 128, n, 512).transpose(2, 1, 0, 3)).reshape(n, 128, 4096)

    def pad_row(v):
        r = np.zeros((1, D), f)
        r[0, :v.size] = v.reshape(-1)
        return r

    def col(v):
        return np.ascontiguousarray(v.reshape(-1, 128).T)

    lay["row_swa"] = sum(r.shape[0] for r in rows)
    lay["col_swa"] = sum(c.shape[1] for c in cols)
    wq, wk, wv, wo = [], [], [], []
    for j in range(2):
        wqkv = inputs["swa_w_qkv"][j]
        b = inputs["swa_b_qkv"][j]
        wq.append(blk(wqkv[:, 0:1024]))
        kd = wqkv[:, 1024:1280].reshape(1024, 4, 1, 64).repeat(2, axis=2).reshape(1024, 512)
        wk.append(blk(kd)[0])
        vd = np.concatenate([wqkv[:, 1280:1536], np.zeros((1024, 256), f)], axis=1)
        wv.append(blk(vd)[0])
        wo.append(blk(inputs["swa_w_o"][j]))
        rows += [pad_row(b[1280:1536]), pad_row(inputs["swa_b_o"][j]), pad_row(inputs["swa_sinks"][j])]
        bkd = b[1024:1280].reshape(4, 1, 64).repeat(2, axis=1).reshape(512)
        cols += [col(b[0:1024]), col(bkd)]
    out = {"swa_wq": np.stack(wq), "swa_wk": np.stack(wk), "swa_wv": np.stack(wv), "swa_wo": np.stack(wo)}
    lay["row_gla"] = sum(r.shape[0] for r in rows)
    lay["col_gla"] = sum(c.shape[1] for c in cols)
    gw = np.concatenate([inputs["gla_w_in"][0], np.zeros((1024, 7 * 512 - 3088), f)], axis=1)
    out["gla_win"] = blk(gw)
    out["gla_wo"] = blk(inputs["gla_w_o"][0])
    out["gla_wg2"] = np.ascontiguousarray(inputs["gla_w_gate2"][0]).astype(f, copy=False)
    rows += [np.tile(inputs["gla_norm_g"][0], 4)[None, :]]
    cols += [col(inputs["gla_b_gate"][0])]
    lay["row_rwkv"] = sum(r.shape[0] for r in rows)
    lay["col_rwkv"] = sum(c.shape[1] for c in cols)
    l1 = np.concatenate([inputs["rwkv_w1"][0], inputs["rwkv_a1"][0], inputs["rwkv_g1"][0],
                         np.zeros((1024, 512 - 288), f)], axis=1)
    out["rwkv_wl1"] = blk(l1)
    out["rwkv_wr"] = blk(inputs["rwkv_w_rkv"][0, 0])
    out["rwkv_wk"] = blk(inputs["rwkv_w_rkv"][0, 1])
    out["rwkv_wv"] = blk(inputs["rwkv_w_rkv"][0, 2])
    out["rwkv_wo"] = blk(inputs["rwkv_w_o"][0])
    out["rwkv_w2"] = np.ascontiguousarray(inputs["rwkv_w2"][0])
    out["rwkv_a2"] = np.ascontiguousarray(inputs["rwkv_a2"][0])
    out["rwkv_g2"] = np.ascontiguousarray(inputs["rwkv_g2"][0])
    rows += [inputs["rwkv_lnx_g"][0][None, :], inputs["rwkv_lnx_b"][0][None, :]]
    cols += [col(inputs["rwkv_mu"][0].reshape(-1)), col(inputs["rwkv_w0"][0]), col(inputs["rwkv_a0"][0]),
             col(inputs["rwkv_k_k"][0]), col(inputs["rwkv_k_a"][0]), col(inputs["rwkv_r_k"][0].reshape(-1))]
    pp = np.arange(128)[:, None]
    ff = np.arange(128)[None, :]
    p6 = pp % 64
    f6 = np.arange(64)[None, :]
    same = (pp // 64 == ff // 64)
    rc = np.concatenate([same & (pp < ff), same & (pp <= ff), same & (pp > ff), same,
                         (pp // 64 == np.arange(2)[None, :]), (p6 == f6)], axis=1)
    out["rconst"] = rc.astype(np.float32).astype(ml_dtypes.bfloat16)
    rows = np.ascontiguousarray(np.concatenate(rows, axis=0)).astype(f, copy=False)
    cols = np.ascontiguousarray(np.concatenate(cols, axis=1)).astype(f, copy=False)
    idb = np.eye(128, dtype=np.float32).astype(ml_dtypes.bfloat16)
    jj = np.arange(128)[:, None]
    ii = np.arange(128)[None, :]
    cm = np.concatenate([np.tile((jj <= ii), (1, 4)), np.tile((jj > ii), (1, 4))], axis=1)
    cmask = cm.astype(np.float32).astype(ml_dtypes.bfloat16)
    out.update({"rows": rows, "cols": cols, "ffn_win": ffn_win, "ffn_wout": ffn_wout, "idb": idb, "cmask": cmask})
    for k in list(out):
        if out[k].dtype == np.float64:
            out[k] = out[k].astype(f)
    return out, lay


_CACHE = {}


def run(inputs, stages, ncores=8, trace=False):
    shared, lay = host_layout(inputs)
    cfg = {"stages": stages, "nrows": shared["rows"].shape[0], "ncols": shared["cols"].shape[1]}
    cfg.update(lay)
    kk = K(cfg)
    with kk.stack:
        nc = kk.build()
    x = np.ascontiguousarray(inputs["x"]).astype(np.float32, copy=False)
    in_maps = []
    for c in range(ncores):
        m = {"x": x[c]}
        for k in kk.dram:
            if k != "x":
                m[k] = shared[k]
        in_maps.append(m)
    res = run_bass_kernel_spmd(nc, in_maps, core_ids=list(range(ncores)), trace=trace)
    out = np.stack([np.asarray(r["y"]) for r in res.results], axis=0)
    return out.astype(np.float32, copy=False), res


def kernel(**inputs):
    out, _ = run(inputs, ALL_STAGES)
    return out
```

```python
import contextlib
import numpy as np
import ml_dtypes
import concourse.bass as bass
import concourse.mybir as mybir
from concourse.bass_utils import run_bass_kernel_spmd

F32 = mybir.dt.float32
BF16 = mybir.dt.bfloat16
AF = mybir.ActivationFunctionType
ALU = mybir.AluOpType

D = 1024
SEQ = 2048
DEPTH = 4
DFF = 2816
NFC = DFF // 128
NSLAB = NFC // 2
TB = 1024
NTT = TB // 128
EPS = 1e-6


class Buf:
    __slots__ = ("w", "rs")

    def __init__(self):
        self.w = None
        self.rs = []


class Inst:
    __slots__ = ("eng", "fn", "deps", "sig", "dma", "sem", "val", "prev_dma")

    def __init__(self, eng, fn, dma):
        self.eng = eng
        self.fn = fn
        self.deps = []
        self.sig = False
        self.dma = dma
        self.sem = None
        self.val = None
        self.prev_dma = None


class _Rec:
    def __init__(self):
        self.call = None

    def __getattr__(self, name):
        def f(*a, **k):
            self.call = (name, a, k)
            return self
        return f


class Prog:
    ENGS = ("pe", "act", "dve", "pool", "sp")
    NDMA = 8

    def __init__(self, nc, stack):
        self.nc = nc
        self.q = {e: [] for e in self.ENGS}
        self.esem = {e: stack.enter_context(nc.semaphore("s_" + e)) for e in self.ENGS}
        self.dsem = {e: [stack.enter_context(nc.semaphore("d_%s%d" % (e, i))) for i in range(self.NDMA)]
                     for e in ("act", "pool", "sp")}
        self.ndma = {e: [] for e in ("act", "pool", "sp")}
        self.bufs = {}

    def b(self, *key):
        if key == ("PO", 2):
            key = ("PT2",)
        bb = self.bufs.get(key)
        if bb is None:
            bb = self.bufs[key] = Buf()
        return bb

    def op(self, eng, fn, r=(), w=(), dma=False):
        rec = _Rec()
        fn(rec)
        inst = Inst(eng, rec.call, dma)

        def dep(o, war=False):
            if o is None or o is inst:
                return
            if not dma and not o.dma and o.eng == eng:
                if eng == "pe":
                    return
            if o not in inst.deps:
                inst.deps.append(o)
                o.sig = True

        for bb in r:
            dep(bb.w)
        for bb in w:
            dep(bb.w)
            for o in bb.rs:
                dep(o, war=True)
        for bb in r:
            bb.rs.append(inst)
        for bb in w:
            bb.w = inst
            bb.rs = []
        if dma:
            lst = self.ndma[eng]
            if len(lst) >= self.NDMA:
                inst.prev_dma = lst[len(lst) - self.NDMA]
            inst.sem = self.dsem[eng][len(lst) % self.NDMA]
            inst.val = 16 * (len(lst) // self.NDMA + 1)
            lst.append(inst)
        self.q[eng].append(inst)
        return inst

    def emit(self):
        nc = self.nc
        for e in self.ENGS:
            c = 0
            for inst in self.q[e]:
                if not inst.dma:
                    inst.sem = self.esem[e]
                    if inst.sig:
                        c += 1
                    inst.val = c if inst.sig else None
        engobj = {"pe": "tensor", "act": "scalar", "dve": "vector", "pool": "gpsimd", "sp": "sync"}

        def run(e, eng):
            known = {}
            for inst in self.q[e]:
                waits = {}
                ds = list(inst.deps)
                if inst.prev_dma is not None:
                    ds.append(inst.prev_dma)
                for o in ds:
                    k = id(o.sem)
                    if o.val > known.get(k, 0) and o.val > waits.get(k, (None, 0))[1]:
                        waits[k] = (o.sem, o.val)
                for k, (s, v) in waits.items():
                    eng.wait_ge(s, v)
                    known[k] = v
                name, a, k = inst.fn
                h = getattr(eng, name)(*a, **k)
                if inst.dma:
                    h.then_inc(inst.sem, 16)
                elif inst.sig:
                    h.then_inc(inst.sem, 1)

        with nc.Block() as block:
            for e in self.ENGS:
                if not self.q[e]:
                    continue
                getattr(block, engobj[e])(lambda eng, e=e: run(e, eng))


class K:
    def __init__(self, cfg):
        self.cfg = cfg
        nc = self.nc = bass.Bass("TRN2", target_bir_lowering=False)
        self.stack = contextlib.ExitStack()
        self.P = Prog(nc, self.stack)
        self.dram = {}
        self.gbi = 0
        self.pi = {}

    def din(self, name, shape, dt=F32):
        t = self.nc.dram_tensor(name, list(shape), dt, kind="ExternalInput")
        self.dram[name] = t
        return t

    def sb(self, name, shape, dt):
        return self.stack.enter_context(self.nc.sbuf_tensor(name, list(shape), dt))

    def ps(self, name, shape, dt):
        return self.stack.enter_context(self.nc.psum_tensor(name, list(shape), dt))

    def rot(self, key, n):
        i = self.pi.get(key, 0)
        self.pi[key] = i + 1
        return i % n

    def load_row_bc(self, row):
        P = self.P
        i = self.rot("gb", 2)
        src = self.dram["rows"].ap()[row:row + 1, :].partition_broadcast(128)
        P.op("sp", lambda e: e.dma_start(out=self.GB[i][:, :], in_=src), w=[P.b("GB", i)], dma=True)
        return i

    def rstd_batch(self, ss, n, scale, eps, half=False, key="rs"):
        P = self.P
        bs = P.b(key)
        P.op("act", lambda e: e.activation(out=ss[:, :n], in_=ss[:, :n], func=AF.Sqrt,
                                           bias=self.epsc[eps][:, 0:1], scale=scale), r=[bs, P.b("epsc")], w=[bs])
        P.op("dve", lambda e: e.reciprocal(out=ss[:, :n], in_=ss[:, :n]), r=[bs], w=[bs])
        if half:
            P.op("dve", lambda e: e.tensor_scalar(out=ss[:, :n], in0=ss[:, :n], scalar1=0.5, scalar2=None,
                                                  op0=ALU.mult), r=[bs], w=[bs])

    def _rstd_tile(self, ss, col, scale, eps, key):
        P = self.P
        P.op("act", lambda e: e.activation(out=ss[:, col:col + 1], in_=ss[:, col:col + 1], func=AF.Sqrt,
                                           bias=self.epsc[eps][:, 0:1], scale=scale), r=[key, P.b("epsc")], w=[key])
        P.op("dve", lambda e: e.reciprocal(out=ss[:, col:col + 1], in_=ss[:, col:col + 1]), r=[key], w=[key])

    def _norm_stages(self, gi, tiles=range(NTT)):
        P = self.P
        ss = self.SS
        for tt in tiles:
            key = P.b("rsn", tt)
            P.op("act", lambda e: e.activation(out=self.JUNK[:, :], in_=self.H[:, tt, :], func=AF.Square,
                                               accum_out=ss[:, tt:tt + 1]), r=[P.b("H", tt)], w=[key])
            self._rstd_tile(ss, tt, 1.0 / D, EPS, key)
        for tt in tiles:
            key = P.b("rsn", tt)
            xi = self.rot("xs", 2)
            P.op("dve", lambda e: e.scalar_tensor_tensor(
                out=self.XS[xi][:, :], in0=self.H[:, tt, :], scalar=ss[:, tt:tt + 1], in1=self.GB[gi][:, :],
                op0=ALU.mult, op1=ALU.mult), r=[P.b("H", tt), key, P.b("GB", gi)], w=[P.b("XS", xi)])
            self.transpose_to_XT(self.XS[xi], P.b("XS", xi), tt)

    def norm_T(self, grow):
        if self.norm_done == grow:
            self.norm_done = None
            return
        gi = self.load_row_bc(grow)
        self._norm_stages(gi)

    def post_norm(self, grow, half, src_key="ACC", bias_row=None):
        P = self.P
        gi = self.load_row_bc(grow)
        nrow = self.next_norm_row
        gn = self.load_row_bc(nrow) if nrow is not None else None
        ss = self.SS
        sc, ep = ((4.0 / D, 4 * EPS) if half else (1.0 / D, EPS))
        for tt in range(NTT):
            key = P.b("rsp", tt)
            P.op("act", lambda e: e.activation(out=self.JUNK[:, :], in_=self.ACC[:, tt, :], func=AF.Square,
                                               accum_out=ss[:, 8 + tt:9 + tt]),
                 r=[P.b("ACC", tt, 0), P.b("ACC", tt, 1)], w=[key])
            self._rstd_tile(ss, 8 + tt, sc, ep, key)
        for tt in range(NTT):
            key = P.b("rsp", tt)
            P.op("dve", lambda e: e.scalar_tensor_tensor(
                out=self.ACC[:, tt, :], in0=self.ACC[:, tt, :], scalar=ss[:, 8 + tt:9 + tt], in1=self.GB[gi][:, :],
                op0=ALU.mult, op1=ALU.mult),
                r=[P.b("ACC", tt, 0), P.b("ACC", tt, 1), key, P.b("GB", gi)],
                w=[P.b("ACC", tt, 0), P.b("ACC", tt, 1)])
            P.op("dve" if tt % 2 == 0 else "pool", lambda e: e.tensor_tensor(
                out=self.H[:, tt, :], in0=self.H[:, tt, :], in1=self.ACC[:, tt, :], op=ALU.add),
                r=[P.b("H", tt), P.b("ACC", tt, 0), P.b("ACC", tt, 1)], w=[P.b("H", tt)])
        if gn is not None:
            self._norm_stages(gn)
            self.norm_done = nrow

    def ffn(self, l, a):
        P = self.P
        self.norm_T(l * 6 + (0 if a == 0 else 4))
        win_d = self.dram["ffn_win"].ap()
        wout_d = self.dram["ffn_wout"].ap()

        def load(s):
            wb = self.rot("wslab", 2)
            P.op("pool", lambda e: e.dma_start(out=self.WIN[wb][:, :], in_=win_d[l, a, s, :, :]),
                 w=[P.b("WIN", wb)], dma=True)
            P.op("pool", lambda e: e.dma_start(out=self.WOUT[wb][:, :], in_=wout_d[l, a, s, :, :]),
                 w=[P.b("WOUT", wb)], dma=True)
            return wb

        def phase1(s, wb):
            for tb in range(2):
                for fcl in range(2):
                    gi = self.rot("pgu", 2)
                    for gu, ps in ((0, self.PG[gi]), (1, self.PU[gi])):
                        for kc in range(8):
                            c0 = kc * 512 + fcl * 256 + gu * 128
                            P.op("pe", lambda e, ps=ps, c0=c0, kc=kc, tb=tb: e.matmul(
                                ps[:, :], lhsT=self.WIN[wb][:, c0:c0 + 128],
                                rhs=self.XT[:, kc, tb * 512:(tb + 1) * 512], start=(kc == 0), stop=(kc == 7)),
                                r=[P.b("WIN", wb)] + [P.b("XT", tb * 4 + i) for i in range(4)],
                                w=[P.b("PG" if gu == 0 else "PU", gi)])
                    P.op("act", lambda e, gi=gi: e.activation(out=self.SG[gi][:, :], in_=self.PG[gi][:, :],
                                                              func=AF.Silu),
                         r=[P.b("PG", gi)], w=[P.b("SG", gi)])
                    P.op("dve", lambda e, gi=gi, fcl=fcl, tb=tb: e.tensor_tensor(
                        out=self.ATS[wb][:, fcl, tb * 512:(tb + 1) * 512], in0=self.SG[gi][:, :],
                        in1=self.PU[gi][:, :], op=ALU.mult),
                        r=[P.b("SG", gi), P.b("PU", gi)], w=[P.b("ATS", wb, fcl, tb)])

        def phase2(s, wb):
            for tt in range(NTT):
                for nh in range(2):
                    oi = self.rot("po", 2)
                    for fcl in range(2):
                        P.op("pe", lambda e, oi=oi, fcl=fcl, tt=tt, nh=nh: e.matmul(
                            self.PO[oi][:, :], lhsT=self.ATS[wb][:, fcl, tt * 128:(tt + 1) * 128],
                            rhs=self.WOUT[wb][:, fcl * 1024 + nh * 512: fcl * 1024 + (nh + 1) * 512],
                            start=(fcl == 0), stop=(fcl == 1)),
                            r=[P.b("ATS", wb, fcl, tt // 4), P.b("WOUT", wb)], w=[P.b("PO", oi)])
                    if s == 0:
                        P.op("act", lambda e, oi=oi, tt=tt, nh=nh: e.activation(
                            out=self.ACC[:, tt, nh * 512:(nh + 1) * 512], in_=self.PO[oi][:, :], func=AF.Copy),
                            r=[P.b("PO", oi)], w=[P.b("ACC", tt, nh)])
                    else:
                        P.op("dve", lambda e, oi=oi, tt=tt, nh=nh: e.tensor_tensor(
                            out=self.ACC[:, tt, nh * 512:(nh + 1) * 512], in0=self.ACC[:, tt, nh * 512:(nh + 1) * 512],
                            in1=self.PO[oi][:, :], op=ALU.add),
                            r=[P.b("PO", oi), P.b("ACC", tt, nh)], w=[P.b("ACC", tt, nh)])

        wbs = {}
        wbs[0] = load(0)
        phase1(0, wbs[0])
        for s in range(NSLAB):
            if s + 1 < NSLAB:
                wbs[s + 1] = load(s + 1)
                phase1(s + 1, wbs[s + 1])
            phase2(s, wbs[s])
        self.post_norm(l * 6 + (1 if a == 0 else 5), half=True)


    def load_w(self, name, *idx):
        P = self.P
        wb = self.rot("wslab", 2)
        src = self.dram[name].ap()
        for i in idx:
            src = src[i]
        P.op("pool", lambda e: e.dma_start(out=self.WIN[wb][:, :], in_=src), w=[P.b("WIN", wb)], dma=True)
        return wb

    def proj_fm(self, wb, c0, M, tb, ps, pkey):
        P = self.P
        for kc in range(8):
            P.op("pe", lambda e, kc=kc: e.matmul(
                ps[:M, :], lhsT=self.WIN[wb][:, kc * 512 + c0: kc * 512 + c0 + M],
                rhs=self.XT[:, kc, tb * 512:(tb + 1) * 512], start=(kc == 0), stop=(kc == 7)),
                r=[P.b("WIN", wb)] + [P.b("XT", tb * 4 + i) for i in range(4)], w=[pkey])

    def proj_tm(self, wb, c0, N, tt, ps, pkey, xt=None, xkey="XT"):
        P = self.P
        xt = self.XT if xt is None else xt
        for kc in range(8):
            P.op("pe", lambda e, kc=kc: e.matmul(
                ps[:, :N], lhsT=xt[:, kc, tt * 128:(tt + 1) * 128],
                rhs=self.WIN[wb][:, kc * 512 + c0: kc * 512 + c0 + N], start=(kc == 0), stop=(kc == 7)),
                r=[P.b("WIN", wb), P.b(xkey, tt)], w=[pkey])

    def transpose_to_XT(self, src, skey, tt):
        P = self.P
        for kc in range(8):
            P.op("pe", lambda e, kc=kc: e.transpose(
                out=self.PT[:, kc * 128:(kc + 1) * 128], in_=src[:, kc * 128:(kc + 1) * 128],
                identity=self.IDB[:, :]), r=[skey, P.b("IDB")], w=[P.b("PT")])
        P.op("act", lambda e: e.activation(
            out=self.XT[:, :, tt * 128:(tt + 1) * 128],
            in_=self.PT[:, :].rearrange("p (k c) -> p k c", k=8), func=AF.Copy),
            r=[P.b("PT")], w=[P.b("XT", tt)])

    def out_proj(self, wname, widx, brow):
        P = self.P
        wbs = [self.load_w(wname, *widx, nh) for nh in range(2)]
        gi = self.load_row_bc(brow) if brow is not None else None
        for tt in range(NTT):
            for nh in range(2):
                oi = self.rot("po", 2)
                self.proj_tm(wbs[nh], 0, 512, tt, self.PO[oi], P.b("PO", oi))
                if gi is None:
                    P.op("act", lambda e, oi=oi, tt=tt, nh=nh: e.activation(
                        out=self.ACC[:, tt, nh * 512:(nh + 1) * 512], in_=self.PO[oi][:, :], func=AF.Copy),
                        r=[P.b("PO", oi)], w=[P.b("ACC", tt, nh)])
                else:
                    P.op("dve", lambda e, oi=oi, tt=tt, nh=nh: e.tensor_tensor(
                        out=self.ACC[:, tt, nh * 512:(nh + 1) * 512], in0=self.PO[oi][:, :],
                        in1=self.GB[gi][:, nh * 512:(nh + 1) * 512], op=ALU.add),
                        r=[P.b("PO", oi), P.b("GB", gi)], w=[P.b("ACC", tt, nh)])

    def swa(self, l):
        P = self.P
        j = l // 3
        half = self.half
        R0 = self.cfg["row_swa"] + j * 3
        C0 = self.cfg["col_swa"] + j * 12
        self.norm_T(l * 6 + 2)
        P.op("dve", lambda e: e.memset(self.VA[:, :, :], 1.0), w=[P.b("VA", i) for i in range(-1, 8)])
        gs = self.load_row_bc(R0 + 2)
        P.op("act", lambda e: e.activation(out=self.ESK[:, :], in_=self.GB[gs][:, 0:16], func=AF.Exp),
             r=[P.b("GB", gs)], w=[P.b("ESK")])
        P.op("dve", lambda e: e.tensor_scalar(out=self.BQ8[:, :], in0=self.COLS[:, C0:C0 + 8], scalar1=0.125,
                                              scalar2=None, op0=ALU.mult), r=[P.b("COLS")], w=[P.b("BQ8")])
        if half == 1:
            P.op("pool", lambda e: e.tensor_copy(out=self.KT[:, :, 0:128], in_=self.KC[j][:, :, :]),
                 r=[P.b("KC", j)], w=[P.b("KT", -1)])
            P.op("pool", lambda e: e.tensor_copy(out=self.VA[:, 0, :], in_=self.VC[j][:, :]),
                 r=[P.b("VC", j)], w=[P.b("VA", -1)])
        for blk in range(2):
            wb = self.load_w("swa_wq", j, blk)
            for ocl in range(4):
                oc = blk * 4 + ocl
                for tb in range(2):
                    gi = self.rot("pgu", 2)
                    self.proj_fm(wb, ocl * 128, 128, tb, self.PG[gi], P.b("PG", gi))
                    P.op("act", lambda e, gi=gi, oc=oc, tb=tb: e.activation(
                        out=self.QT[:, oc, tb * 512:(tb + 1) * 512], in_=self.PG[gi][:, :], func=AF.Identity,
                        bias=self.BQ8[:, oc:oc + 1], scale=0.125),
                        r=[P.b("PG", gi), P.b("BQ8")], w=[P.b("QT", oc, tb)])
        wb = self.load_w("swa_wk", j)
        for hk in range(4):
            for tb in range(2):
                gi = self.rot("pgu", 2)
                self.proj_fm(wb, hk * 128, 128, tb, self.PU[gi], P.b("PU", gi))
                P.op("act", lambda e, gi=gi, hk=hk, tb=tb: e.activation(
                    out=self.KT[:, hk, 128 + tb * 512: 128 + (tb + 1) * 512], in_=self.PU[gi][:, :], func=AF.Identity,
                    bias=self.COLS[:, C0 + 8 + hk:C0 + 9 + hk], scale=1.0),
                    r=[P.b("PU", gi), P.b("COLS")], w=[P.b("KT", tb * 4 + i) for i in range(4)])
        wb = self.load_w("swa_wv", j)
        gv = self.load_row_bc(R0 + 0)
        for tt in range(NTT):
            oi = self.rot("po", 2)
            self.proj_tm(wb, 0, 256, tt, self.PO[oi], P.b("PO", oi))
            P.op("dve", lambda e, oi=oi, tt=tt: e.tensor_tensor(
                out=self.VA[:, tt + 1, :].rearrange("p (h d) -> p h d", h=4)[:, :, 0:64],
                in0=self.PO[oi][:, 0:256].rearrange("p (h d) -> p h d", h=4),
                in1=self.GB[gv][:, 0:256].rearrange("p (h d) -> p h d", h=4), op=ALU.add),
                r=[P.b("PO", oi), P.b("GB", gv)], w=[P.b("VA", tt)])
        for n in range(NTT):
            has_prev = not (half == 0 and n == 0)
            xi = self.rot("xs", 2)
            for hk in range(4):
                srcs = [("cur", self.PG, "PG", 128 + n * 128, n)]
                if has_prev:
                    srcs.append(("prev", self.PU, "PU", n * 128, n - 1))
                pts = []
                for (kind, pss, pname, k0, kblk) in srcs:
                    si = self.rot("sg", 2)
                    for hp in range(2):
                        P.op("pe", lambda e: e.matmul(
                            pss[hp][:, 0:256], lhsT=self.KT[hp * 64:(hp + 1) * 64, hk, k0:k0 + 128],
                            rhs=self.QT[hp * 64:(hp + 1) * 64, 2 * hk:2 * hk + 2, n * 128:(n + 1) * 128],
                            start=True, stop=True),
                            r=[P.b("KT", kblk), P.b("QT", 2 * hk, n // 4), P.b("QT", 2 * hk + 1, n // 4)],
                            w=[P.b(pname, hp)])
                        P.op("act", lambda e: e.activation(out=self.SG[si][:, hp * 256:(hp + 1) * 256],
                                                           in_=pss[hp][:, 0:256], func=AF.Exp),
                             r=[P.b(pname, hp)], w=[P.b("SG", si)])
                    pi = self.rot("pts", 4)
                    mo = 0 if kind == "cur" else 512
                    P.op("dve", lambda e, si=si, pi=pi, mo=mo: e.tensor_tensor(
                        out=self.PTS[pi][:, :], in0=self.SG[si][:, :], in1=self.CMASK[:, mo:mo + 512], op=ALU.mult),
                        r=[P.b("SG", si), P.b("CMASK")], w=[P.b("PTS", pi)])
                    pts.append((pi, kblk))
                oi = self.rot("po", 2)
                for hh in range(4):
                    pos = (hh % 2) * 2 + hh // 2
                    for ii, (pi, kblk) in enumerate(pts):
                        P.op("pe", lambda e, oi=oi, hh=hh, pos=pos, pi=pi, kblk=kblk, ii=ii: e.matmul(
                            self.PO[oi][:, hh * 65:(hh + 1) * 65], lhsT=self.PTS[pi][:, pos * 128:(pos + 1) * 128],
                            rhs=self.VA[:, kblk + 1, hk * 65:(hk + 1) * 65], start=(ii == 0), stop=(ii == len(pts) - 1)),
                            r=[P.b("PTS", pi), P.b("VA", kblk)], w=[P.b("PO", oi)])
                ov = self.PO[oi][:, 0:260].rearrange("p (h d) -> p h d", h=4)
                P.op("dve", lambda e, ov=ov: e.tensor_tensor(
                    out=self.DEN[:, :], in0=ov[:, :, 64], in1=self.ESK[:, hk * 4:(hk + 1) * 4], op=ALU.add),
                    r=[P.b("PO", oi), P.b("ESK")], w=[P.b("DEN")])
                P.op("dve", lambda e: e.reciprocal(out=self.DEN[:, :], in_=self.DEN[:, :]),
                     r=[P.b("DEN")], w=[P.b("DEN")])
                P.op("dve", lambda e, ov=ov, xi=xi: e.tensor_tensor(
                    out=self.XS[xi][:, hk * 256:(hk + 1) * 256].rearrange("p (h d) -> p h d", h=4),
                    in0=ov[:, :, 0:64], in1=self.DEN[:, :].unsqueeze(2).to_broadcast([128, 4, 64]), op=ALU.mult),
                    r=[P.b("PO", oi), P.b("DEN")], w=[P.b("XS", xi)])
            self.transpose_to_XT(self.XS[xi], P.b("XS", xi), n)
        if half == 0:
            P.op("pool", lambda e: e.tensor_copy(out=self.KC[j][:, :, :], in_=self.KT[:, :, 1024:1152]),
                 r=[P.b("KT", 7)], w=[P.b("KC", j)])
            P.op("pool", lambda e: e.tensor_copy(out=self.VC[j][:, :], in_=self.VA[:, 8, :]),
                 r=[P.b("VA", 7)], w=[P.b("VC", j)])
        self.out_proj("swa_wo", (j,), R0 + 1)
        self.post_norm(l * 6 + 3, half=False)


    def gla(self, l):
        P = self.P
        half = self.half
        R0 = self.cfg["row_gla"]
        C0 = self.cfg["col_gla"]
        DK = 128
        LA = self.ATS[0][:, :, :].rearrange("p a b -> p (a b)").bitcast(F32)
        EB = self.ATS[1][:, :, :].rearrange("p a b -> p (a b)").bitcast(F32)
        ENB = self.WOUT[0][:, :].bitcast(F32)
        kLA, kEB, kENB = [P.b("ATS", 0, a, b) for a in range(2) for b in range(2)], \
            [P.b("ATS", 1, a, b) for a in range(2) for b in range(2)], [P.b("WOUT", 0)]
        ACCb = self.ACC[:, :, :].rearrange("p a b -> p (a b)").bitcast(BF16).rearrange("p (a b) -> p a b", a=NTT)
        self.norm_T(l * 6 + 2)
        if half == 0:
            P.op("dve", lambda e: e.memset(self.GS[:, :, :], 0.0), w=[P.b("GS", h) for h in range(4)])
            P.op("dve", lambda e: e.memset(self.GSB[:, :, :], 0.0), w=[P.b("GSB", h) for h in range(4)])
        P.op("dve", lambda e: e.tensor_scalar(out=self.NBG[:, :], in0=self.COLS[:, C0:C0 + 4], scalar1=-1.0,
                                              scalar2=None, op0=ALU.mult), r=[P.b("COLS")], w=[P.b("NBG")])
        P.op("pool", lambda e: e.dma_start(out=self.SG[0][0:16, :], in_=self.dram["gla_wg2"].ap()[:, :]),
             w=[P.b("SG", 0)], dma=True)
        wb = self.load_w("gla_win", 6)
        for tb in range(2):
            gi = self.rot("pgu", 2)
            self.proj_fm(wb, 0, 16, tb, self.PG[gi], P.b("PG", gi))
            P.op("act", lambda e: e.activation(out=self.XS[0][0:16, tb * 512:(tb + 1) * 512], in_=self.PG[gi][0:16, :],
                                               func=AF.Copy), r=[P.b("PG", gi)], w=[P.b("XS", 0)])
        wq = self.load_w("gla_win", 0)
        wk = self.load_w("gla_win", 1)
        for h in range(4):
            for tb in range(2):
                gi = self.rot("pgu", 2)
                P.op("pe", lambda e: e.matmul(self.PU[gi][:, :], lhsT=self.SG[0][0:16, h * 128:(h + 1) * 128],
                                              rhs=self.XS[0][0:16, tb * 512:(tb + 1) * 512], start=True, stop=True),
                     r=[P.b("SG", 0), P.b("XS", 0)], w=[P.b("PU", gi)])
                P.op("act", lambda e: e.activation(out=EB[:, tb * 512:(tb + 1) * 512], in_=self.PU[gi][:, :], func=AF.Exp,
                                                   bias=self.NBG[:, h:h + 1], scale=-1.0),
                     r=[P.b("PU", gi), P.b("NBG")], w=kEB)
                P.op("act", lambda e: e.activation(out=LA[:, tb * 512:(tb + 1) * 512], in_=EB[:, tb * 512:(tb + 1) * 512],
                                                   func=AF.Ln, bias=self.ONE1[:, 0:1], scale=1.0), r=kEB + [P.b("ONES")], w=kLA)
            for c in range(8):
                P.op("dve", lambda e: e.tensor_tensor_scan(
                    out=LA[:, c * 128:(c + 1) * 128], data0=self.ONES[:, :], data1=LA[:, c * 128:(c + 1) * 128],
                    initial=0.0, op0=ALU.mult, op1=ALU.add), r=kLA + [P.b("ONES")], w=kLA)
            P.op("act", lambda e: e.activation(out=EB[:, :], in_=LA[:, :], func=AF.Exp, scale=-1.0 / 16.0), r=kLA, w=kEB)
            P.op("act", lambda e: e.activation(out=ENB[:, :], in_=LA[:, :], func=AF.Exp, scale=1.0 / 16.0), r=kLA, w=kENB)
            P.op("dve", lambda e: e.tensor_copy(out=self.EBLS[:, h, :],
                                                in_=EB.rearrange("p (c t) -> p c t", c=8)[:, :, 127]),
                 r=kEB, w=[P.b("EBLS", h)])
            for tb in range(2):
                gi = self.rot("pgu", 2)
                self.proj_fm(wq, h * 128, 128, tb, self.PG[gi], P.b("PG", gi))
                P.op("dve", lambda e: e.scalar_tensor_tensor(
                    out=self.QT[:, h, tb * 512:(tb + 1) * 512], in0=self.PG[gi][:, :], scalar=DK ** -0.5,
                    in1=EB[:, tb * 512:(tb + 1) * 512], op0=ALU.mult, op1=ALU.mult),
                    r=[P.b("PG", gi)] + kEB, w=[P.b("QT", h, tb)])
                self.proj_fm(wk, h * 128, 128, tb, self.PU[gi], P.b("PU", gi))
                P.op("dve", lambda e: e.tensor_tensor(
                    out=self.QT[:, 4 + h, tb * 512:(tb + 1) * 512], in0=self.PU[gi][:, :],
                    in1=ENB[:, tb * 512:(tb + 1) * 512], op=ALU.mult),
                    r=[P.b("PU", gi)] + kENB, w=[P.b("QT", 4 + h, tb)])
            P.op("dve", lambda e: e.tensor_tensor(
                out=self.KT[:, h, 0:1024].rearrange("p (c t) -> p c t", c=8),
                in0=self.QT[:, 4 + h, :].rearrange("p (c t) -> p c t", c=8),
                in1=self.EBLS[:, h, :].unsqueeze(2).to_broadcast([128, 8, 128]), op=ALU.mult),
                r=[P.b("QT", 4 + h, 0), P.b("QT", 4 + h, 1), P.b("EBLS", h)], w=[P.b("KT", i) for i in range(-1, 8)])
        for blk in range(2):
            wb = self.load_w("gla_win", 2 + blk)
            for tt in range(NTT):
                oi = self.rot("po", 2)
                self.proj_tm(wb, 0, 512, tt, self.PO[oi], P.b("PO", oi))
                P.op("act", lambda e: e.activation(out=ACCb[:, tt, blk * 512:(blk + 1) * 512], in_=self.PO[oi][:, :],
                                                   func=AF.Copy), r=[P.b("PO", oi)], w=[P.b("ACC", tt, 0)])
        gn = self.load_row_bc(R0 + 0)
        for blk in range(2):
            wb = self.load_w("gla_win", 4 + blk)
            for tt in range(NTT):
                oi = self.rot("po", 2)
                self.proj_tm(wb, 0, 512, tt, self.PO[oi], P.b("PO", oi))
                si = self.rot("sg", 2)
                P.op("act", lambda e: e.activation(out=self.SG[si][:, :], in_=self.PO[oi][:, :], func=AF.Silu),
                     r=[P.b("PO", oi)], w=[P.b("SG", si)])
                P.op("dve", lambda e: e.tensor_tensor(
                    out=ACCb[:, tt, 1024 + blk * 512:1024 + (blk + 1) * 512], in0=self.SG[si][:, :],
                    in1=self.GB[gn][:, blk * 512:(blk + 1) * 512], op=ALU.mult),
                    r=[P.b("SG", si), P.b("GB", gn)], w=[P.b("ACC", tt, 1)])
        for c in range(NTT):
            cs = slice(c * 128, (c + 1) * 128)
            for h in range(4):
                gi = self.rot("pgu", 2)
                P.op("pe", lambda e: e.matmul(self.PG[gi][:, 0:128], lhsT=self.QT[:, 4 + h, cs], rhs=self.QT[:, h, cs],
                                              start=True, stop=True),
                     r=[P.b("QT", 4 + h, c // 4), P.b("QT", h, c // 4)], w=[P.b("PG", gi)])
                pa = self.rot("pts", 4)
                P.op("dve", lambda e: e.tensor_tensor(out=self.PTS[pa][:, 0:128], in0=self.PG[gi][:, 0:128],
                                                      in1=self.CMASK[:, 0:128], op=ALU.mult),
                     r=[P.b("PG", gi), P.b("CMASK")], w=[P.b("PTS", pa)])
                oi = self.rot("po", 2)
                P.op("pe", lambda e: e.matmul(self.PO[oi][:, 0:256], lhsT=self.PTS[pa][:, 0:128],
                                              rhs=ACCb[:, c, h * 256:(h + 1) * 256], start=True, stop=False),
                     r=[P.b("PTS", pa), P.b("ACC", c, 0)], w=[P.b("PO", oi)])
                P.op("pe", lambda e: e.matmul(self.PO[oi][:, 0:256], lhsT=self.QT[:, h, cs], rhs=self.GSB[:, h, :],
                                              start=False, stop=True),
                     r=[P.b("QT", h, c // 4), P.b("GSB", h)], w=[P.b("PO", oi)])
                P.op("pe", lambda e: e.transpose(out=self.PT[:, 0:128], in_=self.KT[:, h, cs], identity=self.IDB[:, :]),
                     r=[P.b("KT", c), P.b("IDB")], w=[P.b("PT")])
                pk = self.rot("pts", 4)
                P.op("act", lambda e: e.activation(out=self.PTS[pk][:, 0:128], in_=self.PT[:, 0:128], func=AF.Copy),
                     r=[P.b("PT")], w=[P.b("PTS", pk)])
                P.op("pe", lambda e: e.matmul(self.PU[gi][:, 0:256], lhsT=self.PTS[pk][:, 0:128],
                                              rhs=ACCb[:, c, h * 256:(h + 1) * 256], start=True, stop=True),
                     r=[P.b("PTS", pk), P.b("ACC", c, 0)], w=[P.b("PU", gi)])
                P.op("dve", lambda e: e.scalar_tensor_tensor(
                    out=self.GS[:, h, :], in0=self.GS[:, h, :], scalar=self.EBLS[:, h, c:c + 1], in1=self.PU[gi][:, 0:256],
                    op0=ALU.mult, op1=ALU.add), r=[P.b("GS", h), P.b("EBLS", h), P.b("PU", gi)], w=[P.b("GS", h)])
                P.op("act", lambda e: e.activation(out=self.GSB[:, h, :], in_=self.GS[:, h, :], func=AF.Copy),
                     r=[P.b("GS", h)], w=[P.b("GSB", h)])
                P.op("act", lambda e: e.activation(out=self.JUNK[:, 0:256], in_=self.PO[oi][:, 0:256], func=AF.Square,
                                                   accum_out=self.SSG[:, 0:1]), r=[P.b("PO", oi)], w=[P.b("SSG")])
                self.rstd_batch(self.SSG, 1, 1.0 / 256.0, 1e-5, key="SSG")
                P.op("dve", lambda e: e.scalar_tensor_tensor(
                    out=ACCb[:, c, 1024 + h * 256:1024 + (h + 1) * 256], in0=self.PO[oi][:, 0:256],
                    scalar=self.SSG[:, 0:1], in1=ACCb[:, c, 1024 + h * 256:1024 + (h + 1) * 256],
                    op0=ALU.mult, op1=ALU.mult), r=[P.b("PO", oi), P.b("SSG"), P.b("ACC", c, 1)], w=[P.b("ACC", c, 1)])
        for tt in range(NTT):
            self.transpose_to_XT(ACCb[:, tt, 1024:2048], P.b("ACC", tt, 1), tt)
        self.out_proj("gla_wo", (), None)
        self.post_norm(l * 6 + 3, half=False)


    def proj_mix_fm(self, wa, wb_, c0, M, tb, ps, pkey):
        P = self.P
        n = 0
        for (wbuf, xt) in ((wa, self.XT), (wb_, self.XTs)):
            for kc in range(8):
                P.op("pe", lambda e: e.matmul(
                    ps[:M, :], lhsT=self.WIN[wbuf][:, kc * 512 + c0: kc * 512 + c0 + M],
                    rhs=xt[:, kc, tb * 512:(tb + 1) * 512], start=(n == 0), stop=(n == 15)),
                    r=[P.b("WIN", wbuf), P.b("XTC")] + [P.b("XT", tb * 4 + i) for i in range(max(0, -1), 4)]
                    + ([P.b("XT", tb * 4 - 1)] if tb > 0 else []), w=[pkey])
                n += 1

    def load_mix(self, name, blk, mu0, ranges):
        P = self.P
        src = self.dram[name].ap()[blk]
        P.op("pool", lambda e: e.dma_start(out=self.WIN[0][:, :], in_=src), w=[P.b("WIN", 0)], dma=True)
        for (c0, c1, mi) in ranges:
            for kc in range(8):
                P.op("act", lambda e: e.activation(out=self.WIN[1][:, kc * 512 + c0: kc * 512 + c1],
                                                   in_=self.WIN[0][:, kc * 512 + c0: kc * 512 + c1], func=AF.Copy,
                                                   scale=self.COLS[:, mu0 + mi * 8 + kc: mu0 + mi * 8 + kc + 1]),
                     r=[P.b("WIN", 0), P.b("COLS")], w=[P.b("WIN", 1)])
            for kc in range(8):
                P.op("act", lambda e: e.activation(out=self.WIN[0][:, kc * 512 + c0: kc * 512 + c1],
                                                   in_=self.WIN[0][:, kc * 512 + c0: kc * 512 + c1], func=AF.Copy,
                                                   scale=self.OMMU[:, mi * 8 + kc: mi * 8 + kc + 1]),
                     r=[P.b("WIN", 0), P.b("OMMU")], w=[P.b("WIN", 0)])

    def rwkv(self, l):
        P = self.P
        half = self.half
        R0 = self.cfg["row_rwkv"]
        C0 = self.cfg["col_rwkv"]
        CMU, CW0, CA0, CKK, CKA, CRK = C0, C0 + 48, C0 + 56, C0 + 64, C0 + 72, C0 + 80
        c0e = float(np.exp(-0.5))
        f32v = lambda t: t.rearrange("p a b -> p (a b)").bitcast(F32) if len(t.shape) == 3 else t.bitcast(F32)
        T0 = f32v(self.ATS[0][:, :, :]); k0 = [P.b("ATS", 0, a, b) for a in range(2) for b in range(2)]
        T1 = f32v(self.ATS[1][:, :, :]); k1 = [P.b("ATS", 1, a, b) for a in range(2) for b in range(2)]
        T2 = f32v(self.WOUT[0][:, :]); k2 = [P.b("WOUT", 0)]
        T3 = f32v(self.WOUT[1][:, :]); k3 = [P.b("WOUT", 1)]
        KTf = f32v(self.KT[:, :, :])
        T4 = KTf[:, 0:1024]; k4 = [P.b("KT", i) for i in range(-1, 8)]
        T5 = KTf[:, 1024:2048]; k5 = [P.b("KTb")]
        T6 = f32v(self.VA[:, :, :])[:, 0:1024]; k6 = [P.b("VA", i) for i in range(-1, 8)]
        ACCb = self.ACC[:, :, :].rearrange("p a b -> p (a b)").bitcast(BF16).rearrange("p (a b) -> p a b", a=NTT)
        AR = self.QT[:, 0:2, :].rearrange("p a (c t) -> p (a c t)", t=128).rearrange("p (c a t) -> p c a t", a=2, t=128)
        u128 = lambda ap: ap.rearrange("p (u t) -> p u t", t=128)
        u64 = lambda ap: ap.rearrange("p (u t) -> p u t", t=64)
        W0k = self.WIN[0][:, :].rearrange("p (k u t) -> p k u t", k=2, t=128)
        W1k = self.WIN[1][:, :].rearrange("p (k u t) -> p k u t", k=2, t=128)
        LAK, MRK, PN, MRB = W0k[:, 0], W0k[:, 1], W1k[:, 0], W1k[:, 1]
        PTN = u128(self.ATS[0][:, :, :].rearrange("p a b -> p (a b)"))
        XX = u128(self.ATS[1][:, :, :].rearrange("p a b -> p (a b)"))
        ATOK, BHT = u64(self.WOUT[0][:, 0:1024]), u64(self.WOUT[0][:, 1024:2048])
        KHT, W0s = u64(self.WOUT[1][:, 0:1024]), u64(self.WOUT[1][:, 1024:2048])
        KTb = self.KT[:, :, :].rearrange("p a b -> p (a b)")
        U0s, AHs, MTB = u64(KTb[:, 0:1024]), u64(KTb[:, 1024:2048]), u128(KTb[:, 2048:4096])
        VAb = self.VA[:, :, :].rearrange("p a b -> p (a b)")
        DD, SALL = u64(VAb[:, 0:1024]), u64(VAb[:, 1024:1024 + 17 * 64])
        DG = u64(self.QT[:, 6, :])
        c3t = lambda t: t.rearrange("p (c t) -> p c t", t=128)
        kAR = [P.b("QT", 0, 0), P.b("QT", 0, 1), P.b("QT", 1, 0), P.b("QT", 1, 1)]
        KTL = self.QT[:, 2, :]; kKTL = [P.b("QT", 2, 0), P.b("QT", 2, 1)]
        BTL = self.QT[:, 3, :]; kBTL = [P.b("QT", 3, 0), P.b("QT", 3, 1)]
        KH = self.QT[:, 4, :]; kKH = [P.b("QT", 4, 0), P.b("QT", 4, 1)]
        BH = self.QT[:, 5, :]; kBH = [P.b("QT", 5, 0), P.b("QT", 5, 1)]
        PB = self.QT[:, 6, :]; kPB = [P.b("QT", 6, 0), P.b("QT", 6, 1)]
        TMPB = self.QT[:, 7, :]; kTMPB = [P.b("QT", 7, 0), P.b("QT", 7, 1)]
        c3 = lambda t: t.rearrange("p (c t) -> p c t", t=64)

        self.norm_T(l * 6 + 2)
        if half == 0:
            P.op("dve", lambda e: e.memset(self.XTfull[:, :, 7:8], 0.0), w=[P.b("XTC")])
            P.op("dve", lambda e: e.memset(self.STC[:, :, :], 0.0), w=[P.b("STC", i) for i in range(8)])

        else:
            P.op("dve", lambda e: e.tensor_copy(out=self.XTfull[:, :, 7:8], in_=self.XC[:, :].unsqueeze(2)),
                 r=[P.b("XC")], w=[P.b("XTC")])
        P.op("dve", lambda e: e.tensor_scalar(out=self.OMMU[:, :], in0=self.COLS[:, CMU:CMU + 48], scalar1=-1.0,
                                              scalar2=1.0, op0=ALU.mult, op1=ALU.add), r=[P.b("COLS")], w=[P.b("OMMU")])
        P.op("dve", lambda e: e.tensor_scalar(out=self.OMKA[:, :], in0=self.COLS[:, CKA:CKA + 8], scalar1=-1.0,
                                              scalar2=1.0, op0=ALU.mult, op1=ALU.add), r=[P.b("COLS")], w=[P.b("OMKA")])
        self.load_mix("rwkv_wl1", 0, CMU, [(0, 64, 1), (64, 128, 4), (128, 288, 5)])
        for tb in range(2):
            ts = slice(tb * 512, (tb + 1) * 512)
            for (c0_, M, fn, dst, dkey) in ((0, 64, AF.Tanh, self.LW1[0:64, ts], P.b("LW1", tb)),
                                           (64, 64, AF.Copy, self.LA1[0:64, ts], P.b("LA1", tb)),
                                           (128, 128, AF.Sigmoid, self.XS[0][:, ts], P.b("XS", 0)),
                                           (256, 32, AF.Sigmoid, self.XS[1][0:32, ts], P.b("XS", 1))):
                gi = self.rot("pgu", 2)
                self.proj_mix_fm(0, 1, c0_, M, tb, self.PG[gi], P.b("PG", gi))
                P.op("act", lambda e: e.activation(out=dst, in_=self.PG[gi][0:M, :], func=fn),
                     r=[P.b("PG", gi)], w=[dkey])
        P.op("pool", lambda e: e.dma_start(out=self.L2W[0:64, :], in_=self.dram["rwkv_w2"].ap()[:, :]), w=[P.b("L2W")], dma=True)
        P.op("pool", lambda e: e.dma_start(out=self.L2A[0:64, :], in_=self.dram["rwkv_a2"].ap()[:, :]), w=[P.b("L2A")], dma=True)
        for blk in range(2):
            self.load_mix("rwkv_wv", blk, CMU, [(0, 512, 3)])
            for tt in range(NTT):
                oi = self.rot("po", 2)
                n = 0
                for (wbuf, xt) in ((0, self.XT), (1, self.XTs)):
                    for kc in range(8):
                        P.op("pe", lambda e: e.matmul(
                            self.PO[oi][:, :], lhsT=xt[:, kc, tt * 128:(tt + 1) * 128],
                            rhs=self.WIN[wbuf][:, kc * 512:(kc + 1) * 512], start=(n == 0), stop=(n == 15)),
                            r=[P.b("WIN", wbuf), P.b("XT", tt), P.b("XTC")] + ([P.b("XT", tt - 1)] if tt > 0 else []),
                            w=[P.b("PO", oi)])
                        n += 1
                P.op("act", lambda e: e.activation(out=ACCb[:, tt, blk * 512:(blk + 1) * 512], in_=self.PO[oi][:, :],
                                                   func=AF.Copy), r=[P.b("PO", oi)], w=[P.b("ACC", tt, 0)])
        for kc in range(8):
            blk, cc = kc // 4, (kc % 4) * 128
            self.load_mix("rwkv_wr", blk, CMU, [(cc, cc + 128, 0)])
            for tb in range(2):
                gi = self.rot("pgu", 2)
                self.proj_mix_fm(0, 1, cc, 128, tb, self.PG[gi], P.b("PG", gi))
                P.op("act", lambda e: e.activation(out=T0[:, tb * 512:(tb + 1) * 512], in_=self.PG[gi][:, :], func=AF.Copy),
                     r=[P.b("PG", gi)], w=k0)
            self.load_mix("rwkv_wk", blk, CMU, [(cc, cc + 128, 2)])
            for tb in range(2):
                gi = self.rot("pgu", 2)
                self.proj_mix_fm(0, 1, cc, 128, tb, self.PG[gi], P.b("PG", gi))
                P.op("act", lambda e: e.activation(out=T1[:, tb * 512:(tb + 1) * 512], in_=self.PG[gi][:, :], func=AF.Copy),
                     r=[P.b("PG", gi)], w=k1)
            for tb in range(2):
                ts = slice(tb * 512, (tb + 1) * 512)
                gi = self.rot("pgu", 2)
                P.op("pe", lambda e: e.matmul(self.PU[gi][:, :], lhsT=self.L2W[0:64, kc * 128:(kc + 1) * 128],
                                              rhs=self.LW1[0:64, ts], start=True, stop=True),
                     r=[P.b("L2W"), P.b("LW1", tb)], w=[P.b("PU", gi)])
                P.op("act", lambda e: e.activation(out=T2[:, ts], in_=self.PU[gi][:, :], func=AF.Sigmoid,
                                                   bias=self.COLS[:, CW0 + kc:CW0 + kc + 1], scale=1.0),
                     r=[P.b("PU", gi), P.b("COLS")], w=k2)
            for c in range(16):
                cs = slice(c * 64, (c + 1) * 64)
                P.op("dve", lambda e: e.tensor_tensor_scan(out=T3[:, cs], data0=self.ONES[:, 0:64], data1=T2[:, cs],
                                                           initial=0.0, op0=ALU.mult, op1=ALU.add),
                     r=k2 + [P.b("ONES")], w=k3)
            P.op("dve", lambda e: e.tensor_tensor(out=T4[:, :], in0=T3[:, :], in1=T2[:, :], op=ALU.subtract),
                 r=k2 + k3, w=k4)
            P.op("act", lambda e: e.activation(out=T4[:, :], in_=T4[:, :], func=AF.Exp, scale=-c0e), r=k4, w=k4)
            P.op("act", lambda e: e.activation(out=T5[:, :], in_=T3[:, :], func=AF.Exp, scale=-c0e), r=k3, w=k5)
            P.op("act", lambda e: e.activation(out=T3[:, :], in_=T3[:, :], func=AF.Exp, scale=c0e), r=k3, w=k3)
            P.op("dve", lambda e: e.tensor_copy(out=self.GCC[:, :], in_=c3(T5)[:, :, 63]), r=k5, w=[P.b("GCC")])
            for tb in range(2):
                ts = slice(tb * 512, (tb + 1) * 512)
                gi = self.rot("pgu", 2)
                P.op("pe", lambda e: e.matmul(self.PU[gi][:, :], lhsT=self.L2A[0:64, kc * 128:(kc + 1) * 128],
                                              rhs=self.LA1[0:64, ts], start=True, stop=True),
                     r=[P.b("L2A"), P.b("LA1", tb)], w=[P.b("PU", gi)])
                P.op("act", lambda e: e.activation(out=T2[:, ts], in_=self.PU[gi][:, :], func=AF.Sigmoid,
                                                   bias=self.COLS[:, CA0 + kc:CA0 + kc + 1], scale=1.0),
                     r=[P.b("PU", gi), P.b("COLS")], w=k2)
            P.op("dve", lambda e: e.tensor_scalar(out=T6[:, :], in0=T1[:, :], scalar1=self.COLS[:, CKK + kc:CKK + kc + 1],
                                                  scalar2=None, op0=ALU.mult), r=k1 + [P.b("COLS")], w=k6)
            P.op("dve", lambda e: e.tensor_tensor(out=TMPB, in0=T6[:, :], in1=T6[:, :], op=ALU.mult), r=k6, w=kTMPB)
            for tb in range(2):
                ts = slice(tb * 512, (tb + 1) * 512)
                gi = self.rot("pgu", 2)
                P.op("pe", lambda e: e.matmul(self.PU[gi][:, :], lhsT=self.BLK[:, :], rhs=TMPB[:, ts], start=True, stop=True),
                     r=[P.b("BLK")] + kTMPB, w=[P.b("PU", gi)])
                P.op("act", lambda e: e.activation(out=self.GB[0][:, 0:512], in_=self.PU[gi][:, :], func=AF.Sqrt),
                     r=[P.b("PU", gi)], w=[P.b("GB", 0)])
                P.op("dve", lambda e: e.tensor_scalar(out=self.GB[0][:, 0:512], in0=self.GB[0][:, 0:512], scalar1=1e-12,
                                                      scalar2=None, op0=ALU.max), r=[P.b("GB", 0)], w=[P.b("GB", 0)])
                P.op("dve", lambda e: e.reciprocal(out=self.GB[0][:, 0:512], in_=self.GB[0][:, 0:512]),
                     r=[P.b("GB", 0)], w=[P.b("GB", 0)])
                P.op("dve", lambda e: e.tensor_tensor(out=T6[:, ts], in0=T6[:, ts], in1=self.GB[0][:, 0:512], op=ALU.mult),
                     r=k6 + [P.b("GB", 0)], w=k6)
            P.op("dve", lambda e: e.tensor_scalar(out=TMPB, in0=T2[:, :], scalar1=self.COLS[:, CKA + kc:CKA + kc + 1],
                                                  scalar2=self.OMKA[:, kc:kc + 1], op0=ALU.mult, op1=ALU.add),
                 r=k2 + [P.b("COLS"), P.b("OMKA")], w=kTMPB)
            P.op("dve", lambda e: e.tensor_tensor(out=T1[:, :], in0=T1[:, :], in1=TMPB, op=ALU.mult), r=k1 + kTMPB, w=k1)
            P.op("dve", lambda e: e.scalar_tensor_tensor(out=AR[:, :, 0, :], in0=c3t(T6), scalar=-1.0, in1=c3t(T4),
                                                         op0=ALU.mult, op1=ALU.mult), r=k6 + k4, w=kAR)
            P.op("dve", lambda e: e.tensor_tensor(out=AR[:, :, 1, :], in0=c3t(T0), in1=c3t(T5), op=ALU.mult), r=k0 + k5, w=kAR)
            P.op("dve", lambda e: e.tensor_tensor(out=KTL, in0=T1[:, :], in1=T3[:, :], op=ALU.mult), r=k1 + k3, w=kKTL)
            P.op("dve", lambda e: e.tensor_tensor(out=T6[:, :], in0=T6[:, :], in1=T2[:, :], op=ALU.mult), r=k6 + k2, w=k6)
            P.op("dve", lambda e: e.tensor_tensor(out=BTL, in0=T6[:, :], in1=T3[:, :], op=ALU.mult), r=k6 + k3, w=kBTL)
            gcb = self.GCC[:, :].unsqueeze(2).to_broadcast([128, 16, 64])
            P.op("dve", lambda e: e.tensor_tensor(out=c3(KH), in0=c3(KTL), in1=gcb, op=ALU.mult), r=kKTL + [P.b("GCC")], w=kKH)
            P.op("dve", lambda e: e.tensor_tensor(out=c3(BH), in0=c3(BTL), in1=gcb, op=ALU.mult), r=kBTL + [P.b("GCC")], w=kBH)
            P.op("dve", lambda e: e.scalar_tensor_tensor(out=PB, in0=T0[:, :], scalar=self.COLS[:, CRK + kc:CRK + kc + 1],
                                                         in1=T1[:, :], op0=ALU.mult, op1=ALU.mult),
                 r=k0 + k1 + [P.b("COLS")], w=kPB)
            gi = self.rot("pgu", 2)
            for tt in range(NTT):
                P.op("pe", lambda e: e.matmul(self.PU[gi][:, tt * 2:tt * 2 + 2], lhsT=PB[:, tt * 128:(tt + 1) * 128],
                                              rhs=self.HSEL[:, :], start=True, stop=True),
                     r=kPB + [P.b("HSEL")], w=[P.b("PU", gi)])
            P.op("act", lambda e: e.activation(out=self.BS[:, :, 2 * kc:2 * kc + 2],
                                               in_=self.PU[gi][:, 0:16].rearrange("p (t h) -> p t h", h=2), func=AF.Copy),
                 r=[P.b("PU", gi)], w=[P.b("BS", kc)])
            prs = [slice(0, 64), slice(64, 128)]
            KB = lambda kind, bt: P.b("RK", kind, bt)
            kall = lambda kind: [P.b("RK", kind, bt) for bt in range(4)]
            alias = k0 + k1 + k2 + k3 + k4 + k5 + k6 + kPB + [P.b("WIN", 0), P.b("WIN", 1)]
            rkk = [P.b("RK", kd, bt) for kd in ("LAK", "MRK", "PN", "MRB", "PTN", "XX", "ATOK", "BHT", "KHT", "W0", "U0", "AH", "MTB")
                   for bt in range(4)] + [P.b("DD"), P.b("DG")] + [P.b("SALL", i) for i in range(17)]
            P.op("pool", lambda e: e.memset(self.SEMT[:, 0:1], 0.0), w=alias + rkk + [P.b("SEMT")])
            P.op("pool", lambda e: e.memset(MTB[:, :, :], 0.0), w=kall("MTB"))
            P.op("pool", lambda e: e.tensor_tensor(out=DG, in0=self.ID2[:, :].unsqueeze(1).to_broadcast([128, 16, 64]),
                                                   in1=self.GCC[:, :].unsqueeze(2).to_broadcast([128, 16, 64]), op=ALU.mult),
                 r=[P.b("GCC"), P.b("RMASK")], w=[P.b("DG")])
            P.op("act", lambda e: e.activation(out=SALL[:, 0, :], in_=self.STC[:, kc, :], func=AF.Copy),
                 r=[P.b("STC", kc)], w=[P.b("SALL", 0)])
            for cp in range(8):
                tl = slice(cp * 128, (cp + 1) * 128)
                for hp in range(2):
                    pr = prs[hp]
                    u = cp * 2 + hp
                    bt = u // 4
                    for (src, off, key) in ((KTL, 0, kKTL), (BTL, 256, kBTL)):
                        P.op("pe", lambda e: e.matmul(self.PO[hp][:, off:off + 256], lhsT=src[pr, tl],
                                                      rhs=AR[pr, cp, :, :], start=True, stop=True),
                             r=key + kAR, w=[P.b("PO", hp)])
                    P.op("pe", lambda e: e.matmul(self.PU[hp][:, 0:128], lhsT=AR[pr, cp, 0, :], rhs=BTL[pr, tl],
                                                  start=True, stop=True), r=kAR + kBTL, w=[P.b("PU", hp)])
                    m2 = self.RMASK[:, 0:256].rearrange("p (a t) -> p a t", a=2)
                    P.op("dve", lambda e: e.tensor_tensor(out=W0k[:, :, u, :], in0=self.PO[hp][:, 0:256].rearrange("p (a t) -> p a t", a=2),
                                                          in1=m2, op=ALU.mult),
                         r=[P.b("PO", hp), P.b("RMASK")], w=[KB("LAK", bt), KB("MRK", bt)])
                    P.op("dve", lambda e: e.tensor_tensor(out=W1k[:, :, u, :], in0=self.PO[hp][:, 256:512].rearrange("p (a t) -> p a t", a=2),
                                                          in1=m2, op=ALU.mult),
                         r=[P.b("PO", hp), P.b("RMASK")], w=[KB("PN", bt), KB("MRB", bt)])
                    P.op("dve", lambda e: e.tensor_tensor(out=PTN[:, u, :], in0=self.PU[hp][:, 0:128], in1=self.RMASK[:, 256:384], op=ALU.mult),
                         r=[P.b("PU", hp), P.b("RMASK")], w=[KB("PTN", bt)])
            for bt in range(4):
                P.op("pool", lambda e: e.tensor_tensor(out=XX[:, bt * 4:(bt + 1) * 4, :], in0=PN[:, bt * 4:(bt + 1) * 4, :],
                                                       in1=self.IDB[:, :].unsqueeze(1).to_broadcast([128, 4, 128]), op=ALU.add),
                     r=[KB("PN", bt), P.b("IDB")], w=[KB("XX", bt)])
            for (dst, dkind, srcf, skey) in ((ATOK, "ATOK", lambda cp, pr: AR[pr, cp, 0, :], kAR),
                                             (BHT, "BHT", lambda cp, pr: BH[pr, cp * 128:(cp + 1) * 128], kBH),
                                             (KHT, "KHT", lambda cp, pr: KH[pr, cp * 128:(cp + 1) * 128], kKH)):
                for u in range(16):
                    cp, hp = u // 2, u % 2
                    pt = self.PT if hp == 0 else self.PT2
                    P.op("pe", lambda e: e.transpose(out=pt[:, cp * 64:(cp + 1) * 64], in_=srcf(cp, prs[hp]),
                                                     identity=self.IDB[prs[hp], hp * 64:(hp + 1) * 64]),
                         r=skey + [P.b("IDB")], w=[P.b("PT" if hp == 0 else "PT2")])
                for hp in range(2):
                    pt = self.PT if hp == 0 else self.PT2
                    P.op("act" if hp == 0 else "dve", lambda e: (e.activation(
                        out=dst[:, hp:16:2, :], in_=pt[:, 0:512].rearrange("p (u t) -> p u t", t=64), func=AF.Copy) if hp == 0 else
                        e.tensor_copy(out=dst[:, hp:16:2, :], in_=pt[:, 0:512].rearrange("p (u t) -> p u t", t=64))),
                        r=[P.b("PT" if hp == 0 else "PT2")], w=kall(dkind))
            for m in range(5):
                for bt in range(4):
                    us = range(bt * 4, bt * 4 + 4)
                    pb = bt % 2
                    for i, u in enumerate(us):
                        P.op("pe", lambda e: e.matmul(self.PG[pb][:, i * 128:(i + 1) * 128], lhsT=PN[:, u, :], rhs=PTN[:, u, :],
                                                      start=True, stop=True), r=[KB("PN", bt), KB("PTN", bt)], w=[P.b("PG", pb)])
                    if m < 4:
                        for i, u in enumerate(us):
                            P.op("pe", lambda e: e.matmul(self.PU[pb][:, i * 128:(i + 1) * 128], lhsT=PTN[:, u, :], rhs=PN[:, u, :],
                                                          start=True, stop=True), r=[KB("PN", bt), KB("PTN", bt)], w=[P.b("PU", pb)])
                    P.op("act", lambda e: e.activation(out=PTN[:, bt * 4:(bt + 1) * 4, :],
                                                       in_=self.PG[pb][:, :].rearrange("p (u t) -> p u t", t=128), func=AF.Copy),
                         r=[P.b("PG", pb)], w=[KB("PTN", bt)])
                    if m < 4:
                        P.op("dve", lambda e: e.tensor_copy(out=PN[:, bt * 4:(bt + 1) * 4, :],
                                                            in_=self.PU[pb][:, :].rearrange("p (u t) -> p u t", t=128)),
                             r=[P.b("PU", pb)], w=[KB("PN", bt)])
                    for i, u in enumerate(us):
                        P.op("pe", lambda e: e.matmul(self.PO[pb][:, i * 128:(i + 1) * 128], lhsT=PTN[:, u, :], rhs=XX[:, u, :],
                                                      start=True, stop=True), r=[KB("PTN", bt), KB("XX", bt)], w=[P.b("PO", pb)])
                    P.op("dve", lambda e: e.tensor_tensor(out=XX[:, bt * 4:(bt + 1) * 4, :], in0=XX[:, bt * 4:(bt + 1) * 4, :],
                                                          in1=self.PO[pb][:, :].rearrange("p (u t) -> p u t", t=128), op=ALU.add),
                         r=[KB("XX", bt), P.b("PO", pb)], w=[KB("XX", bt)])
            vcol = lambda u: ACCb[:, u // 2, (2 * kc + u % 2) * 64:(2 * kc + u % 2 + 1) * 64]
            for b8 in range(2):
                for i in range(8):
                    u = b8 * 8 + i
                    P.op("pe", lambda e: e.matmul(self.PG[b8][:, i * 64:(i + 1) * 64], lhsT=LAK[:, u, :], rhs=vcol(u), start=True, stop=True),
                         r=[KB("LAK", u // 4), P.b("ACC", u // 2, 0)], w=[P.b("PG", b8)])
                P.op("act", lambda e: e.activation(out=W0s[:, b8 * 8:(b8 + 1) * 8, :], in_=self.PG[b8][:, :].rearrange("p (u t) -> p u t", t=64),
                                                   func=AF.Copy), r=[P.b("PG", b8)], w=[KB("W0", 2 * b8), KB("W0", 2 * b8 + 1)])
            for b8 in range(2):
                for i in range(8):
                    u = b8 * 8 + i
                    P.op("pe", lambda e: e.matmul(self.PO[b8][:, i * 64:(i + 1) * 64], lhsT=XX[:, u, :], rhs=ATOK[:, u, :], start=True, stop=True),
                         r=[KB("XX", u // 4), KB("ATOK", u // 4)], w=[P.b("PO", b8)])
                P.op("dve", lambda e: e.tensor_copy(out=AHs[:, b8 * 8:(b8 + 1) * 8, :], in_=self.PO[b8][:, :].rearrange("p (u t) -> p u t", t=64)),
                     r=[P.b("PO", b8)], w=[KB("AH", 2 * b8), KB("AH", 2 * b8 + 1)])
            for b8 in range(2):
                for i in range(8):
                    u = b8 * 8 + i
                    P.op("pe", lambda e: e.matmul(self.PU[b8][:, i * 64:(i + 1) * 64], lhsT=XX[:, u, :], rhs=W0s[:, u, :], start=True, stop=True),
                         r=[KB("XX", u // 4), KB("W0", u // 4)], w=[P.b("PU", b8)])
                P.op("act", lambda e: e.activation(out=U0s[:, b8 * 8:(b8 + 1) * 8, :], in_=self.PU[b8][:, :].rearrange("p (u t) -> p u t", t=64),
                                                   func=AF.Copy), r=[P.b("PU", b8)], w=[KB("U0", 2 * b8), KB("U0", 2 * b8 + 1)])
            for cpar in range(2):
                tp = prs[cpar]
                for u in range(16):
                    cp, hp = u // 2, u % 2
                    P.op("pe", lambda e: e.matmul(self.PG[cpar][prs[hp], cp * 64:(cp + 1) * 64], lhsT=AHs[tp, u, :], rhs=BHT[tp, u, :],
                                                  start=True, stop=True), r=[KB("AH", u // 4), KB("BHT", u // 4)], w=[P.b("PG", cpar)])
                for hp in range(2):
                    P.op("dve", lambda e: e.tensor_tensor(
                        out=MTB[prs[hp], cpar:16:2, hp * 64:(hp + 1) * 64],
                        in0=self.PG[cpar][prs[hp], :].rearrange("p (c t) -> p c t", t=64),
                        in1=DG[prs[hp], cpar:16:2, :], op=ALU.add),
                        r=[P.b("PG", cpar), P.b("DG")], w=kall("MTB"))
                for u in range(16):
                    cp, hp = u // 2, u % 2
                    P.op("pe", lambda e: e.matmul(self.PU[cpar][prs[hp], cp * 64:(cp + 1) * 64], lhsT=BHT[tp, u, :], rhs=U0s[tp, u, :],
                                                  start=True, stop=False), r=[KB("BHT", u // 4), KB("U0", u // 4)], w=[P.b("PU", cpar)])
                    P.op("pe", lambda e: e.matmul(self.PU[cpar][prs[hp], cp * 64:(cp + 1) * 64], lhsT=KHT[tp, u, :],
                                                  rhs=ACCb[tp, cp, (2 * kc + hp) * 64:(2 * kc + hp + 1) * 64],
                                                  start=False, stop=True), r=[KB("KHT", u // 4), P.b("ACC", cp, 0)], w=[P.b("PU", cpar)])
                P.op("act", lambda e: e.activation(out=DD[:, cpar:16:2, :], in_=self.PU[cpar][:, :].rearrange("p (c t) -> p c t", t=64),
                                                   func=AF.Copy), r=[P.b("PU", cpar)], w=[P.b("DD")])
            for b4 in range(2):
                for i in range(4):
                    cp = b4 * 4 + i
                    for hp in range(2):
                        u = cp * 2 + hp
                        P.op("pe", lambda e: e.matmul(self.PO[b4][prs[hp], i * 128:(i + 1) * 128], lhsT=AHs[:, u, :], rhs=MRB[:, u, :],
                                                      start=True, stop=True), r=[KB("AH", u // 4), KB("MRB", u // 4)], w=[P.b("PO", b4)])
                P.op("dve", lambda e: e.tensor_tensor(out=AR[:, b4 * 4:(b4 + 1) * 4, 1, :], in0=AR[:, b4 * 4:(b4 + 1) * 4, 1, :],
                                                      in1=self.PO[b4][:, :].rearrange("p (c t) -> p c t", t=128), op=ALU.add),
                     r=kAR + [P.b("PO", b4)], w=kAR)
            for c in range(16):
                pb = c % 2
                P.op("pe", lambda e: e.matmul(self.PG[pb][:, 0:64], lhsT=MTB[:, c, :], rhs=SALL[:, c, :], start=True, stop=True),
                     r=kall("MTB") + [P.b("SALL", c)], w=[P.b("PG", pb)])
                P.op("dve", lambda e: e.tensor_tensor(out=SALL[:, c + 1, :], in0=self.PG[pb][:, 0:64], in1=DD[:, c, :], op=ALU.add),
                     r=[P.b("PG", pb), P.b("DD")], w=[P.b("SALL", c + 1)])
            P.op("act", lambda e: e.activation(out=self.STC[:, kc, :], in_=SALL[:, 16, :], func=AF.Copy),
                 r=[P.b("SALL", 16)], w=[P.b("STC", kc)])
            for hp in range(2):
                pr = prs[hp]
                for cp in range(8):
                    u = cp * 2 + hp
                    oc = slice(cp * 64, (cp + 1) * 64)
                    P.op("pe", lambda e: e.matmul(self.PO[hp][:, oc], lhsT=MRB[:, u, :], rhs=U0s[:, u, :], start=True, stop=False),
                         r=[KB("MRB", u // 4), KB("U0", u // 4)], w=[P.b("PO", hp)])
                    P.op("pe", lambda e: e.matmul(self.PO[hp][:, oc], lhsT=MRK[:, u, :], rhs=vcol(u), start=False, stop=False),
                         r=[KB("MRK", u // 4), P.b("ACC", cp, 0)], w=[P.b("PO", hp)])
                    for cpar in range(2):
                        c = 2 * cp + cpar
                        P.op("pe", lambda e: e.matmul(self.PO[hp][prs[cpar], oc], lhsT=AR[pr, cp, 1, cpar * 64:(cpar + 1) * 64],
                                                      rhs=SALL[pr, c, :], start=False, stop=True),
                             r=kAR + [P.b("SALL", c)], w=[P.b("PO", hp)])
                hd = 2 * kc + hp
                P.op("act", lambda e: e.activation(out=ACCb[:, :, 1024 + hd * 64:1024 + (hd + 1) * 64],
                                                   in_=self.PO[hp][:, :].rearrange("p (c t) -> p c t", t=64), func=AF.Copy),
                     r=[P.b("PO", hp)], w=[P.b("ACC", tt, 1) for tt in range(8)])
            P.op("pool", lambda e: e.memset(self.SEMT[:, 0:1], 0.0), w=alias + rkk + [P.b("SEMT")])
        if half == 0:
            P.op("dve", lambda e: e.tensor_copy(out=self.XC[:, :].unsqueeze(2), in_=self.XTfull[:, :, 8 + TB - 1:8 + TB]),
                 r=[P.b("XT", 7)], w=[P.b("XC")])
        g1 = self.load_row_bc(R0 + 0)
        g2 = self.load_row_bc(R0 + 1)
        h3 = lambda t: t.rearrange("p (h d) -> p h d", d=64)
        for c in range(NTT):
            yb = ACCb[:, c, 1024:2048]
            ky = [P.b("ACC", c, 1)]
            P.op("dve", lambda e: e.tensor_reduce(out=self.S1[:, :], in_=h3(yb), axis=mybir.AxisListType.X, op=ALU.add),
                 r=ky, w=[P.b("S1")])
            P.op("dve", lambda e: e.tensor_tensor(out=T0[:, :], in0=yb, in1=yb, op=ALU.mult), r=ky, w=k0)
            P.op("dve", lambda e: e.tensor_reduce(out=self.S2[:, :], in_=h3(T0[:, :]), axis=mybir.AxisListType.X, op=ALU.add),
                 r=k0, w=[P.b("S2")])
            P.op("dve", lambda e: e.tensor_scalar(out=self.S1[:, :], in0=self.S1[:, :], scalar1=1.0 / 64, scalar2=None,
                                                  op0=ALU.mult), r=[P.b("S1")], w=[P.b("S1")])
            P.op("dve", lambda e: e.tensor_tensor(out=self.S3[:, :], in0=self.S1[:, :], in1=self.S1[:, :], op=ALU.mult),
                 r=[P.b("S1")], w=[P.b("S3")])
            P.op("dve", lambda e: e.scalar_tensor_tensor(out=self.S2[:, :], in0=self.S2[:, :], scalar=1.0 / 64, in1=self.S3[:, :],
                                                         op0=ALU.mult, op1=ALU.subtract), r=[P.b("S2"), P.b("S3")], w=[P.b("S2")])
            self.rstd_batch(self.S2, 16, 1.0, 64e-5, key="S2")
            P.op("dve", lambda e: e.tensor_tensor(out=h3(T0[:, :]), in0=h3(yb), in1=self.S1[:, :].unsqueeze(2).to_broadcast([128, 16, 64]),
                                                  op=ALU.subtract), r=ky + [P.b("S1")], w=k0)
            P.op("dve", lambda e: e.tensor_tensor(out=h3(T0[:, :]), in0=h3(T0[:, :]), in1=self.S2[:, :].unsqueeze(2).to_broadcast([128, 16, 64]),
                                                  op=ALU.mult), r=k0 + [P.b("S2")], w=k0)
            P.op("dve", lambda e: e.tensor_tensor(out=T0[:, :], in0=T0[:, :], in1=self.GB[g1][:, :], op=ALU.mult),
                 r=k0 + [P.b("GB", g1)], w=k0)
            P.op("dve", lambda e: e.tensor_tensor(out=T0[:, :], in0=T0[:, :], in1=self.GB[g2][:, :], op=ALU.add),
                 r=k0 + [P.b("GB", g2)], w=k0)
            P.op("dve", lambda e: e.tensor_tensor(out=h3(T1[:, :]), in0=h3(ACCb[:, c, 0:1024]),
                                                  in1=self.BS[:, c, :].unsqueeze(2).to_broadcast([128, 16, 64]), op=ALU.mult),
                 r=[P.b("ACC", c, 0)] + [P.b("BS", i) for i in range(8)], w=k1)
            P.op("dve", lambda e: e.tensor_tensor(out=yb, in0=T0[:, :], in1=T1[:, :], op=ALU.add), r=k0 + k1, w=ky)
        P.op("pool", lambda e: e.dma_start(out=self.L2W[:, :], in_=self.dram["rwkv_g2"].ap()[0:128, :]), w=[P.b("L2W")], dma=True)
        P.op("pool", lambda e: e.dma_start(out=self.L2A[0:32, :], in_=self.dram["rwkv_g2"].ap()[128:160, :]), w=[P.b("L2A")], dma=True)
        for tt in range(NTT):
            tsl = slice(tt * 128, (tt + 1) * 128)
            for kc in range(8):
                P.op("pe", lambda e: e.transpose(out=self.PT[:, kc * 128:(kc + 1) * 128], in_=ACCb[:, tt, 1024 + kc * 128:1024 + (kc + 1) * 128],
                                                 identity=self.IDB[:, :]), r=[P.b("ACC", tt, 1), P.b("IDB")], w=[P.b("PT")])
            for kc in range(8):
                pg = self.PG[kc // 4]
                P.op("pe", lambda e: e.matmul(pg[:, (kc % 4) * 128:(kc % 4 + 1) * 128], lhsT=self.L2W[:, kc * 128:(kc + 1) * 128],
                                              rhs=self.XS[0][:, tsl], start=True, stop=False),
                     r=[P.b("L2W"), P.b("XS", 0)], w=[P.b("PG", kc // 4)])
                P.op("pe", lambda e: e.matmul(pg[:, (kc % 4) * 128:(kc % 4 + 1) * 128], lhsT=self.L2A[0:32, kc * 128:(kc + 1) * 128],
                                              rhs=self.XS[1][0:32, tsl], start=False, stop=True),
                     r=[P.b("L2A"), P.b("XS", 1)], w=[P.b("PG", kc // 4)])
            for hh in range(2):
                P.op("act", lambda e: e.activation(out=TMPB[:, hh * 512:(hh + 1) * 512], in_=self.PG[hh][:, :], func=AF.Copy),
                     r=[P.b("PG", hh)], w=kTMPB)
            P.op("dve", lambda e: e.tensor_tensor(out=self.XT[:, :, tsl], in0=self.PT[:, :].rearrange("p (k c) -> p k c", k=8),
                                                  in1=TMPB.rearrange("p (k c) -> p k c", k=8), op=ALU.mult),
                 r=[P.b("PT")] + kTMPB, w=[P.b("XT", tt)])
        self.out_proj("rwkv_wo", (), None)
        self.post_norm(l * 6 + 3, half=False)

    def build(self):
        nc, P, cfg = self.nc, self.P, self.cfg
        x_d = self.din("x", [SEQ, D])
        self.din("rows", [cfg["nrows"], D])
        self.din("ffn_win", [DEPTH, 2, NSLAB, 128, 8 * 512])
        self.din("ffn_wout", [DEPTH, 2, NSLAB, 128, 2 * 1024])
        self.din("idb", [128, 128], BF16)
        self.din("cmask", [128, 1024], BF16)
        self.din("cols", [128, cfg["ncols"]])
        self.din("swa_wq", [2, 2, 128, 4096])
        self.din("gla_win", [7, 128, 4096])
        self.din("rwkv_wl1", [1, 128, 4096])
        self.din("rwkv_wr", [2, 128, 4096])
        self.din("rwkv_wk", [2, 128, 4096])
        self.din("rwkv_wv", [2, 128, 4096])
        self.din("rwkv_wo", [2, 128, 4096])
        self.din("rwkv_w2", [64, 1024])
        self.din("rwkv_a2", [64, 1024])
        self.din("rwkv_g2", [160, 1024])
        self.din("rconst", [128, 384 + 128 + 2 + 64], BF16)
        self.din("gla_wo", [2, 128, 4096])
        self.din("gla_wg2", [16, 512])
        self.din("swa_wk", [2, 128, 4096])
        self.din("swa_wv", [2, 128, 4096])
        self.din("swa_wo", [2, 2, 128, 4096])
        y_d = self.nc.dram_tensor("y", [SEQ, D], F32, kind="ExternalOutput")

        self.H = self.sb("H", [128, NTT, D], F32)
        self.XTfull = self.sb("XTfull", [128, 8, TB + 8], BF16)
        self.XT = self.XTfull[:, :, 8:8 + TB]
        self.XTs = self.XTfull[:, :, 7:7 + TB]
        self.RCONST = self.sb("RCONST", [128, 384 + 128 + 2 + 64], BF16)
        self.RMASK = self.RCONST[:, 0:384]
        self.BLK = self.RCONST[:, 384:512]
        self.HSEL = self.RCONST[:, 512:514]
        self.ID2 = self.RCONST[:, 514:578]
        self.STC = self.sb("STC", [128, 8, 64], BF16)
        self.SEMT = self.sb("SEMT", [128, 4], F32)
        self.XC = self.sb("XC", [128, 8], BF16)
        self.OMMU = self.sb("OMMU", [128, 48], F32)
        self.OMKA = self.sb("OMKA", [128, 8], F32)
        self.GCC = self.sb("GCC", [128, 16], F32)
        self.BS = self.sb("BS", [128, 8, 16], F32)
        self.S1 = self.sb("S1", [128, 16], F32)
        self.S2 = self.sb("S2", [128, 16], F32)
        self.S3 = self.sb("S3", [128, 16], F32)
        self.LW1 = self.sb("LW1", [64, TB], BF16)
        self.LA1 = self.sb("LA1", [64, TB], BF16)
        self.L2W = self.sb("L2W", [128, D], BF16)
        self.L2A = self.sb("L2A", [64, D], BF16)
        self.ACC = self.sb("ACC", [128, NTT, D], F32)
        self.ATS = [self.sb("ATS%d" % i, [128, 2, TB], BF16) for i in range(2)]
        self.WIN = [self.sb("WIN%d" % i, [128, 8 * 512], BF16) for i in range(2)]
        self.WOUT = [self.sb("WOUT%d" % i, [128, 2 * 1024], BF16) for i in range(2)]
        self.GB = [self.sb("GB%d" % i, [128, D], F32) for i in range(2)]
        self.XS = [self.sb("XS%d" % i, [128, D], BF16) for i in range(2)]
        self.SG = [self.sb("SG%d" % i, [128, 512], BF16) for i in range(2)]
        self.JUNK = self.sb("JUNK", [128, D], BF16)
        self.SS = self.sb("SS", [128, 16], F32)
        self.IDB = self.sb("IDB", [128, 128], BF16)
        self.epsc = {EPS: self.sb("eps0", [128, 1], F32), 4 * EPS: self.sb("eps4", [128, 1], F32), 1e-5: self.sb("eps1", [128, 1], F32),
                     64e-5: self.sb("eps2", [128, 1], F32)}
        self.GS = self.sb("GS", [128, 4, 256], F32)
        self.GSB = self.sb("GSB", [128, 4, 256], BF16)
        self.EBLS = self.sb("EBLS", [128, 4, 8], F32)
        self.ONES = self.sb("ONES", [128, 128], F32)
        self.ONE1 = self.ONES
        self.NBG = self.sb("NBG", [128, 4], F32)
        self.SSG = self.sb("SSG", [128, 4], F32)
        self.CMASK = self.sb("CMASK", [128, 1024], BF16)
        self.COLS = self.sb("COLS", [128, cfg["ncols"]], F32)
        self.QT = self.sb("QT", [128, 8, TB], BF16)
        self.KT = self.sb("KT", [128, 4, TB + 128], BF16)
        self.VA = self.sb("VA", [128, 9, 260], BF16)
        self.KC = [self.sb("KC%d" % i, [128, 4, 128], BF16) for i in range(2)]
        self.VC = [self.sb("VC%d" % i, [128, 260], BF16) for i in range(2)]
        self.PTS = [self.sb("PTS%d" % i, [128, 512], BF16) for i in range(4)]
        self.ESK = self.sb("ESK", [128, 16], F32)
        self.BQ8 = self.sb("BQ8", [128, 8], F32)
        self.DEN = self.sb("DEN", [128, 4], F32)

        self.PG = [self.ps("PG%d" % i, [128, 512], F32) for i in range(2)]
        self.PU = [self.ps("PU%d" % i, [128, 512], F32) for i in range(2)]
        self.PO = [self.ps("PO%d" % i, [128, 512], F32) for i in range(2)]
        self.PT = self.ps("PT", [128, 1024], BF16)
        self.PT2 = self.ps("PT2", [128, 1024], BF16)

        P.op("sp", lambda e: e.dma_start(out=self.IDB[:, :], in_=self.dram["idb"].ap()[:, :]), w=[P.b("IDB")], dma=True)
        P.op("sp", lambda e: e.dma_start(out=self.CMASK[:, :], in_=self.dram["cmask"].ap()[:, :]), w=[P.b("CMASK")], dma=True)
        P.op("sp", lambda e: e.dma_start(out=self.COLS[:, :], in_=self.dram["cols"].ap()[:, :]), w=[P.b("COLS")], dma=True)
        P.op("dve", lambda e: e.memset(self.VA[:, :, :], 1.0), w=[P.b("VA", i) for i in range(-1, 8)])
        P.op("dve", lambda e: e.memset(self.ONES[:, :], 1.0), w=[P.b("ONES")])
        P.op("sp", lambda e: e.dma_start(out=self.RCONST[:, :], in_=self.dram["rconst"].ap()[:, :]),
             w=[P.b("RMASK"), P.b("BLK"), P.b("HSEL")], dma=True)
        for eps, t in self.epsc.items():
            P.op("dve", lambda e, t=t, eps=eps: e.memset(t[:, :], eps), w=[P.b("epsc")])

        xv = x_d.ap().rearrange("(n p) d -> p n d", p=128)
        yv = y_d.ap().rearrange("(n p) d -> p n d", p=128)
        stages = cfg["stages"]
        for half in range(2):
            self.half = half
            for tt in range(NTT):
                P.op("sp", lambda e, tt=tt, half=half: e.dma_start(out=self.H[:, tt, :], in_=xv[:, half * NTT + tt, :]),
                     w=[P.b("H", tt)], dma=True)
            self.norm_done = None
            for si, (l, what) in enumerate(stages):
                nxt = stages[si + 1] if si + 1 < len(stages) else None
                self.next_norm_row = None if nxt is None else nxt[0] * 6 + {"a": 0, "m": 2, "b": 4}[nxt[1]]
                if not hasattr(self, "marks"):
                    self.marks = []
                self.marks.append(("h%d L%d %s" % (half, l, what), len(P.q["pe"])))
                if what == "a":
                    self.ffn(l, 0)
                elif what == "b":
                    self.ffn(l, 1)
                elif what == "m" and l % 3 == 0:
                    self.swa(l)
                elif what == "m" and l % 3 == 1:
                    self.gla(l)
                elif what == "m" and l % 3 == 2:
                    self.rwkv(l)
            outs = []
            for tt in range(NTT):
                outs.append(P.op("sp", lambda e, tt=tt, half=half: e.dma_start(out=yv[:, half * NTT + tt, :], in_=self.H[:, tt, :]),
                                 r=[P.b("H", tt)], dma=True))
        fin = P.op("sp", lambda e: e.nop(), r=[])
        for lst in (P.ndma["sp"][-Prog.NDMA:],):
            for o in lst:
                fin.deps.append(o)
        P.emit()
        return nc


ALL_STAGES = [(l, w) for l in range(DEPTH) for w in ("a", "m", "b")]


def host_layout(inputs):
    f = np.float32
    win = inputs["ffn_w_in"]
    L = win.shape[0]
    w = win.reshape(L, 2, 8, 128, 2, NSLAB, 2, 128).transpose(0, 1, 5, 3, 2, 6, 4, 7)
    ffn_win = np.ascontiguousarray(w).reshape(L, 2, NSLAB, 128, 8 * 512).astype(f, copy=False)
    wout = inputs["ffn_w_out"]
    w = wout.reshape(L, 2, NSLAB, 2, 128, D).transpose(0, 1, 2, 4, 3, 5)
    ffn_wout = np.ascontiguousarray(w).reshape(L, 2, NSLAB, 128, 2 * 1024).astype(f, copy=False)
    rows = [inputs["norm_g"].reshape(DEPTH * 6, D)]
    cols = []
    lay = {}

    def blk(wm):
        n = wm.shape[1] // 512
        return np.ascontiguousarray(wm.reshape(8, 128, n, 512).transpose(2, 1, 0, 3)).reshape(n, 128, 4096)

    def pad_row(v):
        r = np.zeros((1, D), f)
        r[0, :v.size] = v.reshape(-1)
        return r

    def col(v):
        return np.ascontiguousarray(v.reshape(-1, 128).T)

    lay["row_swa"] = sum(r.shape[0] for r in rows)
    lay["col_swa"] = sum(c.shape[1] for c in cols)
    wq, wk, wv, wo = [], [], [], []
    for j in range(2):
        wqkv = inputs["swa_w_qkv"][j]
        b = inputs["swa_b_qkv"][j]
        wq.append(blk(wqkv[:, 0:1024]))
        kd = wqkv[:, 1024:1280].reshape(1024, 4, 1, 64).repeat(2, axis=2).reshape(1024, 512)
        wk.append(blk(kd)[0])
        vd = np.concatenate([wqkv[:, 1280:1536], np.zeros((1024, 256), f)], axis=1)
        wv.append(blk(vd)[0])
        wo.append(blk(inputs["swa_w_o"][j]))
        rows += [pad_row(b[1280:1536]), pad_row(inputs["swa_b_o"][j]), pad_row(inputs["swa_sinks"][j])]
        bkd = b[1024:1280].reshape(4, 1, 64).repeat(2, axis=1).reshape(512)
        cols += [col(b[0:1024]), col(bkd)]
    out = {"swa_wq": np.stack(wq), "swa_wk": np.stack(wk), "swa_wv": np.stack(wv), "swa_wo": np.stack(wo)}
    lay["row_gla"] = sum(r.shape[0] for r in rows)
    lay["col_gla"] = sum(c.shape[1] for c in cols)
    gw = np.concatenate([inputs["gla_w_in"][0], np.zeros((1024, 7 * 512 - 3088), f)], axis=1)
    out["gla_win"] = blk(gw)
    out["gla_wo"] = blk(inputs["gla_w_o"][0])
    out["gla_wg2"] = np.ascontiguousarray(inputs["gla_w_gate2"][0]).astype(f, copy=False)
    rows += [np.tile(inputs["gla_norm_g"][0], 4)[None, :]]
    cols += [col(inputs["gla_b_gate"][0])]
    lay["row_rwkv"] = sum(r.shape[0] for r in rows)
    lay["col_rwkv"] = sum(c.shape[1] for c in cols)
    l1 = np.concatenate([inputs["rwkv_w1"][0], inputs["rwkv_a1"][0], inputs["rwkv_g1"][0],
                         np.zeros((1024, 512 - 288), f)], axis=1)
    out["rwkv_wl1"] = blk(l1)
    out["rwkv_wr"] = blk(inputs["rwkv_w_rkv"][0, 0])
    out["rwkv_wk"] = blk(inputs["rwkv_w_rkv"][0, 1])
    out["rwkv_wv"] = blk(inputs["rwkv_w_rkv"][0, 2])
    out["rwkv_wo"] = blk(inputs["rwkv_w_o"][0])
    out["rwkv_w2"] = np.ascontiguousarray(inputs["rwkv_w2"][0])
    out["rwkv_a2"] = np.ascontiguousarray(inputs["rwkv_a2"][0])
    out["rwkv_g2"] = np.ascontiguousarray(inputs["rwkv_g2"][0])
    rows += [inputs["rwkv_lnx_g"][0][None, :], inputs["rwkv_lnx_b"][0][None, :]]
    cols += [col(inputs["rwkv_mu"][0].reshape(-1)), col(inputs["rwkv_w0"][0]), col(inputs["rwkv_a0"][0]),
             col(inputs["rwkv_k_k"][0]), col(inputs["rwkv_k_a"][0]), col(inputs["rwkv_r_k"][0].reshape(-1))]
    pp = np.arange(128)[:, None]
    ff = np.arange(128)[None, :]
    p6 = pp % 64
    f6 = np.arange(64)[None, :]
    same = (pp // 64 == ff // 64)
    rc = np.concatenate([same & (pp < ff), same & (pp <= ff), same & (pp > ff), same,
                         (pp // 64 == np.arange(2)[None, :]), (p6 == f6)], axis=1)
    out["rconst"] = rc.astype(np.float32).astype(ml_dtypes.bfloat16)
    rows = np.ascontiguousarray(np.concatenate(rows, axis=0)).astype(f, copy=False)
    cols = np.ascontiguousarray(np.concatenate(cols, axis=1)).astype(f, copy=False)
    idb = np.eye(128, dtype=np.float32).astype(ml_dtypes.bfloat16)
    jj = np.arange(128)[:, None]
    ii = np.arange(128)[None, :]
    cm = np.concatenate([np.tile((jj <= ii), (1, 4)), np.tile((jj > ii), (1, 4))], axis=1)
    cmask = cm.astype(np.float32).astype(ml_dtypes.bfloat16)
    out.update({"rows": rows, "cols": cols, "ffn_win": ffn_win, "ffn_wout": ffn_wout, "idb": idb, "cmask": cmask})
    for k in list(out):
        if out[k].dtype == np.float64:
            out[k] = out[k].astype(f)
    return out, lay


_CACHE = {}


def run(inputs, stages, ncores=8, trace=False):
    shared, lay = host_layout(inputs)
    cfg = {"stages": stages, "nrows": shared["rows"].shape[0], "ncols": shared["cols"].shape[1]}
    cfg.update(lay)
    kk = K(cfg)
    with kk.stack:
        nc = kk.build()
    x = np.ascontiguousarray(inputs["x"]).astype(np.float32, copy=False)
    in_maps = []
    for c in range(ncores):
        m = {"x": x[c]}
        for k in kk.dram:
            if k != "x":
                m[k] = shared[k]
        in_maps.append(m)
    res = run_bass_kernel_spmd(nc, in_maps, core_ids=list(range(ncores)), trace=trace)
    out = np.stack([np.asarray(r["y"]) for r in res.results], axis=0)
    return out.astype(np.float32, copy=False), res


def kernel(**inputs):
    out, _ = run(inputs, ALL_STAGES)
    return out
```

```python
import contextlib
import numpy as np
import ml_dtypes
import concourse.bass as bass
import concourse.mybir as mybir
from concourse.bass_utils import run_bass_kernel_spmd

F32 = mybir.dt.float32
BF16 = mybir.dt.bfloat16
AF = mybir.ActivationFunctionType
ALU = mybir.AluOpType

D = 1024
SEQ = 2048
DEPTH = 4
DFF = 2816
NFC = DFF // 128
NSLAB = NFC // 2
TB = 1024
NTT = TB // 128
EPS = 1e-6


class Buf:
    __slots__ = ("w", "rs")

    def __init__(self):
        self.w = None
        self.rs = []


class Inst:
    __slots__ = ("eng", "fn", "deps", "sig", "dma", "sem", "val", "prev_dma")

    def __init__(self, eng, fn, dma):
        self.eng = eng
        self.fn = fn
        self.deps = []
        self.sig = False
        self.dma = dma
        self.sem = None
        self.val = None
        self.prev_dma = None


class _Rec:
    def __init__(self):
        self.call = None

    def __getattr__(self, name):
        def f(*a, **k):
            self.call = (name, a, k)
            return self
        return f


class Prog:
    ENGS = ("pe", "act", "dve", "pool", "sp")
    NDMA = 8

    def __init__(self, nc, stack):
        self.nc = nc
        self.q = {e: [] for e in self.ENGS}
        self.esem = {e: stack.enter_context(nc.semaphore("s_" + e)) for e in self.ENGS}
        self.dsem = {e: [stack.enter_context(nc.semaphore("d_%s%d" % (e, i))) for i in range(self.NDMA)]
                     for e in ("act", "pool", "sp")}
        self.ndma = {e: [] for e in ("act", "pool", "sp")}
        self.bufs = {}

    def b(self, *key):
        if key == ("PO", 2):
            key = ("PT2",)
        bb = self.bufs.get(key)
        if bb is None:
            bb = self.bufs[key] = Buf()
        return bb

    def op(self, eng, fn, r=(), w=(), dma=False):
        rec = _Rec()
        fn(rec)
        inst = Inst(eng, rec.call, dma)

        def dep(o, war=False):
            if o is None or o is inst:
                return
            if not dma and not o.dma and o.eng == eng:
                if eng == "pe":
                    return
            if o not in inst.deps:
                inst.deps.append(o)
                o.sig = True

        for bb in r:
            dep(bb.w)
        for bb in w:
            dep(bb.w)
            for o in bb.rs:
                dep(o, war=True)
        for bb in r:
            bb.rs.append(inst)
        for bb in w:
            bb.w = inst
            bb.rs = []
        if dma:
            lst = self.ndma[eng]
            if len(lst) >= self.NDMA:
                inst.prev_dma = lst[len(lst) - self.NDMA]
            inst.sem = self.dsem[eng][len(lst) % self.NDMA]
            inst.val = 16 * (len(lst) // self.NDMA + 1)
            lst.append(inst)
        self.q[eng].append(inst)
        return inst

    def emit(self):
        nc = self.nc
        for e in self.ENGS:
            c = 0
            for inst in self.q[e]:
                if not inst.dma:
                    inst.sem = self.esem[e]
                    if inst.sig:
                        c += 1
                    inst.val = c if inst.sig else None
        engobj = {"pe": "tensor", "act": "scalar", "dve": "vector", "pool": "gpsimd", "sp": "sync"}

        def run(e, eng):
            known = {}
            for inst in self.q[e]:
                waits = {}
                ds = list(inst.deps)
                if inst.prev_dma is not None:
                    ds.append(inst.prev_dma)
                for o in ds:
                    k = id(o.sem)
                    if o.val > known.get(k, 0) and o.val > waits.get(k, (None, 0))[1]:
                        waits[k] = (o.sem, o.val)
                for k, (s, v) in waits.items():
                    eng.wait_ge(s, v)
                    known[k] = v
                name, a, k = inst.fn
                h = getattr(eng, name)(*a, **k)
                if inst.dma:
                    h.then_inc(inst.sem, 16)
                elif inst.sig:
                    h.then_inc(inst.sem, 1)

        with nc.Block() as block:
            for e in self.ENGS:
                if not self.q[e]:
                    continue
                getattr(block, engobj[e])(lambda eng, e=e: run(e, eng))


class K:
    def __init__(self, cfg):
        self.cfg = cfg
        nc = self.nc = bass.Bass("TRN2", target_bir_lowering=False)
        self.stack = contextlib.ExitStack()
        self.P = Prog(nc, self.stack)
        self.dram = {}
        self.gbi = 0
        self.pi = {}

    def din(self, name, shape, dt=F32):
        t = self.nc.dram_tensor(name, list(shape), dt, kind="ExternalInput")
        self.dram[name] = t
        return t

    def sb(self, name, shape, dt):
        return self.stack.enter_context(self.nc.sbuf_tensor(name, list(shape), dt))

    def ps(self, name, shape, dt):
        return self.stack.enter_context(self.nc.psum_tensor(name, list(shape), dt))

    def rot(self, key, n):
        i = self.pi.get(key, 0)
        self.pi[key] = i + 1
        return i % n

    def load_row_bc(self, row):
        P = self.P
        i = self.rot("gb", 2)
        src = self.dram["rows"].ap()[row:row + 1, :].partition_broadcast(128)
        P.op("sp", lambda e: e.dma_start(out=self.GB[i][:, :], in_=src), w=[P.b("GB", i)], dma=True)
        return i

    def rstd_batch(self, ss, n, scale, eps, half=False, key="rs"):
        P = self.P
        bs = P.b(key)
        P.op("act", lambda e: e.activation(out=ss[:, :n], in_=ss[:, :n], func=AF.Sqrt,
                                           bias=self.epsc[eps][:, 0:1], scale=scale), r=[bs, P.b("epsc")], w=[bs])
        P.op("dve", lambda e: e.reciprocal(out=ss[:, :n], in_=ss[:, :n]), r=[bs], w=[bs])
        if half:
            P.op("dve", lambda e: e.tensor_scalar(out=ss[:, :n], in0=ss[:, :n], scalar1=0.5, scalar2=None,
                                                  op0=ALU.mult), r=[bs], w=[bs])

    def _rstd_tile(self, ss, col, scale, eps, key):
        P = self.P
        P.op("act", lambda e: e.activation(out=ss[:, col:col + 1], in_=ss[:, col:col + 1], func=AF.Sqrt,
                                           bias=self.epsc[eps][:, 0:1], scale=scale), r=[key, P.b("epsc")], w=[key])
        P.op("dve", lambda e: e.reciprocal(out=ss[:, col:col + 1], in_=ss[:, col:col + 1]), r=[key], w=[key])

    def _norm_stages(self, gi, tiles=range(NTT)):
        P = self.P
        ss = self.SS
        for tt in tiles:
            key = P.b("rsn", tt)
            P.op("act", lambda e: e.activation(out=self.JUNK[:, :], in_=self.H[:, tt, :], func=AF.Square,
                                               accum_out=ss[:, tt:tt + 1]), r=[P.b("H", tt)], w=[key])
            self._rstd_tile(ss, tt, 1.0 / D, EPS, key)
        for tt in tiles:
            key = P.b("rsn", tt)
            xi = self.rot("xs", 2)
            P.op("dve", lambda e: e.scalar_tensor_tensor(
                out=self.XS[xi][:, :], in0=self.H[:, tt, :], scalar=ss[:, tt:tt + 1], in1=self.GB[gi][:, :],
                op0=ALU.mult, op1=ALU.mult), r=[P.b("H", tt), key, P.b("GB", gi)], w=[P.b("XS", xi)])
            self.transpose_to_XT(self.XS[xi], P.b("XS", xi), tt)

    def norm_T(self, grow):
        if self.norm_done == grow:
            self.norm_done = None
            return
        gi = self.load_row_bc(grow)
        self._norm_stages(gi)

    def post_norm(self, grow, half, src_key="ACC", bias_row=None):
        P = self.P
        gi = self.load_row_bc(grow)
        nrow = self.next_norm_row
        gn = self.load_row_bc(nrow) if nrow is not None else None
        ss = self.SS
        sc, ep = ((4.0 / D, 4 * EPS) if half else (1.0 / D, EPS))
        for tt in range(NTT):
            key = P.b("rsp", tt)
            P.op("act", lambda e: e.activation(out=self.JUNK[:, :], in_=self.ACC[:, tt, :], func=AF.Square,
                                               accum_out=ss[:, 8 + tt:9 + tt]),
                 r=[P.b("ACC", tt, 0), P.b("ACC", tt, 1)], w=[key])
            self._rstd_tile(ss, 8 + tt, sc, ep, key)
        for tt in range(NTT):
            key = P.b("rsp", tt)
            P.op("dve", lambda e: e.scalar_tensor_tensor(
                out=self.ACC[:, tt, :], in0=self.ACC[:, tt, :], scalar=ss[:, 8 + tt:9 + tt], in1=self.GB[gi][:, :],
                op0=ALU.mult, op1=ALU.mult),
                r=[P.b("ACC", tt, 0), P.b("ACC", tt, 1), key, P.b("GB", gi)],
                w=[P.b("ACC", tt, 0), P.b("ACC", tt, 1)])
            P.op("dve" if tt % 2 == 0 else "pool", lambda e: e.tensor_tensor(
                out=self.H[:, tt, :], in0=self.H[:, tt, :], in1=self.ACC[:, tt, :], op=ALU.add),
                r=[P.b("H", tt), P.b("ACC", tt, 0), P.b("ACC", tt, 1)], w=[P.b("H", tt)])
        if gn is not None:
            self._norm_stages(gn)
            self.norm_done = nrow

    def ffn(self, l, a):
        P = self.P
        self.norm_T(l * 6 + (0 if a == 0 else 4))
        win_d = self.dram["ffn_win"].ap()
        wout_d = self.dram["ffn_wout"].ap()

        def load(s):
            wb = self.rot("wslab", 2)
            P.op("pool", lambda e: e.dma_start(out=self.WIN[wb][:, :], in_=win_d[l, a, s, :, :]),
                 w=[P.b("WIN", wb)], dma=True)
            P.op("pool", lambda e: e.dma_start(out=self.WOUT[wb][:, :], in_=wout_d[l, a, s, :, :]),
                 w=[P.b("WOUT", wb)], dma=True)
            return wb

        def phase1(s, wb):
            for tb in range(2):
                for fcl in range(2):
                    gi = self.rot("pgu", 2)
                    for gu, ps in ((0, self.PG[gi]), (1, self.PU[gi])):
                        for kc in range(8):
                            c0 = kc * 512 + fcl * 256 + gu * 128
                            P.op("pe", lambda e, ps=ps, c0=c0, kc=kc, tb=tb: e.matmul(
                                ps[:, :], lhsT=self.WIN[wb][:, c0:c0 + 128],
                                rhs=self.XT[:, kc, tb * 512:(tb + 1) * 512], start=(kc == 0), stop=(kc == 7)),
                                r=[P.b("WIN", wb)] + [P.b("XT", tb * 4 + i) for i in range(4)],
                                w=[P.b("PG" if gu == 0 else "PU", gi)])
                    P.op("act", lambda e, gi=gi: e.activation(out=self.SG[gi][:, :], in_=self.PG[gi][:, :],
                                                              func=AF.Silu),
                         r=[P.b("PG", gi)], w=[P.b("SG", gi)])
                    P.op("dve", lambda e, gi=gi, fcl=fcl, tb=tb: e.tensor_tensor(
                        out=self.ATS[wb][:, fcl, tb * 512:(tb + 1) * 512], in0=self.SG[gi][:, :],
                        in1=self.PU[gi][:, :], op=ALU.mult),
                        r=[P.b("SG", gi), P.b("PU", gi)], w=[P.b("ATS", wb, fcl, tb)])

        def phase2(slabs, first):
            nmm = 2 * len(slabs)
            for tt in range(NTT):
                for nh in range(2):
                    oi = self.rot("po", 2)
                    i = 0
                    for (s, wb) in slabs:
                        for fcl in range(2):
                            P.op("pe", lambda e: e.matmul(
                                self.PO[oi][:, :], lhsT=self.ATS[wb][:, fcl, tt * 128:(tt + 1) * 128],
                                rhs=self.WOUT[wb][:, fcl * 1024 + nh * 512: fcl * 1024 + (nh + 1) * 512],
                                start=(i == 0), stop=(i == nmm - 1)),
                                r=[P.b("ATS", wb, fcl, tt // 4), P.b("WOUT", wb)], w=[P.b("PO", oi)])
                            i += 1
                    if first:
                        P.op("act", lambda e: e.activation(
                            out=self.ACC[:, tt, nh * 512:(nh + 1) * 512], in_=self.PO[oi][:, :], func=AF.Copy),
                            r=[P.b("PO", oi)], w=[P.b("ACC", tt, nh)])
                    else:
                        P.op("dve", lambda e: e.tensor_tensor(
                            out=self.ACC[:, tt, nh * 512:(nh + 1) * 512], in0=self.ACC[:, tt, nh * 512:(nh + 1) * 512],
                            in1=self.PO[oi][:, :], op=ALU.add),
                            r=[P.b("PO", oi), P.b("ACC", tt, nh)], w=[P.b("ACC", tt, nh)])

        s = 0
        while s < NSLAB:
            pair = []
            for ss_ in range(s, min(s + 2, NSLAB)):
                wb = load(ss_)
                phase1(ss_, wb)
                pair.append((ss_, wb))
            phase2(pair, first=(s == 0))
            s += 2
        self.post_norm(l * 6 + (1 if a == 0 else 5), half=True)


    def load_w(self, name, *idx):
        P = self.P
        wb = self.rot("wslab", 2)
        src = self.dram[name].ap()
        for i in idx:
            src = src[i]
        P.op("pool", lambda e: e.dma_start(out=self.WIN[wb][:, :], in_=src), w=[P.b("WIN", wb)], dma=True)
        return wb

    def proj_fm(self, wb, c0, M, tb, ps, pkey):
        P = self.P
        for kc in range(8):
            P.op("pe", lambda e, kc=kc: e.matmul(
                ps[:M, :], lhsT=self.WIN[wb][:, kc * 512 + c0: kc * 512 + c0 + M],
                rhs=self.XT[:, kc, tb * 512:(tb + 1) * 512], start=(kc == 0), stop=(kc == 7)),
                r=[P.b("WIN", wb)] + [P.b("XT", tb * 4 + i) for i in range(4)], w=[pkey])

    def proj_tm(self, wb, c0, N, tt, ps, pkey, xt=None, xkey="XT"):
        P = self.P
        xt = self.XT if xt is None else xt
        for kc in range(8):
            P.op("pe", lambda e, kc=kc: e.matmul(
                ps[:, :N], lhsT=xt[:, kc, tt * 128:(tt + 1) * 128],
                rhs=self.WIN[wb][:, kc * 512 + c0: kc * 512 + c0 + N], start=(kc == 0), stop=(kc == 7)),
                r=[P.b("WIN", wb), P.b(xkey, tt)], w=[pkey])

    def transpose_to_XT(self, src, skey, tt):
        P = self.P
        for kc in range(8):
            P.op("pe", lambda e, kc=kc: e.transpose(
                out=self.PT[:, kc * 128:(kc + 1) * 128], in_=src[:, kc * 128:(kc + 1) * 128],
                identity=self.IDB[:, :]), r=[skey, P.b("IDB")], w=[P.b("PT")])
        P.op("act", lambda e: e.activation(
            out=self.XT[:, :, tt * 128:(tt + 1) * 128],
            in_=self.PT[:, :].rearrange("p (k c) -> p k c", k=8), func=AF.Copy),
            r=[P.b("PT")], w=[P.b("XT", tt)])

    def out_proj(self, wname, widx, brow):
        P = self.P
        wbs = [self.load_w(wname, *widx, nh) for nh in range(2)]
        gi = self.load_row_bc(brow) if brow is not None else None
        for tt in range(NTT):
            for nh in range(2):
                oi = self.rot("po", 2)
                self.proj_tm(wbs[nh], 0, 512, tt, self.PO[oi], P.b("PO", oi))
                if gi is None:
                    P.op("act", lambda e, oi=oi, tt=tt, nh=nh: e.activation(
                        out=self.ACC[:, tt, nh * 512:(nh + 1) * 512], in_=self.PO[oi][:, :], func=AF.Copy),
                        r=[P.b("PO", oi)], w=[P.b("ACC", tt, nh)])
                else:
                    P.op("dve", lambda e, oi=oi, tt=tt, nh=nh: e.tensor_tensor(
                        out=self.ACC[:, tt, nh * 512:(nh + 1) * 512], in0=self.PO[oi][:, :],
                        in1=self.GB[gi][:, nh * 512:(nh + 1) * 512], op=ALU.add),
                        r=[P.b("PO", oi), P.b("GB", gi)], w=[P.b("ACC", tt, nh)])

    def swa(self, l):
        P = self.P
        j = l // 3
        half = self.half
        R0 = self.cfg["row_swa"] + j * 3
        C0 = self.cfg["col_swa"] + j * 12
        self.norm_T(l * 6 + 2)
        P.op("dve", lambda e: e.memset(self.VA[:, :, :], 1.0), w=[P.b("VA", i) for i in range(-1, 8)])
        gs = self.load_row_bc(R0 + 2)
        P.op("act", lambda e: e.activation(out=self.ESK[:, :], in_=self.GB[gs][:, 0:16], func=AF.Exp),
             r=[P.b("GB", gs)], w=[P.b("ESK")])
        P.op("dve", lambda e: e.tensor_scalar(out=self.BQ8[:, :], in0=self.COLS[:, C0:C0 + 8], scalar1=0.125,
                                              scalar2=None, op0=ALU.mult), r=[P.b("COLS")], w=[P.b("BQ8")])
        if half == 1:
            P.op("pool", lambda e: e.tensor_copy(out=self.KT[:, :, 0:128], in_=self.KC[j][:, :, :]),
                 r=[P.b("KC", j)], w=[P.b("KT", -1)])
            P.op("pool", lambda e: e.tensor_copy(out=self.VA[:, 0, :], in_=self.VC[j][:, :]),
                 r=[P.b("VC", j)], w=[P.b("VA", -1)])
        for blk in range(2):
            wb = self.load_w("swa_wq", j, blk)
            for ocl in range(4):
                oc = blk * 4 + ocl
                for tb in range(2):
                    gi = self.rot("pgu", 2)
                    self.proj_fm(wb, ocl * 128, 128, tb, self.PG[gi], P.b("PG", gi))
                    P.op("act", lambda e, gi=gi, oc=oc, tb=tb: e.activation(
                        out=self.QT[:, oc, tb * 512:(tb + 1) * 512], in_=self.PG[gi][:, :], func=AF.Identity,
                        bias=self.BQ8[:, oc:oc + 1], scale=0.125),
                        r=[P.b("PG", gi), P.b("BQ8")], w=[P.b("QT", oc, tb)])
        wb = self.load_w("swa_wk", j)
        for hk in range(4):
            for tb in range(2):
                gi = self.rot("pgu", 2)
                self.proj_fm(wb, hk * 128, 128, tb, self.PU[gi], P.b("PU", gi))
                P.op("act", lambda e, gi=gi, hk=hk, tb=tb: e.activation(
                    out=self.KT[:, hk, 128 + tb * 512: 128 + (tb + 1) * 512], in_=self.PU[gi][:, :], func=AF.Identity,
                    bias=self.COLS[:, C0 + 8 + hk:C0 + 9 + hk], scale=1.0),
                    r=[P.b("PU", gi), P.b("COLS")], w=[P.b("KT", tb * 4 + i) for i in range(4)])
        wb = self.load_w("swa_wv", j)
        gv = self.load_row_bc(R0 + 0)
        for tt in range(NTT):
            oi = self.rot("po", 2)
            self.proj_tm(wb, 0, 256, tt, self.PO[oi], P.b("PO", oi))
            P.op("dve", lambda e, oi=oi, tt=tt: e.tensor_tensor(
                out=self.VA[:, tt + 1, :].rearrange("p (h d) -> p h d", h=4)[:, :, 0:64],
                in0=self.PO[oi][:, 0:256].rearrange("p (h d) -> p h d", h=4),
                in1=self.GB[gv][:, 0:256].rearrange("p (h d) -> p h d", h=4), op=ALU.add),
                r=[P.b("PO", oi), P.b("GB", gv)], w=[P.b("VA", tt)])
        for n in range(NTT):
            has_prev = not (half == 0 and n == 0)
            xi = self.rot("xs", 2)
            for hk in range(4):
                srcs = [("cur", self.PG, "PG", 128 + n * 128, n)]
                if has_prev:
                    srcs.append(("prev", self.PU, "PU", n * 128, n - 1))
                pts = []
                for (kind, pss, pname, k0, kblk) in srcs:
                    si = self.rot("sg", 2)
                    for hp in range(2):
                        P.op("pe", lambda e: e.matmul(
                            pss[hp][:, 0:256], lhsT=self.KT[hp * 64:(hp + 1) * 64, hk, k0:k0 + 128],
                            rhs=self.QT[hp * 64:(hp + 1) * 64, 2 * hk:2 * hk + 2, n * 128:(n + 1) * 128],
                            start=True, stop=True),
                            r=[P.b("KT", kblk), P.b("QT", 2 * hk, n // 4), P.b("QT", 2 * hk + 1, n // 4)],
                            w=[P.b(pname, hp)])
                        P.op("act", lambda e: e.activation(out=self.SG[si][:, hp * 256:(hp + 1) * 256],
                                                           in_=pss[hp][:, 0:256], func=AF.Exp),
                             r=[P.b(pname, hp)], w=[P.b("SG", si)])
                    pi = self.rot("pts", 4)
                    mo = 0 if kind == "cur" else 512
                    P.op("dve", lambda e, si=si, pi=pi, mo=mo: e.tensor_tensor(
                        out=self.PTS[pi][:, :], in0=self.SG[si][:, :], in1=self.CMASK[:, mo:mo + 512], op=ALU.mult),
                        r=[P.b("SG", si), P.b("CMASK")], w=[P.b("PTS", pi)])
                    pts.append((pi, kblk))
                oi = self.rot("po", 2)
                for hh in range(4):
                    pos = (hh % 2) * 2 + hh // 2
                    for ii, (pi, kblk) in enumerate(pts):
                        P.op("pe", lambda e, oi=oi, hh=hh, pos=pos, pi=pi, kblk=kblk, ii=ii: e.matmul(
                            self.PO[oi][:, hh * 65:(hh + 1) * 65], lhsT=self.PTS[pi][:, pos * 128:(pos + 1) * 128],
                            rhs=self.VA[:, kblk + 1, hk * 65:(hk + 1) * 65], start=(ii == 0), stop=(ii == len(pts) - 1)),
                            r=[P.b("PTS", pi), P.b("VA", kblk)], w=[P.b("PO", oi)])
                ov = self.PO[oi][:, 0:260].rearrange("p (h d) -> p h d", h=4)
                P.op("dve", lambda e, ov=ov: e.tensor_tensor(
                    out=self.DEN[:, :], in0=ov[:, :, 64], in1=self.ESK[:, hk * 4:(hk + 1) * 4], op=ALU.add),
                    r=[P.b("PO", oi), P.b("ESK")], w=[P.b("DEN")])
                P.op("dve", lambda e: e.reciprocal(out=self.DEN[:, :], in_=self.DEN[:, :]),
                     r=[P.b("DEN")], w=[P.b("DEN")])
                P.op("dve", lambda e, ov=ov, xi=xi: e.tensor_tensor(
                    out=self.XS[xi][:, hk * 256:(hk + 1) * 256].rearrange("p (h d) -> p h d", h=4),
                    in0=ov[:, :, 0:64], in1=self.DEN[:, :].unsqueeze(2).to_broadcast([128, 4, 64]), op=ALU.mult),
                    r=[P.b("PO", oi), P.b("DEN")], w=[P.b("XS", xi)])
            self.transpose_to_XT(self.XS[xi], P.b("XS", xi), n)
        if half == 0:
            P.op("pool", lambda e: e.tensor_copy(out=self.KC[j][:, :, :], in_=self.KT[:, :, 1024:1152]),
                 r=[P.b("KT", 7)], w=[P.b("KC", j)])
            P.op("pool", lambda e: e.tensor_copy(out=self.VC[j][:, :], in_=self.VA[:, 8, :]),
                 r=[P.b("VA", 7)], w=[P.b("VC", j)])
        self.out_proj("swa_wo", (j,), R0 + 1)
        self.post_norm(l * 6 + 3, half=False)


    def gla(self, l):
        P = self.P
        half = self.half
        R0 = self.cfg["row_gla"]
        C0 = self.cfg["col_gla"]
        DK = 128
        LA = self.ATS[0][:, :, :].rearrange("p a b -> p (a b)").bitcast(F32)
        EB = self.ATS[1][:, :, :].rearrange("p a b -> p (a b)").bitcast(F32)
        ENB = self.WOUT[0][:, :].bitcast(F32)
        kLA, kEB, kENB = [P.b("ATS", 0, a, b) for a in range(2) for b in range(2)], \
            [P.b("ATS", 1, a, b) for a in range(2) for b in range(2)], [P.b("WOUT", 0)]
        ACCb = self.ACC[:, :, :].rearrange("p a b -> p (a b)").bitcast(BF16).rearrange("p (a b) -> p a b", a=NTT)
        self.norm_T(l * 6 + 2)
        if half == 0:
            P.op("dve", lambda e: e.memset(self.GS[:, :, :], 0.0), w=[P.b("GS", h) for h in range(4)])
            P.op("dve", lambda e: e.memset(self.GSB[:, :, :], 0.0), w=[P.b("GSB", h) for h in range(4)])
        P.op("dve", lambda e: e.tensor_scalar(out=self.NBG[:, :], in0=self.COLS[:, C0:C0 + 4], scalar1=-1.0,
                                              scalar2=None, op0=ALU.mult), r=[P.b("COLS")], w=[P.b("NBG")])
        P.op("pool", lambda e: e.dma_start(out=self.SG[0][0:16, :], in_=self.dram["gla_wg2"].ap()[:, :]),
             w=[P.b("SG", 0)], dma=True)
        wb = self.load_w("gla_win", 6)
        for tb in range(2):
            gi = self.rot("pgu", 2)
            self.proj_fm(wb, 0, 16, tb, self.PG[gi], P.b("PG", gi))
            P.op("act", lambda e: e.activation(out=self.XS[0][0:16, tb * 512:(tb + 1) * 512], in_=self.PG[gi][0:16, :],
                                               func=AF.Copy), r=[P.b("PG", gi)], w=[P.b("XS", 0)])
        wq = self.load_w("gla_win", 0)
        wk = self.load_w("gla_win", 1)
        for h in range(4):
            for tb in range(2):
                gi = self.rot("pgu", 2)
                P.op("pe", lambda e: e.matmul(self.PU[gi][:, :], lhsT=self.SG[0][0:16, h * 128:(h + 1) * 128],
                                              rhs=self.XS[0][0:16, tb * 512:(tb + 1) * 512], start=True, stop=True),
                     r=[P.b("SG", 0), P.b("XS", 0)], w=[P.b("PU", gi)])
                P.op("act", lambda e: e.activation(out=EB[:, tb * 512:(tb + 1) * 512], in_=self.PU[gi][:, :], func=AF.Exp,
                                                   bias=self.NBG[:, h:h + 1], scale=-1.0),
                     r=[P.b("PU", gi), P.b("NBG")], w=kEB)
                P.op("act", lambda e: e.activation(out=LA[:, tb * 512:(tb + 1) * 512], in_=EB[:, tb * 512:(tb + 1) * 512],
                                                   func=AF.Ln, bias=self.ONE1[:, 0:1], scale=1.0), r=kEB + [P.b("ONES")], w=kLA)
            for c in range(8):
                P.op("dve", lambda e: e.tensor_tensor_scan(
                    out=LA[:, c * 128:(c + 1) * 128], data0=self.ONES[:, :], data1=LA[:, c * 128:(c + 1) * 128],
                    initial=0.0, op0=ALU.mult, op1=ALU.add), r=kLA + [P.b("ONES")], w=kLA)
            P.op("act", lambda e: e.activation(out=EB[:, :], in_=LA[:, :], func=AF.Exp, scale=-1.0 / 16.0), r=kLA, w=kEB)
            P.op("act", lambda e: e.activation(out=ENB[:, :], in_=LA[:, :], func=AF.Exp, scale=1.0 / 16.0), r=kLA, w=kENB)
            P.op("dve", lambda e: e.tensor_copy(out=self.EBLS[:, h, :],
                                                in_=EB.rearrange("p (c t) -> p c t", c=8)[:, :, 127]),
                 r=kEB, w=[P.b("EBLS", h)])
            for tb in range(2):
                gi = self.rot("pgu", 2)
                self.proj_fm(wq, h * 128, 128, tb, self.PG[gi], P.b("PG", gi))
                P.op("dve", lambda e: e.scalar_tensor_tensor(
                    out=self.QT[:, h, tb * 512:(tb + 1) * 512], in0=self.PG[gi][:, :], scalar=DK ** -0.5,
                    in1=EB[:, tb * 512:(tb + 1) * 512], op0=ALU.mult, op1=ALU.mult),
                    r=[P.b("PG", gi)] + kEB, w=[P.b("QT", h, tb)])
                self.proj_fm(wk, h * 128, 128, tb, self.PU[gi], P.b("PU", gi))
                P.op("dve", lambda e: e.tensor_tensor(
                    out=self.QT[:, 4 + h, tb * 512:(tb + 1) * 512], in0=self.PU[gi][:, :],
                    in1=ENB[:, tb * 512:(tb + 1) * 512], op=ALU.mult),
                    r=[P.b("PU", gi)] + kENB, w=[P.b("QT", 4 + h, tb)])
            P.op("dve", lambda e: e.tensor_tensor(
                out=self.KT[:, h, 0:1024].rearrange("p (c t) -> p c t", c=8),
                in0=self.QT[:, 4 + h, :].rearrange("p (c t) -> p c t", c=8),
                in1=self.EBLS[:, h, :].unsqueeze(2).to_broadcast([128, 8, 128]), op=ALU.mult),
                r=[P.b("QT", 4 + h, 0), P.b("QT", 4 + h, 1), P.b("EBLS", h)], w=[P.b("KT", i) for i in range(-1, 8)])
        for blk in range(2):
            wb = self.load_w("gla_win", 2 + blk)
            for tt in range(NTT):
                oi = self.rot("po", 2)
                self.proj_tm(wb, 0, 512, tt, self.PO[oi], P.b("PO", oi))
                P.op("act", lambda e: e.activation(out=ACCb[:, tt, blk * 512:(blk + 1) * 512], in_=self.PO[oi][:, :],
                                                   func=AF.Copy), r=[P.b("PO", oi)], w=[P.b("ACC", tt, 0)])
        gn = self.load_row_bc(R0 + 0)
        for blk in range(2):
            wb = self.load_w("gla_win", 4 + blk)
            for tt in range(NTT):
                oi = self.rot("po", 2)
                self.proj_tm(wb, 0, 512, tt, self.PO[oi], P.b("PO", oi))
                si = self.rot("sg", 2)
                P.op("act", lambda e: e.activation(out=self.SG[si][:, :], in_=self.PO[oi][:, :], func=AF.Silu),
                     r=[P.b("PO", oi)], w=[P.b("SG", si)])
                P.op("dve", lambda e: e.tensor_tensor(
                    out=ACCb[:, tt, 1024 + blk * 512:1024 + (blk + 1) * 512], in0=self.SG[si][:, :],
                    in1=self.GB[gn][:, blk * 512:(blk + 1) * 512], op=ALU.mult),
                    r=[P.b("SG", si), P.b("GB", gn)], w=[P.b("ACC", tt, 1)])
        for c in range(NTT):
            cs = slice(c * 128, (c + 1) * 128)
            for h in range(4):
                gi = self.rot("pgu", 2)
                P.op("pe", lambda e: e.matmul(self.PG[gi][:, 0:128], lhsT=self.QT[:, 4 + h, cs], rhs=self.QT[:, h, cs],
                                              start=True, stop=True),
                     r=[P.b("QT", 4 + h, c // 4), P.b("QT", h, c // 4)], w=[P.b("PG", gi)])
                pa = self.rot("pts", 4)
                P.op("dve", lambda e: e.tensor_tensor(out=self.PTS[pa][:, 0:128], in0=self.PG[gi][:, 0:128],
                                                      in1=self.CMASK[:, 0:128], op=ALU.mult),
                     r=[P.b("PG", gi), P.b("CMASK")], w=[P.b("PTS", pa)])
                oi = self.rot("po", 2)
                P.op("pe", lambda e: e.matmul(self.PO[oi][:, 0:256], lhsT=self.PTS[pa][:, 0:128],
                                              rhs=ACCb[:, c, h * 256:(h + 1) * 256], start=True, stop=False),
                     r=[P.b("PTS", pa), P.b("ACC", c, 0)], w=[P.b("PO", oi)])
                P.op("pe", lambda e: e.matmul(self.PO[oi][:, 0:256], lhsT=self.QT[:, h, cs], rhs=self.GSB[:, h, :],
                                              start=False, stop=True),
                     r=[P.b("QT", h, c // 4), P.b("GSB", h)], w=[P.b("PO", oi)])
                P.op("pe", lambda e: e.transpose(out=self.PT[:, 0:128], in_=self.KT[:, h, cs], identity=self.IDB[:, :]),
                     r=[P.b("KT", c), P.b("IDB")], w=[P.b("PT")])
                pk = self.rot("pts", 4)
                P.op("act", lambda e: e.activation(out=self.PTS[pk][:, 0:128], in_=self.PT[:, 0:128], func=AF.Copy),
                     r=[P.b("PT")], w=[P.b("PTS", pk)])
                P.op("pe", lambda e: e.matmul(self.PU[gi][:, 0:256], lhsT=self.PTS[pk][:, 0:128],
                                              rhs=ACCb[:, c, h * 256:(h + 1) * 256], start=True, stop=True),
                     r=[P.b("PTS", pk), P.b("ACC", c, 0)], w=[P.b("PU", gi)])
                P.op("dve", lambda e: e.scalar_tensor_tensor(
                    out=self.GS[:, h, :], in0=self.GS[:, h, :], scalar=self.EBLS[:, h, c:c + 1], in1=self.PU[gi][:, 0:256],
                    op0=ALU.mult, op1=ALU.add), r=[P.b("GS", h), P.b("EBLS", h), P.b("PU", gi)], w=[P.b("GS", h)])
                P.op("act", lambda e: e.activation(out=self.GSB[:, h, :], in_=self.GS[:, h, :], func=AF.Copy),
                     r=[P.b("GS", h)], w=[P.b("GSB", h)])
                P.op("act", lambda e: e.activation(out=self.JUNK[:, 0:256], in_=self.PO[oi][:, 0:256], func=AF.Square,
                                                   accum_out=self.SSG[:, 0:1]), r=[P.b("PO", oi)], w=[P.b("SSG")])
                self.rstd_batch(self.SSG, 1, 1.0 / 256.0, 1e-5, key="SSG")
                P.op("dve", lambda e: e.scalar_tensor_tensor(
                    out=ACCb[:, c, 1024 + h * 256:1024 + (h + 1) * 256], in0=self.PO[oi][:, 0:256],
                    scalar=self.SSG[:, 0:1], in1=ACCb[:, c, 1024 + h * 256:1024 + (h + 1) * 256],
                    op0=ALU.mult, op1=ALU.mult), r=[P.b("PO", oi), P.b("SSG"), P.b("ACC", c, 1)], w=[P.b("ACC", c, 1)])
        for tt in range(NTT):
            self.transpose_to_XT(ACCb[:, tt, 1024:2048], P.b("ACC", tt, 1), tt)
        self.out_proj("gla_wo", (), None)
        self.post_norm(l * 6 + 3, half=False)


    def proj_mix_fm(self, wa, wb_, c0, M, tb, ps, pkey):
        P = self.P
        n = 0
        for (wbuf, xt) in ((wa, self.XT), (wb_, self.XTs)):
            for kc in range(8):
                P.op("pe", lambda e: e.matmul(
                    ps[:M, :], lhsT=self.WIN[wbuf][:, kc * 512 + c0: kc * 512 + c0 + M],
                    rhs=xt[:, kc, tb * 512:(tb + 1) * 512], start=(n == 0), stop=(n == 15)),
                    r=[P.b("WIN", wbuf), P.b("XTC")] + [P.b("XT", tb * 4 + i) for i in range(max(0, -1), 4)]
                    + ([P.b("XT", tb * 4 - 1)] if tb > 0 else []), w=[pkey])
                n += 1

    def load_mix(self, name, blk, mu0, ranges):
        P = self.P
        src = self.dram[name].ap()[blk]
        P.op("pool", lambda e: e.dma_start(out=self.WIN[0][:, :], in_=src), w=[P.b("WIN", 0)], dma=True)
        for (c0, c1, mi) in ranges:
            for kc in range(8):
                P.op("act", lambda e: e.activation(out=self.WIN[1][:, kc * 512 + c0: kc * 512 + c1],
                                                   in_=self.WIN[0][:, kc * 512 + c0: kc * 512 + c1], func=AF.Copy,
                                                   scale=self.COLS[:, mu0 + mi * 8 + kc: mu0 + mi * 8 + kc + 1]),
                     r=[P.b("WIN", 0), P.b("COLS")], w=[P.b("WIN", 1)])
            for kc in range(8):
                P.op("act", lambda e: e.activation(out=self.WIN[0][:, kc * 512 + c0: kc * 512 + c1],
                                                   in_=self.WIN[0][:, kc * 512 + c0: kc * 512 + c1], func=AF.Copy,
                                                   scale=self.OMMU[:, mi * 8 + kc: mi * 8 + kc + 1]),
                     r=[P.b("WIN", 0), P.b("OMMU")], w=[P.b("WIN", 0)])

    def rwkv(self, l):
        P = self.P
        half = self.half
        R0 = self.cfg["row_rwkv"]
        C0 = self.cfg["col_rwkv"]
        CMU, CW0, CA0, CKK, CKA, CRK = C0, C0 + 48, C0 + 56, C0 + 64, C0 + 72, C0 + 80
        c0e = float(np.exp(-0.5))
        f32v = lambda t: t.rearrange("p a b -> p (a b)").bitcast(F32) if len(t.shape) == 3 else t.bitcast(F32)
        T0 = f32v(self.ATS[0][:, :, :]); k0 = [P.b("ATS", 0, a, b) for a in range(2) for b in range(2)]
        T1 = f32v(self.ATS[1][:, :, :]); k1 = [P.b("ATS", 1, a, b) for a in range(2) for b in range(2)]
        T2 = f32v(self.WOUT[0][:, :]); k2 = [P.b("WOUT", 0)]
        T3 = f32v(self.WOUT[1][:, :]); k3 = [P.b("WOUT", 1)]
        KTf = f32v(self.KT[:, :, :])
        T4 = KTf[:, 0:1024]; k4 = [P.b("KT", i) for i in range(-1, 8)]
        T5 = KTf[:, 1024:2048]; k5 = [P.b("KTb")]
        T6 = f32v(self.VA[:, :, :])[:, 0:1024]; k6 = [P.b("VA", i) for i in range(-1, 8)]
        ACCb = self.ACC[:, :, :].rearrange("p a b -> p (a b)").bitcast(BF16).rearrange("p (a b) -> p a b", a=NTT)
        AR = self.QT[:, 0:2, :].rearrange("p a (c t) -> p (a c t)", t=128).rearrange("p (c a t) -> p c a t", a=2, t=128)
        u128 = lambda ap: ap.rearrange("p (u t) -> p u t", t=128)
        u64 = lambda ap: ap.rearrange("p (u t) -> p u t", t=64)
        W0k = self.WIN[0][:, :].rearrange("p (k u t) -> p k u t", k=2, t=128)
        W1k = self.WIN[1][:, :].rearrange("p (k u t) -> p k u t", k=2, t=128)
        LAK, MRK, PN, MRB = W0k[:, 0], W0k[:, 1], W1k[:, 0], W1k[:, 1]
        PTN = u128(self.ATS[0][:, :, :].rearrange("p a b -> p (a b)"))
        XX = u128(self.ATS[1][:, :, :].rearrange("p a b -> p (a b)"))
        ATOK, BHT = u64(self.WOUT[0][:, 0:1024]), u64(self.WOUT[0][:, 1024:2048])
        KHT, W0s = u64(self.WOUT[1][:, 0:1024]), u64(self.WOUT[1][:, 1024:2048])
        KTb = self.KT[:, :, :].rearrange("p a b -> p (a b)")
        U0s, AHs, MTB = u64(KTb[:, 0:1024]), u64(KTb[:, 1024:2048]), u128(KTb[:, 2048:4096])
        VAb = self.VA[:, :, :].rearrange("p a b -> p (a b)")
        DD, SALL = u64(VAb[:, 0:1024]), u64(VAb[:, 1024:1024 + 17 * 64])
        DG = u64(self.QT[:, 6, :])
        c3t = lambda t: t.rearrange("p (c t) -> p c t", t=128)
        kAR = [P.b("QT", 0, 0), P.b("QT", 0, 1), P.b("QT", 1, 0), P.b("QT", 1, 1)]
        KTL = self.QT[:, 2, :]; kKTL = [P.b("QT", 2, 0), P.b("QT", 2, 1)]
        BTL = self.QT[:, 3, :]; kBTL = [P.b("QT", 3, 0), P.b("QT", 3, 1)]
        KH = self.QT[:, 4, :]; kKH = [P.b("QT", 4, 0), P.b("QT", 4, 1)]
        BH = self.QT[:, 5, :]; kBH = [P.b("QT", 5, 0), P.b("QT", 5, 1)]
        PB = self.QT[:, 6, :]; kPB = [P.b("QT", 6, 0), P.b("QT", 6, 1)]
        TMPB = self.QT[:, 7, :]; kTMPB = [P.b("QT", 7, 0), P.b("QT", 7, 1)]
        c3 = lambda t: t.rearrange("p (c t) -> p c t", t=64)

        self.norm_T(l * 6 + 2)
        if half == 0:
            P.op("dve", lambda e: e.memset(self.XTfull[:, :, 7:8], 0.0), w=[P.b("XTC")])
            P.op("dve", lambda e: e.memset(self.STC[:, :, :], 0.0), w=[P.b("STC", i) for i in range(8)])

        else:
            P.op("dve", lambda e: e.tensor_copy(out=self.XTfull[:, :, 7:8], in_=self.XC[:, :].unsqueeze(2)),
                 r=[P.b("XC")], w=[P.b("XTC")])
        P.op("dve", lambda e: e.tensor_scalar(out=self.OMMU[:, :], in0=self.COLS[:, CMU:CMU + 48], scalar1=-1.0,
                                              scalar2=1.0, op0=ALU.mult, op1=ALU.add), r=[P.b("COLS")], w=[P.b("OMMU")])
        P.op("dve", lambda e: e.tensor_scalar(out=self.OMKA[:, :], in0=self.COLS[:, CKA:CKA + 8], scalar1=-1.0,
                                              scalar2=1.0, op0=ALU.mult, op1=ALU.add), r=[P.b("COLS")], w=[P.b("OMKA")])
        self.load_mix("rwkv_wl1", 0, CMU, [(0, 64, 1), (64, 128, 4), (128, 288, 5)])
        for tb in range(2):
            ts = slice(tb * 512, (tb + 1) * 512)
            for (c0_, M, fn, dst, dkey) in ((0, 64, AF.Tanh, self.LW1[0:64, ts], P.b("LW1", tb)),
                                           (64, 64, AF.Copy, self.LA1[0:64, ts], P.b("LA1", tb)),
                                           (128, 128, AF.Sigmoid, self.XS[0][:, ts], P.b("XS", 0)),
                                           (256, 32, AF.Sigmoid, self.XS[1][0:32, ts], P.b("XS", 1))):
                gi = self.rot("pgu", 2)
                self.proj_mix_fm(0, 1, c0_, M, tb, self.PG[gi], P.b("PG", gi))
                P.op("act", lambda e: e.activation(out=dst, in_=self.PG[gi][0:M, :], func=fn),
                     r=[P.b("PG", gi)], w=[dkey])
        P.op("pool", lambda e: e.dma_start(out=self.L2W[0:64, :], in_=self.dram["rwkv_w2"].ap()[:, :]), w=[P.b("L2W")], dma=True)
        P.op("pool", lambda e: e.dma_start(out=self.L2A[0:64, :], in_=self.dram["rwkv_a2"].ap()[:, :]), w=[P.b("L2A")], dma=True)
        for blk in range(2):
            self.load_mix("rwkv_wv", blk, CMU, [(0, 512, 3)])
            for tt in range(NTT):
                oi = self.rot("po", 2)
                n = 0
                for (wbuf, xt) in ((0, self.XT), (1, self.XTs)):
                    for kc in range(8):
                        P.op("pe", lambda e: e.matmul(
                            self.PO[oi][:, :], lhsT=xt[:, kc, tt * 128:(tt + 1) * 128],
                            rhs=self.WIN[wbuf][:, kc * 512:(kc + 1) * 512], start=(n == 0), stop=(n == 15)),
                            r=[P.b("WIN", wbuf), P.b("XT", tt), P.b("XTC")] + ([P.b("XT", tt - 1)] if tt > 0 else []),
                            w=[P.b("PO", oi)])
                        n += 1
                P.op("act", lambda e: e.activation(out=ACCb[:, tt, blk * 512:(blk + 1) * 512], in_=self.PO[oi][:, :],
                                                   func=AF.Copy), r=[P.b("PO", oi)], w=[P.b("ACC", tt, 0)])
        for kc in range(8):
            blk, cc = kc // 4, (kc % 4) * 128
            P.op("pool", lambda e: e.dma_start(out=self.WIN[0][:, 0:2048], in_=self.dram["rwkv_wrk"].ap()[kc]),
                 w=[P.b("WIN", 0)], dma=True)
            for m, mi in ((0, 0), (1, 2)):
                raw = self.WIN[0][:, m * 1024:(m + 1) * 1024].rearrange("p (k c) -> p k c", k=8)
                wbm = self.WIN[0][:, 2048 + m * 1024:2048 + (m + 1) * 1024].rearrange("p (k c) -> p k c", k=8)
                P.op("dve", lambda e: e.tensor_tensor(out=wbm, in0=raw, in1=self.COLS[:, CMU + mi * 8:CMU + mi * 8 + 8].unsqueeze(2).to_broadcast([128, 8, 128]),
                                                      op=ALU.mult), r=[P.b("WIN", 0), P.b("COLS")], w=[P.b("WIN", 0)])
                P.op("dve", lambda e: e.tensor_tensor(out=raw, in0=raw, in1=self.OMMU[:, mi * 8:mi * 8 + 8].unsqueeze(2).to_broadcast([128, 8, 128]),
                                                      op=ALU.mult), r=[P.b("WIN", 0), P.b("OMMU")], w=[P.b("WIN", 0)])
            for m, (Tm, km) in enumerate(((T0, k0), (T1, k1))):
                for tb in range(2):
                    gi = self.rot("pgu", 2)
                    n = 0
                    for (off, xt) in ((m * 1024, self.XT), (2048 + m * 1024, self.XTs)):
                        for kci in range(8):
                            P.op("pe", lambda e: e.matmul(
                                self.PG[gi][:, :], lhsT=self.WIN[0][:, off + kci * 128: off + (kci + 1) * 128],
                                rhs=xt[:, kci, tb * 512:(tb + 1) * 512], start=(n == 0), stop=(n == 15)),
                                r=[P.b("WIN", 0), P.b("XTC")] + [P.b("XT", tb * 4 + i) for i in range(4)]
                                + ([P.b("XT", tb * 4 - 1)] if tb > 0 else []), w=[P.b("PG", gi)])
                            n += 1
                    P.op("act", lambda e: e.activation(out=Tm[:, tb * 512:(tb + 1) * 512], in_=self.PG[gi][:, :], func=AF.Copy),
                         r=[P.b("PG", gi)], w=km)
            for tb in range(2):
                ts = slice(tb * 512, (tb + 1) * 512)
                gi = self.rot("pgu", 2)
                P.op("pe", lambda e: e.matmul(self.PU[gi][:, :], lhsT=self.L2W[0:64, kc * 128:(kc + 1) * 128],
                                              rhs=self.LW1[0:64, ts], start=True, stop=True),
                     r=[P.b("L2W"), P.b("LW1", tb)], w=[P.b("PU", gi)])
                P.op("act", lambda e: e.activation(out=T2[:, ts], in_=self.PU[gi][:, :], func=AF.Sigmoid,
                                                   bias=self.COLS[:, CW0 + kc:CW0 + kc + 1], scale=1.0),
                     r=[P.b("PU", gi), P.b("COLS")], w=k2)
            for c in range(16):
                cs = slice(c * 64, (c + 1) * 64)
                P.op("dve", lambda e: e.tensor_tensor_scan(out=T3[:, cs], data0=self.ONES[:, 0:64], data1=T2[:, cs],
                                                           initial=0.0, op0=ALU.mult, op1=ALU.add),
                     r=k2 + [P.b("ONES")], w=k3)
            P.op("dve", lambda e: e.tensor_tensor(out=T4[:, :], in0=T3[:, :], in1=T2[:, :], op=ALU.subtract),
                 r=k2 + k3, w=k4)
            P.op("act", lambda e: e.activation(out=T4[:, :], in_=T4[:, :], func=AF.Exp, scale=-c0e), r=k4, w=k4)
            P.op("act", lambda e: e.activation(out=T5[:, :], in_=T3[:, :], func=AF.Exp, scale=-c0e), r=k3, w=k5)
            P.op("act", lambda e: e.activation(out=T3[:, :], in_=T3[:, :], func=AF.Exp, scale=c0e), r=k3, w=k3)
            P.op("dve", lambda e: e.tensor_copy(out=self.GCC[:, :], in_=c3(T5)[:, :, 63]), r=k5, w=[P.b("GCC")])
            for tb in range(2):
                ts = slice(tb * 512, (tb + 1) * 512)
                gi = self.rot("pgu", 2)
                P.op("pe", lambda e: e.matmul(self.PU[gi][:, :], lhsT=self.L2A[0:64, kc * 128:(kc + 1) * 128],
                                              rhs=self.LA1[0:64, ts], start=True, stop=True),
                     r=[P.b("L2A"), P.b("LA1", tb)], w=[P.b("PU", gi)])
                P.op("act", lambda e: e.activation(out=T2[:, ts], in_=self.PU[gi][:, :], func=AF.Sigmoid,
                                                   bias=self.COLS[:, CA0 + kc:CA0 + kc + 1], scale=1.0),
                     r=[P.b("PU", gi), P.b("COLS")], w=k2)
            P.op("act", lambda e: e.activation(out=T6[:, :], in_=T1[:, :], func=AF.Copy, scale=self.COLS[:, CKK + kc:CKK + kc + 1]),
                 r=k1 + [P.b("COLS")], w=k6)
            P.op("act", lambda e: e.activation(out=TMPB, in_=T6[:, :], func=AF.Square), r=k6, w=kTMPB)
            for tb in range(2):
                ts = slice(tb * 512, (tb + 1) * 512)
                gi = self.rot("pgu", 2)
                P.op("pe", lambda e: e.matmul(self.PU[gi][:, :], lhsT=self.BLK[:, :], rhs=TMPB[:, ts], start=True, stop=True),
                     r=[P.b("BLK")] + kTMPB, w=[P.b("PU", gi)])
                P.op("act", lambda e: e.activation(out=self.GB[0][:, ts], in_=self.PU[gi][:, :], func=AF.Ln),
                     r=[P.b("PU", gi)], w=[P.b("GB", 0)])
            P.op("act", lambda e: e.activation(out=self.GB[0][:, :], in_=self.GB[0][:, :], func=AF.Exp, scale=-0.5),
                 r=[P.b("GB", 0)], w=[P.b("GB", 0)])
            P.op("dve", lambda e: e.tensor_tensor(out=T6[:, :], in0=T6[:, :], in1=self.GB[0][:, :], op=ALU.mult),
                 r=k6 + [P.b("GB", 0)], w=k6)
            P.op("act", lambda e: e.activation(out=TMPB, in_=T2[:, :], func=AF.Identity, scale=self.COLS[:, CKA + kc:CKA + kc + 1],
                                               bias=self.OMKA[:, kc:kc + 1]),
                 r=k2 + [P.b("COLS"), P.b("OMKA")], w=kTMPB)
            P.op("dve", lambda e: e.tensor_tensor(out=T1[:, :], in0=T1[:, :], in1=TMPB, op=ALU.mult), r=k1 + kTMPB, w=k1)
            P.op("dve", lambda e: e.scalar_tensor_tensor(out=AR[:, :, 0, :], in0=c3t(T6), scalar=-1.0, in1=c3t(T4),
                                                         op0=ALU.mult, op1=ALU.mult), r=k6 + k4, w=kAR)
            P.op("dve", lambda e: e.tensor_tensor(out=AR[:, :, 1, :], in0=c3t(T0), in1=c3t(T5), op=ALU.mult), r=k0 + k5, w=kAR)
            P.op("dve", lambda e: e.tensor_tensor(out=KTL, in0=T1[:, :], in1=T3[:, :], op=ALU.mult), r=k1 + k3, w=kKTL)
            P.op("dve", lambda e: e.tensor_tensor(out=T6[:, :], in0=T6[:, :], in1=T2[:, :], op=ALU.mult), r=k6 + k2, w=k6)
            P.op("dve", lambda e: e.tensor_tensor(out=BTL, in0=T6[:, :], in1=T3[:, :], op=ALU.mult), r=k6 + k3, w=kBTL)
            gcb = self.GCC[:, :].unsqueeze(2).to_broadcast([128, 16, 64])
            P.op("dve", lambda e: e.tensor_tensor(out=c3(KH), in0=c3(KTL), in1=gcb, op=ALU.mult), r=kKTL + [P.b("GCC")], w=kKH)
            P.op("dve", lambda e: e.tensor_tensor(out=c3(BH), in0=c3(BTL), in1=gcb, op=ALU.mult), r=kBTL + [P.b("GCC")], w=kBH)
            P.op("dve", lambda e: e.scalar_tensor_tensor(out=PB, in0=T0[:, :], scalar=self.COLS[:, CRK + kc:CRK + kc + 1],
                                                         in1=T1[:, :], op0=ALU.mult, op1=ALU.mult),
                 r=k0 + k1 + [P.b("COLS")], w=kPB)
            gi = self.rot("pgu", 2)
            for tt in range(NTT):
                P.op("pe", lambda e: e.matmul(self.PU[gi][:, tt * 2:tt * 2 + 2], lhsT=PB[:, tt * 128:(tt + 1) * 128],
                                              rhs=self.HSEL[:, :], start=True, stop=True),
                     r=kPB + [P.b("HSEL")], w=[P.b("PU", gi)])
            P.op("act", lambda e: e.activation(out=self.BS[:, :, 2 * kc:2 * kc + 2],
                                               in_=self.PU[gi][:, 0:16].rearrange("p (t h) -> p t h", h=2), func=AF.Copy),
                 r=[P.b("PU", gi)], w=[P.b("BS", kc)])
            prs = [slice(0, 64), slice(64, 128)]
            KB = lambda kind, bt: P.b("RK", kind, bt)
            kall = lambda kind: [P.b("RK", kind, bt) for bt in range(4)]
            alias = k0 + k1 + k2 + k3 + k4 + k5 + k6 + kPB + [P.b("WIN", 0), P.b("WIN", 1)]
            rkk = [P.b("RK", kd, bt) for kd in ("LAK", "MRK", "PN", "MRB", "PTN", "XX", "ATOK", "BHT", "KHT", "W0", "U0", "AH", "MTB")
                   for bt in range(4)] + [P.b("DD"), P.b("DG")] + [P.b("SALL", i) for i in range(17)]
            P.op("pool", lambda e: e.memset(self.SEMT[:, 0:1], 0.0), w=alias + rkk + [P.b("SEMT")])
            P.op("pool", lambda e: e.memset(MTB[:, :, :], 0.0), w=kall("MTB"))
            P.op("pool", lambda e: e.tensor_tensor(out=DG, in0=self.ID2[:, :].unsqueeze(1).to_broadcast([128, 16, 64]),
                                                   in1=self.GCC[:, :].unsqueeze(2).to_broadcast([128, 16, 64]), op=ALU.mult),
                 r=[P.b("GCC"), P.b("RMASK")], w=[P.b("DG")])
            P.op("act", lambda e: e.activation(out=SALL[:, 0, :], in_=self.STC[:, kc, :], func=AF.Copy),
                 r=[P.b("STC", kc)], w=[P.b("SALL", 0)])
            for cp in range(8):
                tl = slice(cp * 128, (cp + 1) * 128)
                for hp in range(2):
                    pr = prs[hp]
                    u = cp * 2 + hp
                    bt = u // 4
                    for (src, off, key) in ((KTL, 0, kKTL), (BTL, 256, kBTL)):
                        P.op("pe", lambda e: e.matmul(self.PO[hp][:, off:off + 256], lhsT=src[pr, tl],
                                                      rhs=AR[pr, cp, :, :], start=True, stop=True),
                             r=key + kAR, w=[P.b("PO", hp)])
                    P.op("pe", lambda e: e.matmul(self.PU[hp][:, 0:128], lhsT=AR[pr, cp, 0, :], rhs=BTL[pr, tl],
                                                  start=True, stop=True), r=kAR + kBTL, w=[P.b("PU", hp)])
                    m2 = self.RMASK[:, 0:256].rearrange("p (a t) -> p a t", a=2)
                    P.op("dve", lambda e: e.tensor_tensor(out=W0k[:, :, u, :], in0=self.PO[hp][:, 0:256].rearrange("p (a t) -> p a t", a=2),
                                                          in1=m2, op=ALU.mult),
                         r=[P.b("PO", hp), P.b("RMASK")], w=[KB("LAK", bt), KB("MRK", bt)])
                    P.op("dve", lambda e: e.tensor_tensor(out=W1k[:, :, u, :], in0=self.PO[hp][:, 256:512].rearrange("p (a t) -> p a t", a=2),
                                                          in1=m2, op=ALU.mult),
                         r=[P.b("PO", hp), P.b("RMASK")], w=[KB("PN", bt), KB("MRB", bt)])
                    P.op("dve", lambda e: e.tensor_tensor(out=PTN[:, u, :], in0=self.PU[hp][:, 0:128], in1=self.RMASK[:, 256:384], op=ALU.mult),
                         r=[P.b("PU", hp), P.b("RMASK")], w=[KB("PTN", bt)])
            for bt in range(4):
                P.op("pool", lambda e: e.tensor_tensor(out=XX[:, bt * 4:(bt + 1) * 4, :], in0=PN[:, bt * 4:(bt + 1) * 4, :],
                                                       in1=self.IDB[:, :].unsqueeze(1).to_broadcast([128, 4, 128]), op=ALU.add),
                     r=[KB("PN", bt), P.b("IDB")], w=[KB("XX", bt)])
            for (dst, dkind, srcf, skey) in ((ATOK, "ATOK", lambda cp, pr: AR[pr, cp, 0, :], kAR),
                                             (BHT, "BHT", lambda cp, pr: BH[pr, cp * 128:(cp + 1) * 128], kBH),
                                             (KHT, "KHT", lambda cp, pr: KH[pr, cp * 128:(cp + 1) * 128], kKH)):
                for u in range(16):
                    cp, hp = u // 2, u % 2
                    pt = self.PT if hp == 0 else self.PT2
                    P.op("pe", lambda e: e.transpose(out=pt[:, cp * 64:(cp + 1) * 64], in_=srcf(cp, prs[hp]),
                                                     identity=self.IDB[prs[hp], hp * 64:(hp + 1) * 64]),
                         r=skey + [P.b("IDB")], w=[P.b("PT" if hp == 0 else "PT2")])
                for hp in range(2):
                    pt = self.PT if hp == 0 else self.PT2
                    P.op("act" if hp == 0 else "dve", lambda e: (e.activation(
                        out=dst[:, hp:16:2, :], in_=pt[:, 0:512].rearrange("p (u t) -> p u t", t=64), func=AF.Copy) if hp == 0 else
                        e.tensor_copy(out=dst[:, hp:16:2, :], in_=pt[:, 0:512].rearrange("p (u t) -> p u t", t=64))),
                        r=[P.b("PT" if hp == 0 else "PT2")], w=kall(dkind))
            for m in range(5):
                for bt in range(4):
                    us = range(bt * 4, bt * 4 + 4)
                    pb = bt % 2
                    for i, u in enumerate(us):
                        P.op("pe", lambda e: e.matmul(self.PG[pb][:, i * 128:(i + 1) * 128], lhsT=PN[:, u, :], rhs=PTN[:, u, :],
                                                      start=True, stop=True), r=[KB("PN", bt), KB("PTN", bt)], w=[P.b("PG", pb)])
                    if m < 4:
                        for i, u in enumerate(us):
                            P.op("pe", lambda e: e.matmul(self.PU[pb][:, i * 128:(i + 1) * 128], lhsT=PTN[:, u, :], rhs=PN[:, u, :],
                                                          start=True, stop=True), r=[KB("PN", bt), KB("PTN", bt)], w=[P.b("PU", pb)])
                    P.op("act", lambda e: e.activation(out=PTN[:, bt * 4:(bt + 1) * 4, :],
                                                       in_=self.PG[pb][:, :].rearrange("p (u t) -> p u t", t=128), func=AF.Copy),
                         r=[P.b("PG", pb)], w=[KB("PTN", bt)])
                    if m < 4:
                        P.op("dve", lambda e: e.tensor_copy(out=PN[:, bt * 4:(bt + 1) * 4, :],
                                                            in_=self.PU[pb][:, :].rearrange("p (u t) -> p u t", t=128)),
                             r=[P.b("PU", pb)], w=[KB("PN", bt)])
                    for i, u in enumerate(us):
                        P.op("pe", lambda e: e.matmul(self.PO[pb][:, i * 128:(i + 1) * 128], lhsT=PTN[:, u, :], rhs=XX[:, u, :],
                                                      start=True, stop=True), r=[KB("PTN", bt), KB("XX", bt)], w=[P.b("PO", pb)])
                    P.op("dve", lambda e: e.tensor_tensor(out=XX[:, bt * 4:(bt + 1) * 4, :], in0=XX[:, bt * 4:(bt + 1) * 4, :],
                                                          in1=self.PO[pb][:, :].rearrange("p (u t) -> p u t", t=128), op=ALU.add),
                         r=[KB("XX", bt), P.b("PO", pb)], w=[KB("XX", bt)])
            vcol = lambda u: ACCb[:, u // 2, (2 * kc + u % 2) * 64:(2 * kc + u % 2 + 1) * 64]
            for b8 in range(2):
                for i in range(8):
                    u = b8 * 8 + i
                    P.op("pe", lambda e: e.matmul(self.PG[b8][:, i * 64:(i + 1) * 64], lhsT=LAK[:, u, :], rhs=vcol(u), start=True, stop=True),
                         r=[KB("LAK", u // 4), P.b("ACC", u // 2, 0)], w=[P.b("PG", b8)])
                P.op("act", lambda e: e.activation(out=W0s[:, b8 * 8:(b8 + 1) * 8, :], in_=self.PG[b8][:, :].rearrange("p (u t) -> p u t", t=64),
                                                   func=AF.Copy), r=[P.b("PG", b8)], w=[KB("W0", 2 * b8), KB("W0", 2 * b8 + 1)])
            for b8 in range(2):
                for i in range(8):
                    u = b8 * 8 + i
                    P.op("pe", lambda e: e.matmul(self.PO[b8][:, i * 64:(i + 1) * 64], lhsT=XX[:, u, :], rhs=ATOK[:, u, :], start=True, stop=True),
                         r=[KB("XX", u // 4), KB("ATOK", u // 4)], w=[P.b("PO", b8)])
                P.op("dve", lambda e: e.tensor_copy(out=AHs[:, b8 * 8:(b8 + 1) * 8, :], in_=self.PO[b8][:, :].rearrange("p (u t) -> p u t", t=64)),
                     r=[P.b("PO", b8)], w=[KB("AH", 2 * b8), KB("AH", 2 * b8 + 1)])
            for b8 in range(2):
                for i in range(8):
                    u = b8 * 8 + i
                    P.op("pe", lambda e: e.matmul(self.PU[b8][:, i * 64:(i + 1) * 64], lhsT=XX[:, u, :], rhs=W0s[:, u, :], start=True, stop=True),
                         r=[KB("XX", u // 4), KB("W0", u // 4)], w=[P.b("PU", b8)])
                P.op("act", lambda e: e.activation(out=U0s[:, b8 * 8:(b8 + 1) * 8, :], in_=self.PU[b8][:, :].rearrange("p (u t) -> p u t", t=64),
                                                   func=AF.Copy), r=[P.b("PU", b8)], w=[KB("U0", 2 * b8), KB("U0", 2 * b8 + 1)])
            for cpar in range(2):
                tp = prs[cpar]
                for u in range(16):
                    cp, hp = u // 2, u % 2
                    P.op("pe", lambda e: e.matmul(self.PG[cpar][prs[hp], cp * 64:(cp + 1) * 64], lhsT=AHs[tp, u, :], rhs=BHT[tp, u, :],
                                                  start=True, stop=True), r=[KB("AH", u // 4), KB("BHT", u // 4)], w=[P.b("PG", cpar)])
                for hp in range(2):
                    P.op("dve", lambda e: e.tensor_tensor(
                        out=MTB[prs[hp], cpar:16:2, hp * 64:(hp + 1) * 64],
                        in0=self.PG[cpar][prs[hp], :].rearrange("p (c t) -> p c t", t=64),
                        in1=DG[prs[hp], cpar:16:2, :], op=ALU.add),
                        r=[P.b("PG", cpar), P.b("DG")], w=kall("MTB"))
                for u in range(16):
                    cp, hp = u // 2, u % 2
                    P.op("pe", lambda e: e.matmul(self.PU[cpar][prs[hp], cp * 64:(cp + 1) * 64], lhsT=BHT[tp, u, :], rhs=U0s[tp, u, :],
                                                  start=True, stop=False), r=[KB("BHT", u // 4), KB("U0", u // 4)], w=[P.b("PU", cpar)])
                    P.op("pe", lambda e: e.matmul(self.PU[cpar][prs[hp], cp * 64:(cp + 1) * 64], lhsT=KHT[tp, u, :],
                                                  rhs=ACCb[tp, cp, (2 * kc + hp) * 64:(2 * kc + hp + 1) * 64],
                                                  start=False, stop=True), r=[KB("KHT", u // 4), P.b("ACC", cp, 0)], w=[P.b("PU", cpar)])
                P.op("act", lambda e: e.activation(out=DD[:, cpar:16:2, :], in_=self.PU[cpar][:, :].rearrange("p (c t) -> p c t", t=64),
                                                   func=AF.Copy), r=[P.b("PU", cpar)], w=[P.b("DD")])
            for b4 in range(2):
                for i in range(4):
                    cp = b4 * 4 + i
                    for hp in range(2):
                        u = cp * 2 + hp
                        P.op("pe", lambda e: e.matmul(self.PO[b4][prs[hp], i * 128:(i + 1) * 128], lhsT=AHs[:, u, :], rhs=MRB[:, u, :],
                                                      start=True, stop=True), r=[KB("AH", u // 4), KB("MRB", u // 4)], w=[P.b("PO", b4)])
                P.op("dve", lambda e: e.tensor_tensor(out=AR[:, b4 * 4:(b4 + 1) * 4, 1, :], in0=AR[:, b4 * 4:(b4 + 1) * 4, 1, :],
                                                      in1=self.PO[b4][:, :].rearrange("p (c t) -> p c t", t=128), op=ALU.add),
                     r=kAR + [P.b("PO", b4)], w=kAR)
            for c in range(16):
                pb = c % 2
                P.op("pe", lambda e: e.matmul(self.PG[pb][:, 0:64], lhsT=MTB[:, c, :], rhs=SALL[:, c, :], start=True, stop=True),
                     r=kall("MTB") + [P.b("SALL", c)], w=[P.b("PG", pb)])
                P.op("dve", lambda e: e.tensor_tensor(out=SALL[:, c + 1, :], in0=self.PG[pb][:, 0:64], in1=DD[:, c, :], op=ALU.add),
                     r=[P.b("PG", pb), P.b("DD")], w=[P.b("SALL", c + 1)])
            P.op("act", lambda e: e.activation(out=self.STC[:, kc, :], in_=SALL[:, 16, :], func=AF.Copy),
                 r=[P.b("SALL", 16)], w=[P.b("STC", kc)])
            for hp in range(2):
                pr = prs[hp]
                for cp in range(8):
                    u = cp * 2 + hp
                    oc = slice(cp * 64, (cp + 1) * 64)
                    P.op("pe", lambda e: e.matmul(self.PO[hp][:, oc], lhsT=MRB[:, u, :], rhs=U0s[:, u, :], start=True, stop=False),
                         r=[KB("MRB", u // 4), KB("U0", u // 4)], w=[P.b("PO", hp)])
                    P.op("pe", lambda e: e.matmul(self.PO[hp][:, oc], lhsT=MRK[:, u, :], rhs=vcol(u), start=False, stop=False),
                         r=[KB("MRK", u // 4), P.b("ACC", cp, 0)], w=[P.b("PO", hp)])
                    for cpar in range(2):
                        c = 2 * cp + cpar
                        P.op("pe", lambda e: e.matmul(self.PO[hp][prs[cpar], oc], lhsT=AR[pr, cp, 1, cpar * 64:(cpar + 1) * 64],
                                                      rhs=SALL[pr, c, :], start=False, stop=True),
                             r=kAR + [P.b("SALL", c)], w=[P.b("PO", hp)])
                hd = 2 * kc + hp
                P.op("act", lambda e: e.activation(out=ACCb[:, :, 1024 + hd * 64:1024 + (hd + 1) * 64],
                                                   in_=self.PO[hp][:, :].rearrange("p (c t) -> p c t", t=64), func=AF.Copy),
                     r=[P.b("PO", hp)], w=[P.b("ACC", tt, 1) for tt in range(8)])
            P.op("pool", lambda e: e.memset(self.SEMT[:, 0:1], 0.0), w=alias + rkk + [P.b("SEMT")])
        if half == 0:
            P.op("dve", lambda e: e.tensor_copy(out=self.XC[:, :].unsqueeze(2), in_=self.XTfull[:, :, 8 + TB - 1:8 + TB]),
                 r=[P.b("XT", 7)], w=[P.b("XC")])
        g1 = self.load_row_bc(R0 + 0)
        g2 = self.load_row_bc(R0 + 1)
        h3 = lambda t: t.rearrange("p (h d) -> p h d", d=64)
        for c in range(NTT):
            yb = ACCb[:, c, 1024:2048]
            ky = [P.b("ACC", c, 1)]
            P.op("dve", lambda e: e.tensor_reduce(out=self.S1[:, :], in_=h3(yb), axis=mybir.AxisListType.X, op=ALU.add),
                 r=ky, w=[P.b("S1")])
            P.op("dve", lambda e: e.tensor_tensor(out=T0[:, :], in0=yb, in1=yb, op=ALU.mult), r=ky, w=k0)
            P.op("dve", lambda e: e.tensor_reduce(out=self.S2[:, :], in_=h3(T0[:, :]), axis=mybir.AxisListType.X, op=ALU.add),
                 r=k0, w=[P.b("S2")])
            P.op("dve", lambda e: e.tensor_scalar(out=self.S1[:, :], in0=self.S1[:, :], scalar1=1.0 / 64, scalar2=None,
                                                  op0=ALU.mult), r=[P.b("S1")], w=[P.b("S1")])
            P.op("dve", lambda e: e.tensor_tensor(out=self.S3[:, :], in0=self.S1[:, :], in1=self.S1[:, :], op=ALU.mult),
                 r=[P.b("S1")], w=[P.b("S3")])
            P.op("dve", lambda e: e.scalar_tensor_tensor(out=self.S2[:, :], in0=self.S2[:, :], scalar=1.0 / 64, in1=self.S3[:, :],
                                                         op0=ALU.mult, op1=ALU.subtract), r=[P.b("S2"), P.b("S3")], w=[P.b("S2")])
            self.rstd_batch(self.S2, 16, 1.0, 64e-5, key="S2")
            P.op("dve", lambda e: e.tensor_tensor(out=h3(T0[:, :]), in0=h3(yb), in1=self.S1[:, :].unsqueeze(2).to_broadcast([128, 16, 64]),
                                                  op=ALU.subtract), r=ky + [P.b("S1")], w=k0)
            P.op("dve", lambda e: e.tensor_tensor(out=h3(T0[:, :]), in0=h3(T0[:, :]), in1=self.S2[:, :].unsqueeze(2).to_broadcast([128, 16, 64]),
                                                  op=ALU.mult), r=k0 + [P.b("S2")], w=k0)
            P.op("dve", lambda e: e.tensor_tensor(out=T0[:, :], in0=T0[:, :], in1=self.GB[g1][:, :], op=ALU.mult),
                 r=k0 + [P.b("GB", g1)], w=k0)
            P.op("dve", lambda e: e.tensor_tensor(out=T0[:, :], in0=T0[:, :], in1=self.GB[g2][:, :], op=ALU.add),
                 r=k0 + [P.b("GB", g2)], w=k0)
            P.op("dve", lambda e: e.tensor_tensor(out=h3(T1[:, :]), in0=h3(ACCb[:, c, 0:1024]),
                                                  in1=self.BS[:, c, :].unsqueeze(2).to_broadcast([128, 16, 64]), op=ALU.mult),
                 r=[P.b("ACC", c, 0)] + [P.b("BS", i) for i in range(8)], w=k1)
            P.op("dve", lambda e: e.tensor_tensor(out=yb, in0=T0[:, :], in1=T1[:, :], op=ALU.add), r=k0 + k1, w=ky)
        P.op("pool", lambda e: e.dma_start(out=self.L2W[:, :], in_=self.dram["rwkv_g2"].ap()[0:128, :]), w=[P.b("L2W")], dma=True)
        P.op("pool", lambda e: e.dma_start(out=self.L2A[0:32, :], in_=self.dram["rwkv_g2"].ap()[128:160, :]), w=[P.b("L2A")], dma=True)
        for tt in range(NTT):
            tsl = slice(tt * 128, (tt + 1) * 128)
            for kc in range(8):
                P.op("pe", lambda e: e.transpose(out=self.PT[:, kc * 128:(kc + 1) * 128], in_=ACCb[:, tt, 1024 + kc * 128:1024 + (kc + 1) * 128],
                                                 identity=self.IDB[:, :]), r=[P.b("ACC", tt, 1), P.b("IDB")], w=[P.b("PT")])
            for kc in range(8):
                pg = self.PG[kc // 4]
                P.op("pe", lambda e: e.matmul(pg[:, (kc % 4) * 128:(kc % 4 + 1) * 128], lhsT=self.L2W[:, kc * 128:(kc + 1) * 128],
                                              rhs=self.XS[0][:, tsl], start=True, stop=False),
                     r=[P.b("L2W"), P.b("XS", 0)], w=[P.b("PG", kc // 4)])
                P.op("pe", lambda e: e.matmul(pg[:, (kc % 4) * 128:(kc % 4 + 1) * 128], lhsT=self.L2A[0:32, kc * 128:(kc + 1) * 128],
                                              rhs=self.XS[1][0:32, tsl], start=False, stop=True),
                     r=[P.b("L2A"), P.b("XS", 1)], w=[P.b("PG", kc // 4)])
            for hh in range(2):
                P.op("act", lambda e: e.activation(out=TMPB[:, hh * 512:(hh + 1) * 512], in_=self.PG[hh][:, :], func=AF.Copy),
                     r=[P.b("PG", hh)], w=kTMPB)
            P.op("dve", lambda e: e.tensor_tensor(out=self.XT[:, :, tsl], in0=self.PT[:, :].rearrange("p (k c) -> p k c", k=8),
                                                  in1=TMPB.rearrange("p (k c) -> p k c", k=8), op=ALU.mult),
                 r=[P.b("PT")] + kTMPB, w=[P.b("XT", tt)])
        self.out_proj("rwkv_wo", (), None)
        self.post_norm(l * 6 + 3, half=False)

    def build(self):
        nc, P, cfg = self.nc, self.P, self.cfg
        x_d = self.din("x", [SEQ, D])
        self.din("rows", [cfg["nrows"], D])
        self.din("ffn_win", [DEPTH, 2, NSLAB, 128, 8 * 512])
        self.din("ffn_wout", [DEPTH, 2, NSLAB, 128, 2 * 1024])
        self.din("idb", [128, 128], BF16)
        self.din("cmask", [128, 1024], BF16)
        self.din("cols", [128, cfg["ncols"]])
        self.din("swa_wq", [2, 2, 128, 4096])
        self.din("gla_win", [7, 128, 4096])
        self.din("rwkv_wl1", [1, 128, 4096])
        self.din("rwkv_wrk", [8, 128, 2048])
        self.din("rwkv_wv", [2, 128, 4096])
        self.din("rwkv_wo", [2, 128, 4096])
        self.din("rwkv_w2", [64, 1024])
        self.din("rwkv_a2", [64, 1024])
        self.din("rwkv_g2", [160, 1024])
        self.din("rconst", [128, 384 + 128 + 2 + 64], BF16)
        self.din("gla_wo", [2, 128, 4096])
        self.din("gla_wg2", [16, 512])
        self.din("swa_wk", [2, 128, 4096])
        self.din("swa_wv", [2, 128, 4096])
        self.din("swa_wo", [2, 2, 128, 4096])
        y_d = self.nc.dram_tensor("y", [SEQ, D], F32, kind="ExternalOutput")

        self.H = self.sb("H", [128, NTT, D], F32)
        self.XTfull = self.sb("XTfull", [128, 8, TB + 8], BF16)
        self.XT = self.XTfull[:, :, 8:8 + TB]
        self.XTs = self.XTfull[:, :, 7:7 + TB]
        self.RCONST = self.sb("RCONST", [128, 384 + 128 + 2 + 64], BF16)
        self.RMASK = self.RCONST[:, 0:384]
        self.BLK = self.RCONST[:, 384:512]
        self.HSEL = self.RCONST[:, 512:514]
        self.ID2 = self.RCONST[:, 514:578]
        self.STC = self.sb("STC", [128, 8, 64], BF16)
        self.SEMT = self.sb("SEMT", [128, 4], F32)
        self.XC = self.sb("XC", [128, 8], BF16)
        self.OMMU = self.sb("OMMU", [128, 48], F32)
        self.OMKA = self.sb("OMKA", [128, 8], F32)
        self.GCC = self.sb("GCC", [128, 16], F32)
        self.BS = self.sb("BS", [128, 8, 16], F32)
        self.S1 = self.sb("S1", [128, 16], F32)
        self.S2 = self.sb("S2", [128, 16], F32)
        self.S3 = self.sb("S3", [128, 16], F32)
        self.LW1 = self.sb("LW1", [64, TB], BF16)
        self.LA1 = self.sb("LA1", [64, TB], BF16)
        self.L2W = self.sb("L2W", [128, D], BF16)
        self.L2A = self.sb("L2A", [64, D], BF16)
        self.ACC = self.sb("ACC", [128, NTT, D], F32)
        self.ATS = [self.sb("ATS%d" % i, [128, 2, TB], BF16) for i in range(2)]
        self.WIN = [self.sb("WIN%d" % i, [128, 8 * 512], BF16) for i in range(2)]
        self.WOUT = [self.sb("WOUT%d" % i, [128, 2 * 1024], BF16) for i in range(2)]
        self.GB = [self.sb("GB%d" % i, [128, D], F32) for i in range(2)]
        self.XS = [self.sb("XS%d" % i, [128, D], BF16) for i in range(2)]
        self.SG = [self.sb("SG%d" % i, [128, 512], BF16) for i in range(2)]
        self.JUNK = self.sb("JUNK", [128, D], BF16)
        self.SS = self.sb("SS", [128, 16], F32)
        self.IDB = self.sb("IDB", [128, 128], BF16)
        self.epsc = {EPS: self.sb("eps0", [128, 1], F32), 4 * EPS: self.sb("eps4", [128, 1], F32), 1e-5: self.sb("eps1", [128, 1], F32),
                     64e-5: self.sb("eps2", [128, 1], F32)}
        self.GS = self.sb("GS", [128, 4, 256], F32)
        self.GSB = self.sb("GSB", [128, 4, 256], BF16)
        self.EBLS = self.sb("EBLS", [128, 4, 8], F32)
        self.ONES = self.sb("ONES", [128, 128], F32)
        self.ONE1 = self.ONES
        self.NBG = self.sb("NBG", [128, 4], F32)
        self.SSG = self.sb("SSG", [128, 4], F32)
        self.CMASK = self.sb("CMASK", [128, 1024], BF16)
        self.COLS = self.sb("COLS", [128, cfg["ncols"]], F32)
        self.QT = self.sb("QT", [128, 8, TB], BF16)
        self.KT = self.sb("KT", [128, 4, TB + 128], BF16)
        self.VA = self.sb("VA", [128, 9, 260], BF16)
        self.KC = [self.sb("KC%d" % i, [128, 4, 128], BF16) for i in range(2)]
        self.VC = [self.sb("VC%d" % i, [128, 260], BF16) for i in range(2)]
        self.PTS = [self.sb("PTS%d" % i, [128, 512], BF16) for i in range(4)]
        self.ESK = self.sb("ESK", [128, 16], F32)
        self.BQ8 = self.sb("BQ8", [128, 8], F32)
        self.DEN = self.sb("DEN", [128, 4], F32)

        self.PG = [self.ps("PG%d" % i, [128, 512], F32) for i in range(2)]
        self.PU = [self.ps("PU%d" % i, [128, 512], F32) for i in range(2)]
        self.PO = [self.ps("PO%d" % i, [128, 512], F32) for i in range(2)]
        self.PT = self.ps("PT", [128, 1024], BF16)
        self.PT2 = self.ps("PT2", [128, 1024], BF16)

        P.op("sp", lambda e: e.dma_start(out=self.IDB[:, :], in_=self.dram["idb"].ap()[:, :]), w=[P.b("IDB")], dma=True)
        P.op("sp", lambda e: e.dma_start(out=self.CMASK[:, :], in_=self.dram["cmask"].ap()[:, :]), w=[P.b("CMASK")], dma=True)
        P.op("sp", lambda e: e.dma_start(out=self.COLS[:, :], in_=self.dram["cols"].ap()[:, :]), w=[P.b("COLS")], dma=True)
        P.op("dve", lambda e: e.memset(self.VA[:, :, :], 1.0), w=[P.b("VA", i) for i in range(-1, 8)])
        P.op("dve", lambda e: e.memset(self.ONES[:, :], 1.0), w=[P.b("ONES")])
        P.op("sp", lambda e: e.dma_start(out=self.RCONST[:, :], in_=self.dram["rconst"].ap()[:, :]),
             w=[P.b("RMASK"), P.b("BLK"), P.b("HSEL")], dma=True)
        for eps, t in self.epsc.items():
            P.op("dve", lambda e, t=t, eps=eps: e.memset(t[:, :], eps), w=[P.b("epsc")])

        xv = x_d.ap().rearrange("(n p) d -> p n d", p=128)
        yv = y_d.ap().rearrange("(n p) d -> p n d", p=128)
        stages = cfg["stages"]
        for half in range(2):
            self.half = half
            for tt in range(NTT):
                P.op("sp", lambda e, tt=tt, half=half: e.dma_start(out=self.H[:, tt, :], in_=xv[:, half * NTT + tt, :]),
                     w=[P.b("H", tt)], dma=True)
            self.norm_done = None
            for si, (l, what) in enumerate(stages):
                nxt = stages[si + 1] if si + 1 < len(stages) else None
                self.next_norm_row = None if nxt is None else nxt[0] * 6 + {"a": 0, "m": 2, "b": 4}[nxt[1]]
                if not hasattr(self, "marks"):
                    self.marks = []
                self.marks.append(("h%d L%d %s" % (half, l, what), len(P.q["pe"])))
                if what == "a":
                    self.ffn(l, 0)
                elif what == "b":
                    self.ffn(l, 1)
                elif what == "m" and l % 3 == 0:
                    self.swa(l)
                elif what == "m" and l % 3 == 1:
                    self.gla(l)
                elif what == "m" and l % 3 == 2:
                    self.rwkv(l)
            outs = []
            for tt in range(NTT):
                outs.append(P.op("sp", lambda e, tt=tt, half=half: e.dma_start(out=yv[:, half * NTT + tt, :], in_=self.H[:, tt, :]),
                                 r=[P.b("H", tt)], dma=True))
        fin = P.op("sp", lambda e: e.nop(), r=[])
        for lst in (P.ndma["sp"][-Prog.NDMA:],):
            for o in lst:
                fin.deps.append(o)
        P.emit()
        return nc


ALL_STAGES = [(l, w) for l in range(DEPTH) for w in ("a", "m", "b")]


def host_layout(inputs):
    f = np.float32
    win = inputs["ffn_w_in"]
    L = win.shape[0]
    w = win.reshape(L, 2, 8, 128, 2, NSLAB, 2, 128).transpose(0, 1, 5, 3, 2, 6, 4, 7)
    ffn_win = np.ascontiguousarray(w).reshape(L, 2, NSLAB, 128, 8 * 512).astype(f, copy=False)
    wout = inputs["ffn_w_out"]
    w = wout.reshape(L, 2, NSLAB, 2, 128, D).transpose(0, 1, 2, 4, 3, 5)
    ffn_wout = np.ascontiguousarray(w).reshape(L, 2, NSLAB, 128, 2 * 1024).astype(f, copy=False)
    rows = [inputs["norm_g"].reshape(DEPTH * 6, D)]
    cols = []
    lay = {}

    def blk(wm):
        n = wm.shape[1] // 512
        return np.ascontiguousarray(wm.reshape(8, 128, n, 512).transpose(2, 1, 0, 3)).reshape(n, 128, 4096)

    def pad_row(v):
        r = np.zeros((1, D), f)
        r[0, :v.size] = v.reshape(-1)
        return r

    def col(v):
        return np.ascontiguousarray(v.reshape(-1, 128).T)

    lay["row_swa"] = sum(r.shape[0] for r in rows)
    lay["col_swa"] = sum(c.shape[1] for c in cols)
    wq, wk, wv, wo = [], [], [], []
    for j in range(2):
        wqkv = inputs["swa_w_qkv"][j]
        b = inputs["swa_b_qkv"][j]
        wq.append(blk(wqkv[:, 0:1024]))
        kd = wqkv[:, 1024:1280].reshape(1024, 4, 1, 64).repeat(2, axis=2).reshape(1024, 512)
        wk.append(blk(kd)[0])
        vd = np.concatenate([wqkv[:, 1280:1536], np.zeros((1024, 256), f)], axis=1)
        wv.append(blk(vd)[0])
        wo.append(blk(inputs["swa_w_o"][j]))
        rows += [pad_row(b[1280:1536]), pad_row(inputs["swa_b_o"][j]), pad_row(inputs["swa_sinks"][j])]
        bkd = b[1024:1280].reshape(4, 1, 64).repeat(2, axis=1).reshape(512)
        cols += [col(b[0:1024]), col(bkd)]
    out = {"swa_wq": np.stack(wq), "swa_wk": np.stack(wk), "swa_wv": np.stack(wv), "swa_wo": np.stack(wo)}
    lay["row_gla"] = sum(r.shape[0] for r in rows)
    lay["col_gla"] = sum(c.shape[1] for c in cols)
    gw = np.concatenate([inputs["gla_w_in"][0], np.zeros((1024, 7 * 512 - 3088), f)], axis=1)
    out["gla_win"] = blk(gw)
    out["gla_wo"] = blk(inputs["gla_w_o"][0])
    out["gla_wg2"] = np.ascontiguousarray(inputs["gla_w_gate2"][0]).astype(f, copy=False)
    rows += [np.tile(inputs["gla_norm_g"][0], 4)[None, :]]
    cols += [col(inputs["gla_b_gate"][0])]
    lay["row_rwkv"] = sum(r.shape[0] for r in rows)
    lay["col_rwkv"] = sum(c.shape[1] for c in cols)
    l1 = np.concatenate([inputs["rwkv_w1"][0], inputs["rwkv_a1"][0], inputs["rwkv_g1"][0],
                         np.zeros((1024, 512 - 288), f)], axis=1)
    out["rwkv_wl1"] = blk(l1)
    out["rwkv_wv"] = blk(inputs["rwkv_w_rkv"][0, 2])
    wrk = inputs["rwkv_w_rkv"][0, 0:2]
    out["rwkv_wrk"] = np.ascontiguousarray(wrk.reshape(2, 8, 128, 8, 128).transpose(3, 2, 0, 1, 4)).reshape(8, 128, 2048)
    out["rwkv_wo"] = blk(inputs["rwkv_w_o"][0])
    out["rwkv_w2"] = np.ascontiguousarray(inputs["rwkv_w2"][0])
    out["rwkv_a2"] = np.ascontiguousarray(inputs["rwkv_a2"][0])
    out["rwkv_g2"] = np.ascontiguousarray(inputs["rwkv_g2"][0])
    rows += [inputs["rwkv_lnx_g"][0][None, :], inputs["rwkv_lnx_b"][0][None, :]]
    cols += [col(inputs["rwkv_mu"][0].reshape(-1)), col(inputs["rwkv_w0"][0]), col(inputs["rwkv_a0"][0]),
             col(inputs["rwkv_k_k"][0]), col(inputs["rwkv_k_a"][0]), col(inputs["rwkv_r_k"][0].reshape(-1))]
    pp = np.arange(128)[:, None]
    ff = np.arange(128)[None, :]
    p6 = pp % 64
    f6 = np.arange(64)[None, :]
    same = (pp // 64 == ff // 64)
    rc = np.concatenate([same & (pp < ff), same & (pp <= ff), same & (pp > ff), same,
                         (pp // 64 == np.arange(2)[None, :]), (p6 == f6)], axis=1)
    out["rconst"] = rc.astype(np.float32).astype(ml_dtypes.bfloat16)
    rows = np.ascontiguousarray(np.concatenate(rows, axis=0)).astype(f, copy=False)
    cols = np.ascontiguousarray(np.concatenate(cols, axis=1)).astype(f, copy=False)
    idb = np.eye(128, dtype=np.float32).astype(ml_dtypes.bfloat16)
    jj = np.arange(128)[:, None]
    ii = np.arange(128)[None, :]
    cm = np.concatenate([np.tile((jj <= ii), (1, 4)), np.tile((jj > ii), (1, 4))], axis=1)
    cmask = cm.astype(np.float32).astype(ml_dtypes.bfloat16)
    out.update({"rows": rows, "cols": cols, "ffn_win": ffn_win, "ffn_wout": ffn_wout, "idb": idb, "cmask": cmask})
    for k in list(out):
        if out[k].dtype == np.float64:
            out[k] = out[k].astype(f)
    return out, lay


_CACHE = {}


def run(inputs, stages, ncores=8, trace=False):
    shared, lay = host_layout(inputs)
    cfg = {"stages": stages, "nrows": shared["rows"].shape[0], "ncols": shared["cols"].shape[1]}
    cfg.update(lay)
    kk = K(cfg)
    with kk.stack:
        nc = kk.build()
    x = np.ascontiguousarray(inputs["x"]).astype(np.float32, copy=False)
    in_maps = []
    for c in range(ncores):
        m = {"x": x[c]}
        for k in kk.dram:
            if k != "x":
                m[k] = shared[k]
        in_maps.append(m)
    res = run_bass_kernel_spmd(nc, in_maps, core_ids=list(range(ncores)), trace=trace)
    out = np.stack([np.asarray(r["y"]) for r in res.results], axis=0)
    return out.astype(np.float32, copy=False), res


def kernel(**inputs):
    out, _ = run(inputs, ALL_STAGES)
    return out
```

```python
import contextlib
import numpy as np
import ml_dtypes
import concourse.bass as bass
import concourse.mybir as mybir
from concourse.bass_utils import run_bass_kernel_spmd

F32 = mybir.dt.float32
BF16 = mybir.dt.bfloat16
AF = mybir.ActivationFunctionType
ALU = mybir.AluOpType

D = 1024
SEQ = 2048
DEPTH = 4
DFF = 2816
NFC = DFF // 128
NSLAB = NFC // 2
TB = 1024
NTT = TB // 128
EPS = 1e-6


class Buf:
    __slots__ = ("w", "rs")

    def __init__(self):
        self.w = None
        self.rs = []


class Inst:
    __slots__ = ("eng", "fn", "deps", "sig", "dma", "sem", "val", "prev_dma")

    def __init__(self, eng, fn, dma):
        self.eng = eng
        self.fn = fn
        self.deps = []
        self.sig = False
        self.dma = dma
        self.sem = None
        self.val = None
        self.prev_dma = None


class _Rec:
    def __init__(self):
        self.call = None

    def __getattr__(self, name):
        def f(*a, **k):
            self.call = (name, a, k)
            return self
        return f


class Prog:
    ENGS = ("pe", "act", "dve", "pool", "sp")
    NDMA = 8

    def __init__(self, nc, stack):
        self.nc = nc
        self.q = {e: [] for e in self.ENGS}
        self.esem = {e: stack.enter_context(nc.semaphore("s_" + e)) for e in self.ENGS}
        self.dsem = {e: [stack.enter_context(nc.semaphore("d_%s%d" % (e, i))) for i in range(self.NDMA)]
                     for e in ("act", "pool", "sp")}
        self.ndma = {e: [] for e in ("act", "pool", "sp")}
        self.bufs = {}

    def b(self, *key):
        if key == ("PO", 2):
            key = ("PT2",)
        bb = self.bufs.get(key)
        if bb is None:
            bb = self.bufs[key] = Buf()
        return bb

    def op(self, eng, fn, r=(), w=(), dma=False):
        rec = _Rec()
        fn(rec)
        inst = Inst(eng, rec.call, dma)

        def dep(o, war=False):
            if o is None or o is inst:
                return
            if not dma and not o.dma and o.eng == eng:
                if eng == "pe":
                    return
            if o not in inst.deps:
                inst.deps.append(o)
                o.sig = True

        for bb in r:
            dep(bb.w)
        for bb in w:
            dep(bb.w)
            for o in bb.rs:
                dep(o, war=True)
        for bb in r:
            bb.rs.append(inst)
        for bb in w:
            bb.w = inst
            bb.rs = []
        if dma:
            lst = self.ndma[eng]
            if len(lst) >= self.NDMA:
                inst.prev_dma = lst[len(lst) - self.NDMA]
            inst.sem = self.dsem[eng][len(lst) % self.NDMA]
            inst.val = 16 * (len(lst) // self.NDMA + 1)
            lst.append(inst)
        self.q[eng].append(inst)
        return inst

    def emit(self):
        nc = self.nc
        for e in self.ENGS:
            c = 0
            for inst in self.q[e]:
                if not inst.dma:
                    inst.sem = self.esem[e]
                    if inst.sig:
                        c += 1
                    inst.val = c if inst.sig else None
        engobj = {"pe": "tensor", "act": "scalar", "dve": "vector", "pool": "gpsimd", "sp": "sync"}

        def run(e, eng):
            known = {}
            for inst in self.q[e]:
                waits = {}
                ds = list(inst.deps)
                if inst.prev_dma is not None:
                    ds.append(inst.prev_dma)
                for o in ds:
                    k = id(o.sem)
                    if o.val > known.get(k, 0) and o.val > waits.get(k, (None, 0))[1]:
                        waits[k] = (o.sem, o.val)
                for k, (s, v) in waits.items():
                    eng.wait_ge(s, v)
                    known[k] = v
                name, a, k = inst.fn
                h = getattr(eng, name)(*a, **k)
                if inst.dma:
                    h.then_inc(inst.sem, 16)
                elif inst.sig:
                    h.then_inc(inst.sem, 1)

        with nc.Block() as block:
            for e in self.ENGS:
                if not self.q[e]:
                    continue
                getattr(block, engobj[e])(lambda eng, e=e: run(e, eng))


class K:
    def __init__(self, cfg):
        self.cfg = cfg
        nc = self.nc = bass.Bass("TRN2", target_bir_lowering=False)
        self.stack = contextlib.ExitStack()
        self.P = Prog(nc, self.stack)
        self.dram = {}
        self.gbi = 0
        self.pi = {}

    def din(self, name, shape, dt=F32):
        t = self.nc.dram_tensor(name, list(shape), dt, kind="ExternalInput")
        self.dram[name] = t
        return t

    def sb(self, name, shape, dt):
        return self.stack.enter_context(self.nc.sbuf_tensor(name, list(shape), dt))

    def ps(self, name, shape, dt):
        return self.stack.enter_context(self.nc.psum_tensor(name, list(shape), dt))

    def rot(self, key, n):
        i = self.pi.get(key, 0)
        self.pi[key] = i + 1
        return i % n

    def load_row_bc(self, row):
        P = self.P
        i = self.rot("gb", 2)
        src = self.dram["rows"].ap()[row:row + 1, :].partition_broadcast(128)
        P.op("sp", lambda e: e.dma_start(out=self.GB[i][:, :], in_=src), w=[P.b("GB", i)], dma=True)
        return i

    def rstd_batch(self, ss, n, scale, eps, half=False, key="rs"):
        P = self.P
        bs = P.b(key)
        P.op("act", lambda e: e.activation(out=ss[:, :n], in_=ss[:, :n], func=AF.Sqrt,
                                           bias=self.epsc[eps][:, 0:1], scale=scale), r=[bs, P.b("epsc")], w=[bs])
        P.op("dve", lambda e: e.reciprocal(out=ss[:, :n], in_=ss[:, :n]), r=[bs], w=[bs])
        if half:
            P.op("dve", lambda e: e.tensor_scalar(out=ss[:, :n], in0=ss[:, :n], scalar1=0.5, scalar2=None,
                                                  op0=ALU.mult), r=[bs], w=[bs])

    def _rstd_tile(self, ss, col, scale, eps, key):
        P = self.P
        P.op("act", lambda e: e.activation(out=ss[:, col:col + 1], in_=ss[:, col:col + 1], func=AF.Sqrt,
                                           bias=self.epsc[eps][:, 0:1], scale=scale), r=[key, P.b("epsc")], w=[key])
        P.op("dve", lambda e: e.reciprocal(out=ss[:, col:col + 1], in_=ss[:, col:col + 1]), r=[key], w=[key])

    def _norm_stages(self, gi, tiles=range(NTT)):
        P = self.P
        ss = self.SS
        for tt in tiles:
            key = P.b("rsn", tt)
            P.op("act", lambda e: e.activation(out=self.JUNK[:, :], in_=self.H[:, tt, :], func=AF.Square,
                                               accum_out=ss[:, tt:tt + 1]), r=[P.b("H", tt)], w=[key])
            self._rstd_tile(ss, tt, 1.0 / D, EPS, key)
        for tt in tiles:
            key = P.b("rsn", tt)
            xi = self.rot("xs", 2)
            P.op("dve", lambda e: e.scalar_tensor_tensor(
                out=self.XS[xi][:, :], in0=self.H[:, tt, :], scalar=ss[:, tt:tt + 1], in1=self.GB[gi][:, :],
                op0=ALU.mult, op1=ALU.mult), r=[P.b("H", tt), key, P.b("GB", gi)], w=[P.b("XS", xi)])
            self.transpose_to_XT(self.XS[xi], P.b("XS", xi), tt)

    def norm_T(self, grow):
        if self.norm_done == grow:
            self.norm_done = None
            return
        gi = self.load_row_bc(grow)
        self._norm_stages(gi)

    def post_norm(self, grow, half, src_key="ACC", bias_row=None):
        P = self.P
        gi = self.load_row_bc(grow)
        nrow = self.next_norm_row
        gn = self.load_row_bc(nrow) if nrow is not None else None
        ss = self.SS
        sc, ep = ((4.0 / D, 4 * EPS) if half else (1.0 / D, EPS))
        for tt in range(NTT):
            key = P.b("rsp", tt)
            P.op("act", lambda e: e.activation(out=self.JUNK[:, :], in_=self.ACC[:, tt, :], func=AF.Square,
                                               accum_out=ss[:, 8 + tt:9 + tt]),
                 r=[P.b("ACC", tt, 0), P.b("ACC", tt, 1)], w=[key])
            self._rstd_tile(ss, 8 + tt, sc, ep, key)
        for tt in range(NTT):
            key = P.b("rsp", tt)
            P.op("dve", lambda e: e.scalar_tensor_tensor(
                out=self.ACC[:, tt, :], in0=self.ACC[:, tt, :], scalar=ss[:, 8 + tt:9 + tt], in1=self.GB[gi][:, :],
                op0=ALU.mult, op1=ALU.mult),
                r=[P.b("ACC", tt, 0), P.b("ACC", tt, 1), key, P.b("GB", gi)],
                w=[P.b("ACC", tt, 0), P.b("ACC", tt, 1)])
            P.op("dve" if tt % 2 == 0 else "pool", lambda e: e.tensor_tensor(
                out=self.H[:, tt, :], in0=self.H[:, tt, :], in1=self.ACC[:, tt, :], op=ALU.add),
                r=[P.b("H", tt), P.b("ACC", tt, 0), P.b("ACC", tt, 1)], w=[P.b("H", tt)])
        if gn is not None:
            self._norm_stages(gn)
            self.norm_done = nrow

    def ffn(self, l, a):
        P = self.P
        self.norm_T(l * 6 + (0 if a == 0 else 4))
        win_d = self.dram["ffn_win"].ap()
        wout_d = self.dram["ffn_wout"].ap()

        def load(s):
            wb = self.rot("wslab", 2)
            P.op("pool", lambda e: e.dma_start(out=self.WIN[wb][:, :], in_=win_d[l, a, s, :, :]),
                 w=[P.b("WIN", wb)], dma=True)
            P.op("pool", lambda e: e.dma_start(out=self.WOUT[wb][:, :], in_=wout_d[l, a, s, :, :]),
                 w=[P.b("WOUT", wb)], dma=True)
            return wb

        def phase1(s, wb):
            for tb in range(2):
                for fcl in range(2):
                    gi = self.rot("pgu", 2)
                    for gu, ps in ((0, self.PG[gi]), (1, self.PU[gi])):
                        for kc in range(8):
                            c0 = kc * 512 + fcl * 256 + gu * 128
                            P.op("pe", lambda e, ps=ps, c0=c0, kc=kc, tb=tb: e.matmul(
                                ps[:, :], lhsT=self.WIN[wb][:, c0:c0 + 128],
                                rhs=self.XT[:, kc, tb * 512:(tb + 1) * 512], start=(kc == 0), stop=(kc == 7)),
                                r=[P.b("WIN", wb)] + [P.b("XT", tb * 4 + i) for i in range(4)],
                                w=[P.b("PG" if gu == 0 else "PU", gi)])
                    P.op("act", lambda e, gi=gi: e.activation(out=self.SG[gi][:, :], in_=self.PG[gi][:, :],
                                                              func=AF.Silu),
                         r=[P.b("PG", gi)], w=[P.b("SG", gi)])
                    P.op("dve", lambda e, gi=gi, fcl=fcl, tb=tb: e.tensor_tensor(
                        out=self.ATS[wb][:, fcl, tb * 512:(tb + 1) * 512], in0=self.SG[gi][:, :],
                        in1=self.PU[gi][:, :], op=ALU.mult),
                        r=[P.b("SG", gi), P.b("PU", gi)], w=[P.b("ATS", wb, fcl, tb)])

        def phase2(slabs, first):
            nmm = 2 * len(slabs)
            for tt in range(NTT):
                for nh in range(2):
                    oi = self.rot("po", 2)
                    i = 0
                    for (s, wb) in slabs:
                        for fcl in range(2):
                            P.op("pe", lambda e: e.matmul(
                                self.PO[oi][:, :], lhsT=self.ATS[wb][:, fcl, tt * 128:(tt + 1) * 128],
                                rhs=self.WOUT[wb][:, fcl * 1024 + nh * 512: fcl * 1024 + (nh + 1) * 512],
                                start=(i == 0), stop=(i == nmm - 1)),
                                r=[P.b("ATS", wb, fcl, tt // 4), P.b("WOUT", wb)], w=[P.b("PO", oi)])
                            i += 1
                    if first:
                        P.op("act", lambda e: e.activation(
                            out=self.ACC[:, tt, nh * 512:(nh + 1) * 512], in_=self.PO[oi][:, :], func=AF.Copy),
                            r=[P.b("PO", oi)], w=[P.b("ACC", tt, nh)])
                    else:
                        P.op("dve", lambda e: e.tensor_tensor(
                            out=self.ACC[:, tt, nh * 512:(nh + 1) * 512], in0=self.ACC[:, tt, nh * 512:(nh + 1) * 512],
                            in1=self.PO[oi][:, :], op=ALU.add),
                            r=[P.b("PO", oi), P.b("ACC", tt, nh)], w=[P.b("ACC", tt, nh)])

        s = 0
        while s < NSLAB:
            pair = []
            for ss_ in range(s, min(s + 2, NSLAB)):
                wb = load(ss_)
                phase1(ss_, wb)
                pair.append((ss_, wb))
            phase2(pair, first=(s == 0))
            s += 2
        self.post_norm(l * 6 + (1 if a == 0 else 5), half=True)


    def load_w(self, name, *idx):
        P = self.P
        wb = self.rot("wslab", 2)
        src = self.dram[name].ap()
        for i in idx:
            src = src[i]
        P.op("pool", lambda e: e.dma_start(out=self.WIN[wb][:, :], in_=src), w=[P.b("WIN", wb)], dma=True)
        return wb

    def proj_fm(self, wb, c0, M, tb, ps, pkey):
        P = self.P
        for kc in range(8):
            P.op("pe", lambda e, kc=kc: e.matmul(
                ps[:M, :], lhsT=self.WIN[wb][:, kc * 512 + c0: kc * 512 + c0 + M],
                rhs=self.XT[:, kc, tb * 512:(tb + 1) * 512], start=(kc == 0), stop=(kc == 7)),
                r=[P.b("WIN", wb)] + [P.b("XT", tb * 4 + i) for i in range(4)], w=[pkey])

    def proj_tm(self, wb, c0, N, tt, ps, pkey, xt=None, xkey="XT"):
        P = self.P
        xt = self.XT if xt is None else xt
        for kc in range(8):
            P.op("pe", lambda e, kc=kc: e.matmul(
                ps[:, :N], lhsT=xt[:, kc, tt * 128:(tt + 1) * 128],
                rhs=self.WIN[wb][:, kc * 512 + c0: kc * 512 + c0 + N], start=(kc == 0), stop=(kc == 7)),
                r=[P.b("WIN", wb), P.b(xkey, tt)], w=[pkey])

    def transpose_to_XT(self, src, skey, tt):
        P = self.P
        for kc in range(8):
            P.op("pe", lambda e, kc=kc: e.transpose(
                out=self.PT[:, kc * 128:(kc + 1) * 128], in_=src[:, kc * 128:(kc + 1) * 128],
                identity=self.IDB[:, :]), r=[skey, P.b("IDB")], w=[P.b("PT")])
        P.op("act", lambda e: e.activation(
            out=self.XT[:, :, tt * 128:(tt + 1) * 128],
            in_=self.PT[:, :].rearrange("p (k c) -> p k c", k=8), func=AF.Copy),
            r=[P.b("PT")], w=[P.b("XT", tt)])

    def out_proj(self, wname, widx, brow):
        P = self.P
        wbs = [self.load_w(wname, *widx, nh) for nh in range(2)]
        gi = self.load_row_bc(brow) if brow is not None else None
        for tt in range(NTT):
            for nh in range(2):
                oi = self.rot("po", 2)
                self.proj_tm(wbs[nh], 0, 512, tt, self.PO[oi], P.b("PO", oi))
                if gi is None:
                    P.op("act", lambda e, oi=oi, tt=tt, nh=nh: e.activation(
                        out=self.ACC[:, tt, nh * 512:(nh + 1) * 512], in_=self.PO[oi][:, :], func=AF.Copy),
                        r=[P.b("PO", oi)], w=[P.b("ACC", tt, nh)])
                else:
                    P.op("dve", lambda e, oi=oi, tt=tt, nh=nh: e.tensor_tensor(
                        out=self.ACC[:, tt, nh * 512:(nh + 1) * 512], in0=self.PO[oi][:, :],
                        in1=self.GB[gi][:, nh * 512:(nh + 1) * 512], op=ALU.add),
                        r=[P.b("PO", oi), P.b("GB", gi)], w=[P.b("ACC", tt, nh)])

    def swa(self, l):
        P = self.P
        j = l // 3
        half = self.half
        R0 = self.cfg["row_swa"] + j * 3
        C0 = self.cfg["col_swa"] + j * 12
        self.norm_T(l * 6 + 2)
        P.op("dve", lambda e: e.memset(self.VA[:, :, :], 1.0), w=[P.b("VA", i) for i in range(-1, 8)])
        gs = self.load_row_bc(R0 + 2)
        P.op("act", lambda e: e.activation(out=self.ESK[:, :], in_=self.GB[gs][:, 0:16], func=AF.Exp),
             r=[P.b("GB", gs)], w=[P.b("ESK")])
        P.op("dve", lambda e: e.tensor_scalar(out=self.BQ8[:, :], in0=self.COLS[:, C0:C0 + 8], scalar1=0.125,
                                              scalar2=None, op0=ALU.mult), r=[P.b("COLS")], w=[P.b("BQ8")])
        if half == 1:
            P.op("pool", lambda e: e.tensor_copy(out=self.KT[:, :, 0:128], in_=self.KC[j][:, :, :]),
                 r=[P.b("KC", j)], w=[P.b("KT", -1)])
            P.op("pool", lambda e: e.tensor_copy(out=self.VA[:, 0, :], in_=self.VC[j][:, :]),
                 r=[P.b("VC", j)], w=[P.b("VA", -1)])
        for blk in range(2):
            wb = self.load_w("swa_wq", j, blk)
            for ocl in range(4):
                oc = blk * 4 + ocl
                for tb in range(2):
                    gi = self.rot("pgu", 2)
                    self.proj_fm(wb, ocl * 128, 128, tb, self.PG[gi], P.b("PG", gi))
                    P.op("act", lambda e, gi=gi, oc=oc, tb=tb: e.activation(
                        out=self.QT[:, oc, tb * 512:(tb + 1) * 512], in_=self.PG[gi][:, :], func=AF.Identity,
                        bias=self.BQ8[:, oc:oc + 1], scale=0.125),
                        r=[P.b("PG", gi), P.b("BQ8")], w=[P.b("QT", oc, tb)])
        wb = self.load_w("swa_wk", j)
        for hk in range(4):
            for tb in range(2):
                gi = self.rot("pgu", 2)
                self.proj_fm(wb, hk * 128, 128, tb, self.PU[gi], P.b("PU", gi))
                P.op("act", lambda e, gi=gi, hk=hk, tb=tb: e.activation(
                    out=self.KT[:, hk, 128 + tb * 512: 128 + (tb + 1) * 512], in_=self.PU[gi][:, :], func=AF.Identity,
                    bias=self.COLS[:, C0 + 8 + hk:C0 + 9 + hk], scale=1.0),
                    r=[P.b("PU", gi), P.b("COLS")], w=[P.b("KT", tb * 4 + i) for i in range(4)])
        wb = self.load_w("swa_wv", j)
        gv = self.load_row_bc(R0 + 0)
        for tt in range(NTT):
            oi = self.rot("po", 2)
            self.proj_tm(wb, 0, 256, tt, self.PO[oi], P.b("PO", oi))
            P.op("dve", lambda e, oi=oi, tt=tt: e.tensor_tensor(
                out=self.VA[:, tt + 1, :].rearrange("p (h d) -> p h d", h=4)[:, :, 0:64],
                in0=self.PO[oi][:, 0:256].rearrange("p (h d) -> p h d", h=4),
                in1=self.GB[gv][:, 0:256].rearrange("p (h d) -> p h d", h=4), op=ALU.add),
                r=[P.b("PO", oi), P.b("GB", gv)], w=[P.b("VA", tt)])
        for n in range(NTT):
            has_prev = not (half == 0 and n == 0)
            xi = self.rot("xs", 2)
            for hk in range(4):
                grp = self.rot("swag", 2)
                pss, pname = (self.PG, "PG") if grp == 0 else (self.PU, "PU")
                srcs = [(0, 128 + n * 128, n)]
                if has_prev:
                    srcs.append((256, n * 128, n - 1))
                ncol = 256 * len(srcs)
                ptsi = []
                for hp in range(2):
                    for (co, k0, kblk) in srcs:
                        P.op("pe", lambda e: e.matmul(
                            pss[hp][:, co:co + 256], lhsT=self.KT[hp * 64:(hp + 1) * 64, hk, k0:k0 + 128],
                            rhs=self.QT[hp * 64:(hp + 1) * 64, 2 * hk:2 * hk + 2, n * 128:(n + 1) * 128],
                            start=True, stop=True),
                            r=[P.b("KT", kblk), P.b("QT", 2 * hk, n // 4), P.b("QT", 2 * hk + 1, n // 4)],
                            w=[P.b(pname, hp)])
                    si = self.rot("sg", 2)
                    P.op("act", lambda e: e.activation(out=self.SG[si][:, 0:ncol], in_=pss[hp][:, 0:ncol], func=AF.Exp),
                         r=[P.b(pname, hp)], w=[P.b("SG", si)])
                    pi = self.rot("pts", 4)
                    P.op("dve", lambda e: e.tensor_tensor(
                        out=self.PTS[pi][:, 0:ncol], in0=self.SG[si][:, 0:ncol], in1=self.CMASK[:, 256:256 + ncol], op=ALU.mult),
                        r=[P.b("SG", si), P.b("CMASK")], w=[P.b("PTS", pi)])
                    ptsi.append(pi)
                oi = self.rot("po", 2)
                for hh in range(4):
                    ocl, hp = hh // 2, hh % 2
                    pi = ptsi[hp]
                    for ii, (co, k0, kblk) in enumerate(srcs):
                        P.op("pe", lambda e: e.matmul(
                            self.PO[oi][:, hh * 65:(hh + 1) * 65], lhsT=self.PTS[pi][:, co + ocl * 128:co + (ocl + 1) * 128],
                            rhs=self.VA[:, kblk + 1, hk * 65:(hk + 1) * 65], start=(ii == 0), stop=(ii == len(srcs) - 1)),
                            r=[P.b("PTS", pi), P.b("VA", kblk)], w=[P.b("PO", oi)])
                ov = self.PO[oi][:, 0:260].rearrange("p (h d) -> p h d", h=4)
                P.op("dve", lambda e, ov=ov: e.tensor_tensor(
                    out=self.DEN[:, :], in0=ov[:, :, 64], in1=self.ESK[:, hk * 4:(hk + 1) * 4], op=ALU.add),
                    r=[P.b("PO", oi), P.b("ESK")], w=[P.b("DEN")])
                P.op("dve", lambda e: e.reciprocal(out=self.DEN[:, :], in_=self.DEN[:, :]),
                     r=[P.b("DEN")], w=[P.b("DEN")])
                P.op("dve", lambda e, ov=ov, xi=xi: e.tensor_tensor(
                    out=self.XS[xi][:, hk * 256:(hk + 1) * 256].rearrange("p (h d) -> p h d", h=4),
                    in0=ov[:, :, 0:64], in1=self.DEN[:, :].unsqueeze(2).to_broadcast([128, 4, 64]), op=ALU.mult),
                    r=[P.b("PO", oi), P.b("DEN")], w=[P.b("XS", xi)])
            self.transpose_to_XT(self.XS[xi], P.b("XS", xi), n)
        if half == 0:
            P.op("pool", lambda e: e.tensor_copy(out=self.KC[j][:, :, :], in_=self.KT[:, :, 1024:1152]),
                 r=[P.b("KT", 7)], w=[P.b("KC", j)])
            P.op("pool", lambda e: e.tensor_copy(out=self.VC[j][:, :], in_=self.VA[:, 8, :]),
                 r=[P.b("VA", 7)], w=[P.b("VC", j)])
        self.out_proj("swa_wo", (j,), R0 + 1)
        self.post_norm(l * 6 + 3, half=False)


    def gla(self, l):
        P = self.P
        half = self.half
        R0 = self.cfg["row_gla"]
        C0 = self.cfg["col_gla"]
        DK = 128
        LA = self.ATS[0][:, :, :].rearrange("p a b -> p (a b)").bitcast(F32)
        EB = self.ATS[1][:, :, :].rearrange("p a b -> p (a b)").bitcast(F32)
        ENB = self.WOUT[0][:, :].bitcast(F32)
        kLA, kEB, kENB = [P.b("ATS", 0, a, b) for a in range(2) for b in range(2)], \
            [P.b("ATS", 1, a, b) for a in range(2) for b in range(2)], [P.b("WOUT", 0)]
        ACCb = self.ACC[:, :, :].rearrange("p a b -> p (a b)").bitcast(BF16).rearrange("p (a b) -> p a b", a=NTT)
        self.norm_T(l * 6 + 2)
        if half == 0:
            P.op("dve", lambda e: e.memset(self.GS[:, :, :], 0.0), w=[P.b("GS", h) for h in range(4)])
            P.op("dve", lambda e: e.memset(self.GSB[:, :, :], 0.0), w=[P.b("GSB", h) for h in range(4)])
        P.op("dve", lambda e: e.tensor_scalar(out=self.NBG[:, :], in0=self.COLS[:, C0:C0 + 4], scalar1=-1.0,
                                              scalar2=None, op0=ALU.mult), r=[P.b("COLS")], w=[P.b("NBG")])
        P.op("pool", lambda e: e.dma_start(out=self.SG[0][0:16, :], in_=self.dram["gla_wg2"].ap()[:, :]),
             w=[P.b("SG", 0)], dma=True)
        wb = self.load_w("gla_win", 6)
        for tb in range(2):
            gi = self.rot("pgu", 2)
            self.proj_fm(wb, 0, 16, tb, self.PG[gi], P.b("PG", gi))
            P.op("act", lambda e: e.activation(out=self.XS[0][0:16, tb * 512:(tb + 1) * 512], in_=self.PG[gi][0:16, :],
                                               func=AF.Copy), r=[P.b("PG", gi)], w=[P.b("XS", 0)])
        wq = self.load_w("gla_win", 0)
        wk = self.load_w("gla_win", 1)
        for h in range(4):
            for tb in range(2):
                gi = self.rot("pgu", 2)
                P.op("pe", lambda e: e.matmul(self.PU[gi][:, :], lhsT=self.SG[0][0:16, h * 128:(h + 1) * 128],
                                              rhs=self.XS[0][0:16, tb * 512:(tb + 1) * 512], start=True, stop=True),
                     r=[P.b("SG", 0), P.b("XS", 0)], w=[P.b("PU", gi)])
                P.op("act", lambda e: e.activation(out=EB[:, tb * 512:(tb + 1) * 512], in_=self.PU[gi][:, :], func=AF.Exp,
                                                   bias=self.NBG[:, h:h + 1], scale=-1.0),
                     r=[P.b("PU", gi), P.b("NBG")], w=kEB)
                P.op("act", lambda e: e.activation(out=LA[:, tb * 512:(tb + 1) * 512], in_=EB[:, tb * 512:(tb + 1) * 512],
                                                   func=AF.Ln, bias=self.ONE1[:, 0:1], scale=1.0), r=kEB + [P.b("ONES")], w=kLA)
            for c in range(8):
                P.op("dve", lambda e: e.tensor_tensor_scan(
                    out=LA[:, c * 128:(c + 1) * 128], data0=self.ONES[:, :], data1=LA[:, c * 128:(c + 1) * 128],
                    initial=0.0, op0=ALU.mult, op1=ALU.add), r=kLA + [P.b("ONES")], w=kLA)
            P.op("act", lambda e: e.activation(out=EB[:, :], in_=LA[:, :], func=AF.Exp, scale=-1.0 / 16.0), r=kLA, w=kEB)
            P.op("act", lambda e: e.activation(out=ENB[:, :], in_=LA[:, :], func=AF.Exp, scale=1.0 / 16.0), r=kLA, w=kENB)
            P.op("dve", lambda e: e.tensor_copy(out=self.EBLS[:, h, :],
                                                in_=EB.rearrange("p (c t) -> p c t", c=8)[:, :, 127]),
                 r=kEB, w=[P.b("EBLS", h)])
            for tb in range(2):
                gi = self.rot("pgu", 2)
                self.proj_fm(wq, h * 128, 128, tb, self.PG[gi], P.b("PG", gi))
                P.op("dve", lambda e: e.scalar_tensor_tensor(
                    out=self.QT[:, h, tb * 512:(tb + 1) * 512], in0=self.PG[gi][:, :], scalar=DK ** -0.5,
                    in1=EB[:, tb * 512:(tb + 1) * 512], op0=ALU.mult, op1=ALU.mult),
                    r=[P.b("PG", gi)] + kEB, w=[P.b("QT", h, tb)])
                self.proj_fm(wk, h * 128, 128, tb, self.PU[gi], P.b("PU", gi))
                P.op("dve", lambda e: e.tensor_tensor(
                    out=self.QT[:, 4 + h, tb * 512:(tb + 1) * 512], in0=self.PU[gi][:, :],
                    in1=ENB[:, tb * 512:(tb + 1) * 512], op=ALU.mult),
                    r=[P.b("PU", gi)] + kENB, w=[P.b("QT", 4 + h, tb)])
            P.op("dve", lambda e: e.tensor_tensor(
                out=self.KT[:, h, 0:1024].rearrange("p (c t) -> p c t", c=8),
                in0=self.QT[:, 4 + h, :].rearrange("p (c t) -> p c t", c=8),
                in1=self.EBLS[:, h, :].unsqueeze(2).to_broadcast([128, 8, 128]), op=ALU.mult),
                r=[P.b("QT", 4 + h, 0), P.b("QT", 4 + h, 1), P.b("EBLS", h)], w=[P.b("KT", i) for i in range(-1, 8)])
        for blk in range(2):
            wb = self.load_w("gla_win", 2 + blk)
            for tt in range(NTT):
                oi = self.rot("po", 2)
                self.proj_tm(wb, 0, 512, tt, self.PO[oi], P.b("PO", oi))
                P.op("act", lambda e: e.activation(out=ACCb[:, tt, blk * 512:(blk + 1) * 512], in_=self.PO[oi][:, :],
                                                   func=AF.Copy), r=[P.b("PO", oi)], w=[P.b("ACC", tt, 0)])
        gn = self.load_row_bc(R0 + 0)
        for blk in range(2):
            wb = self.load_w("gla_win", 4 + blk)
            for tt in range(NTT):
                oi = self.rot("po", 2)
                self.proj_tm(wb, 0, 512, tt, self.PO[oi], P.b("PO", oi))
                si = self.rot("sg", 2)
                P.op("act", lambda e: e.activation(out=self.SG[si][:, :], in_=self.PO[oi][:, :], func=AF.Silu),
                     r=[P.b("PO", oi)], w=[P.b("SG", si)])
                P.op("dve", lambda e: e.tensor_tensor(
                    out=ACCb[:, tt, 1024 + blk * 512:1024 + (blk + 1) * 512], in0=self.SG[si][:, :],
                    in1=self.GB[gn][:, blk * 512:(blk + 1) * 512], op=ALU.mult),
                    r=[P.b("SG", si), P.b("GB", gn)], w=[P.b("ACC", tt, 1)])
        for c in range(NTT):
            cs = slice(c * 128, (c + 1) * 128)
            for h in range(4):
                gi = self.rot("pgu", 2)
                P.op("pe", lambda e: e.matmul(self.PG[gi][:, 0:128], lhsT=self.QT[:, 4 + h, cs], rhs=self.QT[:, h, cs],
                                              start=True, stop=True),
                     r=[P.b("QT", 4 + h, c // 4), P.b("QT", h, c // 4)], w=[P.b("PG", gi)])
                pa = self.rot("pts", 4)
                P.op("dve", lambda e: e.tensor_tensor(out=self.PTS[pa][:, 0:128], in0=self.PG[gi][:, 0:128],
                                                      in1=self.CMASK[:, 0:128], op=ALU.mult),
                     r=[P.b("PG", gi), P.b("CMASK")], w=[P.b("PTS", pa)])
                oi = self.rot("po", 2)
                P.op("pe", lambda e: e.matmul(self.PO[oi][:, 0:256], lhsT=self.PTS[pa][:, 0:128],
                                              rhs=ACCb[:, c, h * 256:(h + 1) * 256], start=True, stop=False),
                     r=[P.b("PTS", pa), P.b("ACC", c, 0)], w=[P.b("PO", oi)])
                P.op("pe", lambda e: e.matmul(self.PO[oi][:, 0:256], lhsT=self.QT[:, h, cs], rhs=self.GSB[:, h, :],
                                              start=False, stop=True),
                     r=[P.b("QT", h, c // 4), P.b("GSB", h)], w=[P.b("PO", oi)])
                P.op("pe", lambda e: e.transpose(out=self.PT[:, 0:128], in_=self.KT[:, h, cs], identity=self.IDB[:, :]),
                     r=[P.b("KT", c), P.b("IDB")], w=[P.b("PT")])
                pk = self.rot("pts", 4)
                P.op("act", lambda e: e.activation(out=self.PTS[pk][:, 0:128], in_=self.PT[:, 0:128], func=AF.Copy),
                     r=[P.b("PT")], w=[P.b("PTS", pk)])
                P.op("pe", lambda e: e.matmul(self.PU[gi][:, 0:256], lhsT=self.PTS[pk][:, 0:128],
                                              rhs=ACCb[:, c, h * 256:(h + 1) * 256], start=True, stop=True),
                     r=[P.b("PTS", pk), P.b("ACC", c, 0)], w=[P.b("PU", gi)])
                P.op("dve", lambda e: e.scalar_tensor_tensor(
                    out=self.GS[:, h, :], in0=self.GS[:, h, :], scalar=self.EBLS[:, h, c:c + 1], in1=self.PU[gi][:, 0:256],
                    op0=ALU.mult, op1=ALU.add), r=[P.b("GS", h), P.b("EBLS", h), P.b("PU", gi)], w=[P.b("GS", h)])
                P.op("act", lambda e: e.activation(out=self.GSB[:, h, :], in_=self.GS[:, h, :], func=AF.Copy),
                     r=[P.b("GS", h)], w=[P.b("GSB", h)])
                P.op("act", lambda e: e.activation(out=self.JUNK[:, 0:256], in_=self.PO[oi][:, 0:256], func=AF.Square,
                                                   accum_out=self.SSG[:, h:h + 1]), r=[P.b("PO", oi)], w=[P.b("SSG", h)])
                self._rstd_tile(self.SSG, h, 1.0 / 256.0, 1e-5, P.b("SSG", h))
                P.op("dve", lambda e: e.scalar_tensor_tensor(
                    out=ACCb[:, c, 1024 + h * 256:1024 + (h + 1) * 256], in0=self.PO[oi][:, 0:256],
                    scalar=self.SSG[:, h:h + 1], in1=ACCb[:, c, 1024 + h * 256:1024 + (h + 1) * 256],
                    op0=ALU.mult, op1=ALU.mult), r=[P.b("PO", oi), P.b("SSG", h), P.b("ACC", c, 1)], w=[P.b("ACC", c, 1)])
        for tt in range(NTT):
            self.transpose_to_XT(ACCb[:, tt, 1024:2048], P.b("ACC", tt, 1), tt)
        self.out_proj("gla_wo", (), None)
        self.post_norm(l * 6 + 3, half=False)


    def proj_mix_fm(self, wa, wb_, c0, M, tb, ps, pkey):
        P = self.P
        n = 0
        for (wbuf, xt) in ((wa, self.XT), (wb_, self.XTs)):
            for kc in range(8):
                P.op("pe", lambda e: e.matmul(
                    ps[:M, :], lhsT=self.WIN[wbuf][:, kc * 512 + c0: kc * 512 + c0 + M],
                    rhs=xt[:, kc, tb * 512:(tb + 1) * 512], start=(n == 0), stop=(n == 15)),
                    r=[P.b("WIN", wbuf), P.b("XTC")] + [P.b("XT", tb * 4 + i) for i in range(max(0, -1), 4)]
                    + ([P.b("XT", tb * 4 - 1)] if tb > 0 else []), w=[pkey])
                n += 1

    def load_mix(self, name, blk, mu0, ranges):
        P = self.P
        src = self.dram[name].ap()[blk]
        P.op("pool", lambda e: e.dma_start(out=self.WIN[0][:, :], in_=src), w=[P.b("WIN", 0)], dma=True)
        for (c0, c1, mi) in ranges:
            for kc in range(8):
                P.op("act", lambda e: e.activation(out=self.WIN[1][:, kc * 512 + c0: kc * 512 + c1],
                                                   in_=self.WIN[0][:, kc * 512 + c0: kc * 512 + c1], func=AF.Copy,
                                                   scale=self.COLS[:, mu0 + mi * 8 + kc: mu0 + mi * 8 + kc + 1]),
                     r=[P.b("WIN", 0), P.b("COLS")], w=[P.b("WIN", 1)])
            for kc in range(8):
                P.op("act", lambda e: e.activation(out=self.WIN[0][:, kc * 512 + c0: kc * 512 + c1],
                                                   in_=self.WIN[0][:, kc * 512 + c0: kc * 512 + c1], func=AF.Copy,
                                                   scale=self.OMMU[:, mi * 8 + kc: mi * 8 + kc + 1]),
                     r=[P.b("WIN", 0), P.b("OMMU")], w=[P.b("WIN", 0)])

    def rwkv(self, l):
        P = self.P
        half = self.half
        R0 = self.cfg["row_rwkv"]
        C0 = self.cfg["col_rwkv"]
        CMU, CW0, CA0, CKK, CKA, CRK = C0, C0 + 48, C0 + 56, C0 + 64, C0 + 72, C0 + 80
        c0e = float(np.exp(-0.5))
        f32v = lambda t: t.rearrange("p a b -> p (a b)").bitcast(F32) if len(t.shape) == 3 else t.bitcast(F32)
        T0 = f32v(self.ATS[0][:, :, :]); k0 = [P.b("ATS", 0, a, b) for a in range(2) for b in range(2)]
        T1 = f32v(self.ATS[1][:, :, :]); k1 = [P.b("ATS", 1, a, b) for a in range(2) for b in range(2)]
        T2 = f32v(self.WOUT[0][:, :]); k2 = [P.b("WOUT", 0)]
        T3 = f32v(self.WOUT[1][:, :]); k3 = [P.b("WOUT", 1)]
        KTf = f32v(self.KT[:, :, :])
        T4 = KTf[:, 0:1024]; k4 = [P.b("KT", i) for i in range(-1, 8)]
        T5 = KTf[:, 1024:2048]; k5 = [P.b("KTb")]
        T6 = f32v(self.VA[:, :, :])[:, 0:1024]; k6 = [P.b("VA", i) for i in range(-1, 8)]
        ACCb = self.ACC[:, :, :].rearrange("p a b -> p (a b)").bitcast(BF16).rearrange("p (a b) -> p a b", a=NTT)
        AR = self.QT[:, 0:2, :].rearrange("p a (c t) -> p (a c t)", t=128).rearrange("p (c a t) -> p c a t", a=2, t=128)
        u128 = lambda ap: ap.rearrange("p (u t) -> p u t", t=128)
        u64 = lambda ap: ap.rearrange("p (u t) -> p u t", t=64)
        W0k = self.WIN[0][:, :].rearrange("p (k u t) -> p k u t", k=2, t=128)
        W1k = self.WIN[1][:, :].rearrange("p (k u t) -> p k u t", k=2, t=128)
        LAK, MRK, PN, MRB = W0k[:, 0], W0k[:, 1], W1k[:, 0], W1k[:, 1]
        PTN = u128(self.ATS[0][:, :, :].rearrange("p a b -> p (a b)"))
        XX = u128(self.ATS[1][:, :, :].rearrange("p a b -> p (a b)"))
        ATOK, BHT = u64(self.WOUT[0][:, 0:1024]), u64(self.WOUT[0][:, 1024:2048])
        KHT, W0s = u64(self.WOUT[1][:, 0:1024]), u64(self.WOUT[1][:, 1024:2048])
        KTb = self.KT[:, :, :].rearrange("p a b -> p (a b)")
        U0s, AHs, MTB = u64(KTb[:, 0:1024]), u64(KTb[:, 1024:2048]), u128(KTb[:, 2048:4096])
        VAb = self.VA[:, :, :].rearrange("p a b -> p (a b)")
        DD, SALL = u64(VAb[:, 0:1024]), u64(VAb[:, 1024:1024 + 17 * 64])
        DG = u64(self.QT[:, 6, :])
        c3t = lambda t: t.rearrange("p (c t) -> p c t", t=128)
        kAR = [P.b("QT", 0, 0), P.b("QT", 0, 1), P.b("QT", 1, 0), P.b("QT", 1, 1)]
        KTL = self.QT[:, 2, :]; kKTL = [P.b("QT", 2, 0), P.b("QT", 2, 1)]
        BTL = self.QT[:, 3, :]; kBTL = [P.b("QT", 3, 0), P.b("QT", 3, 1)]
        KH = self.QT[:, 4, :]; kKH = [P.b("QT", 4, 0), P.b("QT", 4, 1)]
        BH = self.QT[:, 5, :]; kBH = [P.b("QT", 5, 0), P.b("QT", 5, 1)]
        PB = self.QT[:, 6, :]; kPB = [P.b("QT", 6, 0), P.b("QT", 6, 1)]
        TMPB = self.QT[:, 7, :]; kTMPB = [P.b("QT", 7, 0), P.b("QT", 7, 1)]
        c3 = lambda t: t.rearrange("p (c t) -> p c t", t=64)

        self.norm_T(l * 6 + 2)
        if half == 0:
            P.op("dve", lambda e: e.memset(self.XTfull[:, :, 7:8], 0.0), w=[P.b("XTC")])
            P.op("dve", lambda e: e.memset(self.STC[:, :, :], 0.0), w=[P.b("STC", i) for i in range(8)])

        else:
            P.op("dve", lambda e: e.tensor_copy(out=self.XTfull[:, :, 7:8], in_=self.XC[:, :].unsqueeze(2)),
                 r=[P.b("XC")], w=[P.b("XTC")])
        P.op("dve", lambda e: e.tensor_scalar(out=self.OMMU[:, :], in0=self.COLS[:, CMU:CMU + 48], scalar1=-1.0,
                                              scalar2=1.0, op0=ALU.mult, op1=ALU.add), r=[P.b("COLS")], w=[P.b("OMMU")])
        P.op("dve", lambda e: e.tensor_scalar(out=self.OMKA[:, :], in0=self.COLS[:, CKA:CKA + 8], scalar1=-1.0,
                                              scalar2=1.0, op0=ALU.mult, op1=ALU.add), r=[P.b("COLS")], w=[P.b("OMKA")])
        self.load_mix("rwkv_wl1", 0, CMU, [(0, 64, 1), (64, 128, 4), (128, 288, 5)])
        for tb in range(2):
            ts = slice(tb * 512, (tb + 1) * 512)
            for (c0_, M, fn, dst, dkey) in ((0, 64, AF.Tanh, self.LW1[0:64, ts], P.b("LW1", tb)),
                                           (64, 64, AF.Copy, self.LA1[0:64, ts], P.b("LA1", tb)),
                                           (128, 128, AF.Sigmoid, self.XS[0][:, ts], P.b("XS", 0)),
                                           (256, 32, AF.Sigmoid, self.XS[1][0:32, ts], P.b("XS", 1))):
                gi = self.rot("pgu", 2)
                self.proj_mix_fm(0, 1, c0_, M, tb, self.PG[gi], P.b("PG", gi))
                P.op("act", lambda e: e.activation(out=dst, in_=self.PG[gi][0:M, :], func=fn),
                     r=[P.b("PG", gi)], w=[dkey])
        P.op("pool", lambda e: e.dma_start(out=self.L2W[0:64, :], in_=self.dram["rwkv_w2"].ap()[:, :]), w=[P.b("L2W")], dma=True)
        P.op("pool", lambda e: e.dma_start(out=self.L2A[0:64, :], in_=self.dram["rwkv_a2"].ap()[:, :]), w=[P.b("L2A")], dma=True)
        for blk in range(2):
            self.load_mix("rwkv_wv", blk, CMU, [(0, 512, 3)])
            for tt in range(NTT):
                oi = self.rot("po", 2)
                n = 0
                for (wbuf, xt) in ((0, self.XT), (1, self.XTs)):
                    for kc in range(8):
                        P.op("pe", lambda e: e.matmul(
                            self.PO[oi][:, :], lhsT=xt[:, kc, tt * 128:(tt + 1) * 128],
                            rhs=self.WIN[wbuf][:, kc * 512:(kc + 1) * 512], start=(n == 0), stop=(n == 15)),
                            r=[P.b("WIN", wbuf), P.b("XT", tt), P.b("XTC")] + ([P.b("XT", tt - 1)] if tt > 0 else []),
                            w=[P.b("PO", oi)])
                        n += 1
                P.op("act", lambda e: e.activation(out=ACCb[:, tt, blk * 512:(blk + 1) * 512], in_=self.PO[oi][:, :],
                                                   func=AF.Copy), r=[P.b("PO", oi)], w=[P.b("ACC", tt, 0)])
        for kc in range(8):
            blk, cc = kc // 4, (kc % 4) * 128
            P.op("pool", lambda e: e.dma_start(out=self.WIN[0][:, 0:2048], in_=self.dram["rwkv_wrk"].ap()[kc]),
                 w=[P.b("WIN", 0)], dma=True)
            for m, mi in ((0, 0), (1, 2)):
                raw = self.WIN[0][:, m * 1024:(m + 1) * 1024].rearrange("p (k c) -> p k c", k=8)
                wbm = self.WIN[0][:, 2048 + m * 1024:2048 + (m + 1) * 1024].rearrange("p (k c) -> p k c", k=8)
                P.op("dve", lambda e: e.tensor_tensor(out=wbm, in0=raw, in1=self.COLS[:, CMU + mi * 8:CMU + mi * 8 + 8].unsqueeze(2).to_broadcast([128, 8, 128]),
                                                      op=ALU.mult), r=[P.b("WIN", 0), P.b("COLS")], w=[P.b("WIN", 0)])
                P.op("dve", lambda e: e.tensor_tensor(out=raw, in0=raw, in1=self.OMMU[:, mi * 8:mi * 8 + 8].unsqueeze(2).to_broadcast([128, 8, 128]),
                                                      op=ALU.mult), r=[P.b("WIN", 0), P.b("OMMU")], w=[P.b("WIN", 0)])
            for m, (Tm, km) in enumerate(((T0, k0), (T1, k1))):
                for tb in range(2):
                    gi = self.rot("pgu", 2)
                    n = 0
                    for (off, xt) in ((m * 1024, self.XT), (2048 + m * 1024, self.XTs)):
                        for kci in range(8):
                            P.op("pe", lambda e: e.matmul(
                                self.PG[gi][:, :], lhsT=self.WIN[0][:, off + kci * 128: off + (kci + 1) * 128],
                                rhs=xt[:, kci, tb * 512:(tb + 1) * 512], start=(n == 0), stop=(n == 15)),
                                r=[P.b("WIN", 0), P.b("XTC")] + [P.b("XT", tb * 4 + i) for i in range(4)]
                                + ([P.b("XT", tb * 4 - 1)] if tb > 0 else []), w=[P.b("PG", gi)])
                            n += 1
                    P.op("act", lambda e: e.activation(out=Tm[:, tb * 512:(tb + 1) * 512], in_=self.PG[gi][:, :], func=AF.Copy),
                         r=[P.b("PG", gi)], w=km)
            for tb in range(2):
                ts = slice(tb * 512, (tb + 1) * 512)
                gi = self.rot("pgu", 2)
                P.op("pe", lambda e: e.matmul(self.PU[gi][:, :], lhsT=self.L2W[0:64, kc * 128:(kc + 1) * 128],
                                              rhs=self.LW1[0:64, ts], start=True, stop=True),
                     r=[P.b("L2W"), P.b("LW1", tb)], w=[P.b("PU", gi)])
                P.op("act", lambda e: e.activation(out=T2[:, ts], in_=self.PU[gi][:, :], func=AF.Sigmoid,
                                                   bias=self.COLS[:, CW0 + kc:CW0 + kc + 1], scale=1.0),
                     r=[P.b("PU", gi), P.b("COLS")], w=k2)
            for c in range(16):
                cs = slice(c * 64, (c + 1) * 64)
                P.op("dve", lambda e: e.tensor_tensor_scan(out=T3[:, cs], data0=self.ONES[:, 0:64], data1=T2[:, cs],
                                                           initial=0.0, op0=ALU.mult, op1=ALU.add),
                     r=k2 + [P.b("ONES")], w=k3)
            P.op("dve", lambda e: e.tensor_tensor(out=T4[:, :], in0=T3[:, :], in1=T2[:, :], op=ALU.subtract),
                 r=k2 + k3, w=k4)
            P.op("act", lambda e: e.activation(out=T4[:, :], in_=T4[:, :], func=AF.Exp, scale=-c0e), r=k4, w=k4)
            P.op("act", lambda e: e.activation(out=T5[:, :], in_=T3[:, :], func=AF.Exp, scale=-c0e), r=k3, w=k5)
            P.op("act", lambda e: e.activation(out=T3[:, :], in_=T3[:, :], func=AF.Exp, scale=c0e), r=k3, w=k3)
            P.op("dve", lambda e: e.tensor_copy(out=self.GCC[:, :], in_=c3(T5)[:, :, 63]), r=k5, w=[P.b("GCC")])
            for tb in range(2):
                ts = slice(tb * 512, (tb + 1) * 512)
                gi = self.rot("pgu", 2)
                P.op("pe", lambda e: e.matmul(self.PU[gi][:, :], lhsT=self.L2A[0:64, kc * 128:(kc + 1) * 128],
                                              rhs=self.LA1[0:64, ts], start=True, stop=True),
                     r=[P.b("L2A"), P.b("LA1", tb)], w=[P.b("PU", gi)])
                P.op("act", lambda e: e.activation(out=T2[:, ts], in_=self.PU[gi][:, :], func=AF.Sigmoid,
                                                   bias=self.COLS[:, CA0 + kc:CA0 + kc + 1], scale=1.0),
                     r=[P.b("PU", gi), P.b("COLS")], w=k2)
            P.op("act", lambda e: e.activation(out=T6[:, :], in_=T1[:, :], func=AF.Copy, scale=self.COLS[:, CKK + kc:CKK + kc + 1]),
                 r=k1 + [P.b("COLS")], w=k6)
            P.op("act", lambda e: e.activation(out=TMPB, in_=T6[:, :], func=AF.Square), r=k6, w=kTMPB)
            for tb in range(2):
                ts = slice(tb * 512, (tb + 1) * 512)
                gi = self.rot("pgu", 2)
                P.op("pe", lambda e: e.matmul(self.PU[gi][:, :], lhsT=self.BLK[:, :], rhs=TMPB[:, ts], start=True, stop=True),
                     r=[P.b("BLK")] + kTMPB, w=[P.b("PU", gi)])
                P.op("act", lambda e: e.activation(out=self.GB[0][:, ts], in_=self.PU[gi][:, :], func=AF.Ln),
                     r=[P.b("PU", gi)], w=[P.b("GB", 0)])
            P.op("act", lambda e: e.activation(out=self.GB[0][:, :], in_=self.GB[0][:, :], func=AF.Exp, scale=-0.5),
                 r=[P.b("GB", 0)], w=[P.b("GB", 0)])
            P.op("dve", lambda e: e.tensor_tensor(out=T6[:, :], in0=T6[:, :], in1=self.GB[0][:, :], op=ALU.mult),
                 r=k6 + [P.b("GB", 0)], w=k6)
            P.op("act", lambda e: e.activation(out=TMPB, in_=T2[:, :], func=AF.Identity, scale=self.COLS[:, CKA + kc:CKA + kc + 1],
                                               bias=self.OMKA[:, kc:kc + 1]),
                 r=k2 + [P.b("COLS"), P.b("OMKA")], w=kTMPB)
            P.op("dve", lambda e: e.tensor_tensor(out=T1[:, :], in0=T1[:, :], in1=TMPB, op=ALU.mult), r=k1 + kTMPB, w=k1)
            P.op("dve", lambda e: e.scalar_tensor_tensor(out=AR[:, :, 0, :], in0=c3t(T6), scalar=-1.0, in1=c3t(T4),
                                                         op0=ALU.mult, op1=ALU.mult), r=k6 + k4, w=kAR)
            P.op("dve", lambda e: e.tensor_tensor(out=AR[:, :, 1, :], in0=c3t(T0), in1=c3t(T5), op=ALU.mult), r=k0 + k5, w=kAR)
            P.op("dve", lambda e: e.tensor_tensor(out=KTL, in0=T1[:, :], in1=T3[:, :], op=ALU.mult), r=k1 + k3, w=kKTL)
            P.op("dve", lambda e: e.tensor_tensor(out=T6[:, :], in0=T6[:, :], in1=T2[:, :], op=ALU.mult), r=k6 + k2, w=k6)
            P.op("dve", lambda e: e.tensor_tensor(out=BTL, in0=T6[:, :], in1=T3[:, :], op=ALU.mult), r=k6 + k3, w=kBTL)
            gcb = self.GCC[:, :].unsqueeze(2).to_broadcast([128, 16, 64])
            P.op("dve", lambda e: e.tensor_tensor(out=c3(KH), in0=c3(KTL), in1=gcb, op=ALU.mult), r=kKTL + [P.b("GCC")], w=kKH)
            P.op("dve", lambda e: e.tensor_tensor(out=c3(BH), in0=c3(BTL), in1=gcb, op=ALU.mult), r=kBTL + [P.b("GCC")], w=kBH)
            P.op("dve", lambda e: e.scalar_tensor_tensor(out=PB, in0=T0[:, :], scalar=self.COLS[:, CRK + kc:CRK + kc + 1],
                                                         in1=T1[:, :], op0=ALU.mult, op1=ALU.mult),
                 r=k0 + k1 + [P.b("COLS")], w=kPB)
            gi = self.rot("pgu", 2)
            for tt in range(NTT):
                P.op("pe", lambda e: e.matmul(self.PU[gi][:, tt * 2:tt * 2 + 2], lhsT=PB[:, tt * 128:(tt + 1) * 128],
                                              rhs=self.HSEL[:, :], start=True, stop=True),
                     r=kPB + [P.b("HSEL")], w=[P.b("PU", gi)])
            P.op("act", lambda e: e.activation(out=self.BS[:, :, 2 * kc:2 * kc + 2],
                                               in_=self.PU[gi][:, 0:16].rearrange("p (t h) -> p t h", h=2), func=AF.Copy),
                 r=[P.b("PU", gi)], w=[P.b("BS", kc)])
            prs = [slice(0, 64), slice(64, 128)]
            KB = lambda kind, bt: P.b("RK", kind, bt)
            kall = lambda kind: [P.b("RK", kind, bt) for bt in range(4)]
            alias = k0 + k1 + k2 + k3 + k4 + k5 + k6 + kPB + [P.b("WIN", 0), P.b("WIN", 1)]
            rkk = [P.b("RK", kd, bt) for kd in ("LAK", "MRK", "PN", "MRB", "PTN", "XX", "ATOK", "BHT", "KHT", "W0", "U0", "AH", "MTB")
                   for bt in range(4)] + [P.b("DD"), P.b("DG")] + [P.b("SALL", i) for i in range(17)]
            P.op("pool", lambda e: e.memset(self.SEMT[:, 0:1], 0.0), w=alias + rkk + [P.b("SEMT")])
            P.op("pool", lambda e: e.memset(MTB[:, :, :], 0.0), w=kall("MTB"))
            P.op("pool", lambda e: e.tensor_tensor(out=DG, in0=self.ID2[:, :].unsqueeze(1).to_broadcast([128, 16, 64]),
                                                   in1=self.GCC[:, :].unsqueeze(2).to_broadcast([128, 16, 64]), op=ALU.mult),
                 r=[P.b("GCC"), P.b("RMASK")], w=[P.b("DG")])
            P.op("act", lambda e: e.activation(out=SALL[:, 0, :], in_=self.STC[:, kc, :], func=AF.Copy),
                 r=[P.b("STC", kc)], w=[P.b("SALL", 0)])
            for cp in range(8):
                tl = slice(cp * 128, (cp + 1) * 128)
                for hp in range(2):
                    pr = prs[hp]
                    u = cp * 2 + hp
                    bt = u // 4
                    for (src, off, key) in ((KTL, 0, kKTL), (BTL, 256, kBTL)):
                        P.op("pe", lambda e: e.matmul(self.PO[hp][:, off:off + 256], lhsT=src[pr, tl],
                                                      rhs=AR[pr, cp, :, :], start=True, stop=True),
                             r=key + kAR, w=[P.b("PO", hp)])
                    P.op("pe", lambda e: e.matmul(self.PU[hp][:, 0:128], lhsT=AR[pr, cp, 0, :], rhs=BTL[pr, tl],
                                                  start=True, stop=True), r=kAR + kBTL, w=[P.b("PU", hp)])
                    m2 = self.RMASK[:, 0:256].rearrange("p (a t) -> p a t", a=2)
                    P.op("dve", lambda e: e.tensor_tensor(out=W0k[:, :, u, :], in0=self.PO[hp][:, 0:256].rearrange("p (a t) -> p a t", a=2),
                                                          in1=m2, op=ALU.mult),
                         r=[P.b("PO", hp), P.b("RMASK")], w=[KB("LAK", bt), KB("MRK", bt)])
                    P.op("dve", lambda e: e.tensor_tensor(out=W1k[:, :, u, :], in0=self.PO[hp][:, 256:512].rearrange("p (a t) -> p a t", a=2),
                                                          in1=m2, op=ALU.mult),
                         r=[P.b("PO", hp), P.b("RMASK")], w=[KB("PN", bt), KB("MRB", bt)])
                    P.op("dve", lambda e: e.tensor_tensor(out=PTN[:, u, :], in0=self.PU[hp][:, 0:128], in1=self.RMASK[:, 256:384], op=ALU.mult),
                         r=[P.b("PU", hp), P.b("RMASK")], w=[KB("PTN", bt)])
            for bt in range(4):
                P.op("pool", lambda e: e.tensor_tensor(out=XX[:, bt * 4:(bt + 1) * 4, :], in0=PN[:, bt * 4:(bt + 1) * 4, :],
                                                       in1=self.IDB[:, :].unsqueeze(1).to_broadcast([128, 4, 128]), op=ALU.add),
                     r=[KB("PN", bt), P.b("IDB")], w=[KB("XX", bt)])
            for (dst, dkind, srcf, skey) in ((ATOK, "ATOK", lambda cp, pr: AR[pr, cp, 0, :], kAR),
                                             (BHT, "BHT", lambda cp, pr: BH[pr, cp * 128:(cp + 1) * 128], kBH),
                                             (KHT, "KHT", lambda cp, pr: KH[pr, cp * 128:(cp + 1) * 128], kKH)):
                for u in range(16):
                    cp, hp = u // 2, u % 2
                    pt = self.PT if hp == 0 else self.PT2
                    P.op("pe", lambda e: e.transpose(out=pt[:, cp * 64:(cp + 1) * 64], in_=srcf(cp, prs[hp]),
                                                     identity=self.IDB[prs[hp], hp * 64:(hp + 1) * 64]),
                         r=skey + [P.b("IDB")], w=[P.b("PT" if hp == 0 else "PT2")])
                for hp in range(2):
                    pt = self.PT if hp == 0 else self.PT2
                    P.op("act" if hp == 0 else "dve", lambda e: (e.activation(
                        out=dst[:, hp:16:2, :], in_=pt[:, 0:512].rearrange("p (u t) -> p u t", t=64), func=AF.Copy) if hp == 0 else
                        e.tensor_copy(out=dst[:, hp:16:2, :], in_=pt[:, 0:512].rearrange("p (u t) -> p u t", t=64))),
                        r=[P.b("PT" if hp == 0 else "PT2")], w=kall(dkind))
            for m in range(5):
                for bt in range(4):
                    us = range(bt * 4, bt * 4 + 4)
                    pb = bt % 2
                    for i, u in enumerate(us):
                        P.op("pe", lambda e: e.matmul(self.PG[pb][:, i * 128:(i + 1) * 128], lhsT=PN[:, u, :], rhs=PTN[:, u, :],
                                                      start=True, stop=True), r=[KB("PN", bt), KB("PTN", bt)], w=[P.b("PG", pb)])
                    if m < 4:
                        for i, u in enumerate(us):
                            P.op("pe", lambda e: e.matmul(self.PU[pb][:, i * 128:(i + 1) * 128], lhsT=PTN[:, u, :], rhs=PN[:, u, :],
                                                          start=True, stop=True), r=[KB("PN", bt), KB("PTN", bt)], w=[P.b("PU", pb)])
                    P.op("act", lambda e: e.activation(out=PTN[:, bt * 4:(bt + 1) * 4, :],
                                                       in_=self.PG[pb][:, :].rearrange("p (u t) -> p u t", t=128), func=AF.Copy),
                         r=[P.b("PG", pb)], w=[KB("PTN", bt)])
                    if m < 4:
                        P.op("dve", lambda e: e.tensor_copy(out=PN[:, bt * 4:(bt + 1) * 4, :],
                                                            in_=self.PU[pb][:, :].rearrange("p (u t) -> p u t", t=128)),
                             r=[P.b("PU", pb)], w=[KB("PN", bt)])
                    for i, u in enumerate(us):
                        P.op("pe", lambda e: e.matmul(self.PO[pb][:, i * 128:(i + 1) * 128], lhsT=PTN[:, u, :], rhs=XX[:, u, :],
                                                      start=True, stop=True), r=[KB("PTN", bt), KB("XX", bt)], w=[P.b("PO", pb)])
                    P.op("dve", lambda e: e.tensor_tensor(out=XX[:, bt * 4:(bt + 1) * 4, :], in0=XX[:, bt * 4:(bt + 1) * 4, :],
                                                          in1=self.PO[pb][:, :].rearrange("p (u t) -> p u t", t=128), op=ALU.add),
                         r=[KB("XX", bt), P.b("PO", pb)], w=[KB("XX", bt)])
            vcol = lambda u: ACCb[:, u // 2, (2 * kc + u % 2) * 64:(2 * kc + u % 2 + 1) * 64]
            for b8 in range(2):
                for i in range(8):
                    u = b8 * 8 + i
                    P.op("pe", lambda e: e.matmul(self.PG[b8][:, i * 64:(i + 1) * 64], lhsT=LAK[:, u, :], rhs=vcol(u), start=True, stop=True),
                         r=[KB("LAK", u // 4), P.b("ACC", u // 2, 0)], w=[P.b("PG", b8)])
                P.op("act", lambda e: e.activation(out=W0s[:, b8 * 8:(b8 + 1) * 8, :], in_=self.PG[b8][:, :].rearrange("p (u t) -> p u t", t=64),
                                                   func=AF.Copy), r=[P.b("PG", b8)], w=[KB("W0", 2 * b8), KB("W0", 2 * b8 + 1)])
            for b8 in range(2):
                for i in range(8):
                    u = b8 * 8 + i
                    P.op("pe", lambda e: e.matmul(self.PO[b8][:, i * 64:(i + 1) * 64], lhsT=XX[:, u, :], rhs=ATOK[:, u, :], start=True, stop=True),
                         r=[KB("XX", u // 4), KB("ATOK", u // 4)], w=[P.b("PO", b8)])
                P.op("dve", lambda e: e.tensor_copy(out=AHs[:, b8 * 8:(b8 + 1) * 8, :], in_=self.PO[b8][:, :].rearrange("p (u t) -> p u t", t=64)),
                     r=[P.b("PO", b8)], w=[KB("AH", 2 * b8), KB("AH", 2 * b8 + 1)])
            for b8 in range(2):
                for i in range(8):
                    u = b8 * 8 + i
                    P.op("pe", lambda e: e.matmul(self.PU[b8][:, i * 64:(i + 1) * 64], lhsT=XX[:, u, :], rhs=W0s[:, u, :], start=True, stop=True),
                         r=[KB("XX", u // 4), KB("W0", u // 4)], w=[P.b("PU", b8)])
                P.op("act", lambda e: e.activation(out=U0s[:, b8 * 8:(b8 + 1) * 8, :], in_=self.PU[b8][:, :].rearrange("p (u t) -> p u t", t=64),
                                                   func=AF.Copy), r=[P.b("PU", b8)], w=[KB("U0", 2 * b8), KB("U0", 2 * b8 + 1)])
            for cpar in range(2):
                tp = prs[cpar]
                for u in range(16):
                    cp, hp = u // 2, u % 2
                    P.op("pe", lambda e: e.matmul(self.PG[cpar][prs[hp], cp * 64:(cp + 1) * 64], lhsT=AHs[tp, u, :], rhs=BHT[tp, u, :],
                                                  start=True, stop=True), r=[KB("AH", u // 4), KB("BHT", u // 4)], w=[P.b("PG", cpar)])
                for hp in range(2):
                    P.op("dve", lambda e: e.tensor_tensor(
                        out=MTB[prs[hp], cpar:16:2, hp * 64:(hp + 1) * 64],
                        in0=self.PG[cpar][prs[hp], :].rearrange("p (c t) -> p c t", t=64),
                        in1=DG[prs[hp], cpar:16:2, :], op=ALU.add),
                        r=[P.b("PG", cpar), P.b("DG")], w=kall("MTB"))
                for u in range(16):
                    cp, hp = u // 2, u % 2
                    P.op("pe", lambda e: e.matmul(self.PU[cpar][prs[hp], cp * 64:(cp + 1) * 64], lhsT=BHT[tp, u, :], rhs=U0s[tp, u, :],
                                                  start=True, stop=False), r=[KB("BHT", u // 4), KB("U0", u // 4)], w=[P.b("PU", cpar)])
                    P.op("pe", lambda e: e.matmul(self.PU[cpar][prs[hp], cp * 64:(cp + 1) * 64], lhsT=KHT[tp, u, :],
                                                  rhs=ACCb[tp, cp, (2 * kc + hp) * 64:(2 * kc + hp + 1) * 64],
                                                  start=False, stop=True), r=[KB("KHT", u // 4), P.b("ACC", cp, 0)], w=[P.b("PU", cpar)])
                P.op("act", lambda e: e.activation(out=DD[:, cpar:16:2, :], in_=self.PU[cpar][:, :].rearrange("p (c t) -> p c t", t=64),
                                                   func=AF.Copy), r=[P.b("PU", cpar)], w=[P.b("DD")])
            for b4 in range(2):
                for i in range(4):
                    cp = b4 * 4 + i
                    for hp in range(2):
                        u = cp * 2 + hp
                        P.op("pe", lambda e: e.matmul(self.PO[b4][prs[hp], i * 128:(i + 1) * 128], lhsT=AHs[:, u, :], rhs=MRB[:, u, :],
                                                      start=True, stop=True), r=[KB("AH", u // 4), KB("MRB", u // 4)], w=[P.b("PO", b4)])
                P.op("dve", lambda e: e.tensor_tensor(out=AR[:, b4 * 4:(b4 + 1) * 4, 1, :], in0=AR[:, b4 * 4:(b4 + 1) * 4, 1, :],
                                                      in1=self.PO[b4][:, :].rearrange("p (c t) -> p c t", t=128), op=ALU.add),
                     r=kAR + [P.b("PO", b4)], w=kAR)
            for c in range(16):
                pb = c % 2
                P.op("pe", lambda e: e.matmul(self.PG[pb][:, 0:64], lhsT=MTB[:, c, :], rhs=SALL[:, c, :], start=True, stop=True),
                     r=kall("MTB") + [P.b("SALL", c)], w=[P.b("PG", pb)])
                P.op("dve", lambda e: e.tensor_tensor(out=SALL[:, c + 1, :], in0=self.PG[pb][:, 0:64], in1=DD[:, c, :], op=ALU.add),
                     r=[P.b("PG", pb), P.b("DD")], w=[P.b("SALL", c + 1)])
            P.op("act", lambda e: e.activation(out=self.STC[:, kc, :], in_=SALL[:, 16, :], func=AF.Copy),
                 r=[P.b("SALL", 16)], w=[P.b("STC", kc)])
            for hp in range(2):
                pr = prs[hp]
                for cp in range(8):
                    u = cp * 2 + hp
                    oc = slice(cp * 64, (cp + 1) * 64)
                    P.op("pe", lambda e: e.matmul(self.PO[hp][:, oc], lhsT=MRB[:, u, :], rhs=U0s[:, u, :], start=True, stop=False),
                         r=[KB("MRB", u // 4), KB("U0", u // 4)], w=[P.b("PO", hp)])
                    P.op("pe", lambda e: e.matmul(self.PO[hp][:, oc], lhsT=MRK[:, u, :], rhs=vcol(u), start=False, stop=False),
                         r=[KB("MRK", u // 4), P.b("ACC", cp, 0)], w=[P.b("PO", hp)])
                    for cpar in range(2):
                        c = 2 * cp + cpar
                        P.op("pe", lambda e: e.matmul(self.PO[hp][prs[cpar], oc], lhsT=AR[pr, cp, 1, cpar * 64:(cpar + 1) * 64],
                                                      rhs=SALL[pr, c, :], start=False, stop=True),
                             r=kAR + [P.b("SALL", c)], w=[P.b("PO", hp)])
                hd = 2 * kc + hp
                P.op("act", lambda e: e.activation(out=ACCb[:, :, 1024 + hd * 64:1024 + (hd + 1) * 64],
                                                   in_=self.PO[hp][:, :].rearrange("p (c t) -> p c t", t=64), func=AF.Copy),
                     r=[P.b("PO", hp)], w=[P.b("ACC", tt, 1) for tt in range(8)])
            P.op("pool", lambda e: e.memset(self.SEMT[:, 0:1], 0.0), w=alias + rkk + [P.b("SEMT")])
        if half == 0:
            P.op("dve", lambda e: e.tensor_copy(out=self.XC[:, :].unsqueeze(2), in_=self.XTfull[:, :, 8 + TB - 1:8 + TB]),
                 r=[P.b("XT", 7)], w=[P.b("XC")])
        g1 = self.load_row_bc(R0 + 0)
        g2 = self.load_row_bc(R0 + 1)
        h3 = lambda t: t.rearrange("p (h d) -> p h d", d=64)
        for c in range(NTT):
            yb = ACCb[:, c, 1024:2048]
            ky = [P.b("ACC", c, 1)]
            P.op("dve", lambda e: e.tensor_reduce(out=self.S1[:, :], in_=h3(yb), axis=mybir.AxisListType.X, op=ALU.add),
                 r=ky, w=[P.b("S1")])
            P.op("dve", lambda e: e.tensor_tensor(out=T0[:, :], in0=yb, in1=yb, op=ALU.mult), r=ky, w=k0)
            P.op("dve", lambda e: e.tensor_reduce(out=self.S2[:, :], in_=h3(T0[:, :]), axis=mybir.AxisListType.X, op=ALU.add),
                 r=k0, w=[P.b("S2")])
            P.op("dve", lambda e: e.tensor_scalar(out=self.S1[:, :], in0=self.S1[:, :], scalar1=1.0 / 64, scalar2=None,
                                                  op0=ALU.mult), r=[P.b("S1")], w=[P.b("S1")])
            P.op("dve", lambda e: e.tensor_tensor(out=self.S3[:, :], in0=self.S1[:, :], in1=self.S1[:, :], op=ALU.mult),
                 r=[P.b("S1")], w=[P.b("S3")])
            P.op("dve", lambda e: e.scalar_tensor_tensor(out=self.S2[:, :], in0=self.S2[:, :], scalar=1.0 / 64, in1=self.S3[:, :],
                                                         op0=ALU.mult, op1=ALU.subtract), r=[P.b("S2"), P.b("S3")], w=[P.b("S2")])
            self.rstd_batch(self.S2, 16, 1.0, 64e-5, key="S2")
            P.op("dve", lambda e: e.tensor_tensor(out=h3(T0[:, :]), in0=h3(yb), in1=self.S1[:, :].unsqueeze(2).to_broadcast([128, 16, 64]),
                                                  op=ALU.subtract), r=ky + [P.b("S1")], w=k0)
            P.op("dve", lambda e: e.tensor_tensor(out=h3(T0[:, :]), in0=h3(T0[:, :]), in1=self.S2[:, :].unsqueeze(2).to_broadcast([128, 16, 64]),
                                                  op=ALU.mult), r=k0 + [P.b("S2")], w=k0)
            P.op("dve", lambda e: e.tensor_tensor(out=T0[:, :], in0=T0[:, :], in1=self.GB[g1][:, :], op=ALU.mult),
                 r=k0 + [P.b("GB", g1)], w=k0)
            P.op("dve", lambda e: e.tensor_tensor(out=T0[:, :], in0=T0[:, :], in1=self.GB[g2][:, :], op=ALU.add),
                 r=k0 + [P.b("GB", g2)], w=k0)
            P.op("dve", lambda e: e.tensor_tensor(out=h3(T1[:, :]), in0=h3(ACCb[:, c, 0:1024]),
                                                  in1=self.BS[:, c, :].unsqueeze(2).to_broadcast([128, 16, 64]), op=ALU.mult),
                 r=[P.b("ACC", c, 0)] + [P.b("BS", i) for i in range(8)], w=k1)
            P.op("dve", lambda e: e.tensor_tensor(out=yb, in0=T0[:, :], in1=T1[:, :], op=ALU.add), r=k0 + k1, w=ky)
        P.op("pool", lambda e: e.dma_start(out=self.L2W[:, :], in_=self.dram["rwkv_g2"].ap()[0:128, :]), w=[P.b("L2W")], dma=True)
        P.op("pool", lambda e: e.dma_start(out=self.L2A[0:32, :], in_=self.dram["rwkv_g2"].ap()[128:160, :]), w=[P.b("L2A")], dma=True)
        for tt in range(NTT):
            tsl = slice(tt * 128, (tt + 1) * 128)
            for kc in range(8):
                P.op("pe", lambda e: e.transpose(out=self.PT[:, kc * 128:(kc + 1) * 128], in_=ACCb[:, tt, 1024 + kc * 128:1024 + (kc + 1) * 128],
                                                 identity=self.IDB[:, :]), r=[P.b("ACC", tt, 1), P.b("IDB")], w=[P.b("PT")])
            for kc in range(8):
                pg = self.PG[kc // 4]
                P.op("pe", lambda e: e.matmul(pg[:, (kc % 4) * 128:(kc % 4 + 1) * 128], lhsT=self.L2W[:, kc * 128:(kc + 1) * 128],
                                              rhs=self.XS[0][:, tsl], start=True, stop=False),
                     r=[P.b("L2W"), P.b("XS", 0)], w=[P.b("PG", kc // 4)])
                P.op("pe", lambda e: e.matmul(pg[:, (kc % 4) * 128:(kc % 4 + 1) * 128], lhsT=self.L2A[0:32, kc * 128:(kc + 1) * 128],
                                              rhs=self.XS[1][0:32, tsl], start=False, stop=True),
                     r=[P.b("L2A"), P.b("XS", 1)], w=[P.b("PG", kc // 4)])
            for hh in range(2):
                P.op("act", lambda e: e.activation(out=TMPB[:, hh * 512:(hh + 1) * 512], in_=self.PG[hh][:, :], func=AF.Copy),
                     r=[P.b("PG", hh)], w=kTMPB)
            P.op("dve", lambda e: e.tensor_tensor(out=self.XT[:, :, tsl], in0=self.PT[:, :].rearrange("p (k c) -> p k c", k=8),
                                                  in1=TMPB.rearrange("p (k c) -> p k c", k=8), op=ALU.mult),
                 r=[P.b("PT")] + kTMPB, w=[P.b("XT", tt)])
        self.out_proj("rwkv_wo", (), None)
        self.post_norm(l * 6 + 3, half=False)

    def build(self):
        nc, P, cfg = self.nc, self.P, self.cfg
        x_d = self.din("x", [SEQ, D])
        self.din("rows", [cfg["nrows"], D])
        self.din("ffn_win", [DEPTH, 2, NSLAB, 128, 8 * 512])
        self.din("ffn_wout", [DEPTH, 2, NSLAB, 128, 2 * 1024])
        self.din("idb", [128, 128], BF16)
        self.din("cmask", [128, 1024], BF16)
        self.din("cols", [128, cfg["ncols"]])
        self.din("swa_wq", [2, 2, 128, 4096])
        self.din("gla_win", [7, 128, 4096])
        self.din("rwkv_wl1", [1, 128, 4096])
        self.din("rwkv_wrk", [8, 128, 2048])
        self.din("rwkv_wv", [2, 128, 4096])
        self.din("rwkv_wo", [2, 128, 4096])
        self.din("rwkv_w2", [64, 1024])
        self.din("rwkv_a2", [64, 1024])
        self.din("rwkv_g2", [160, 1024])
        self.din("rconst", [128, 384 + 128 + 2 + 64], BF16)
        self.din("gla_wo", [2, 128, 4096])
        self.din("gla_wg2", [16, 512])
        self.din("swa_wk", [2, 128, 4096])
        self.din("swa_wv", [2, 128, 4096])
        self.din("swa_wo", [2, 2, 128, 4096])
        y_d = self.nc.dram_tensor("y", [SEQ, D], F32, kind="ExternalOutput")

        self.H = self.sb("H", [128, NTT, D], F32)
        self.XTfull = self.sb("XTfull", [128, 8, TB + 8], BF16)
        self.XT = self.XTfull[:, :, 8:8 + TB]
        self.XTs = self.XTfull[:, :, 7:7 + TB]
        self.RCONST = self.sb("RCONST", [128, 384 + 128 + 2 + 64], BF16)
        self.RMASK = self.RCONST[:, 0:384]
        self.BLK = self.RCONST[:, 384:512]
        self.HSEL = self.RCONST[:, 512:514]
        self.ID2 = self.RCONST[:, 514:578]
        self.STC = self.sb("STC", [128, 8, 64], BF16)
        self.SEMT = self.sb("SEMT", [128, 4], F32)
        self.XC = self.sb("XC", [128, 8], BF16)
        self.OMMU = self.sb("OMMU", [128, 48], F32)
        self.OMKA = self.sb("OMKA", [128, 8], F32)
        self.GCC = self.sb("GCC", [128, 16], F32)
        self.BS = self.sb("BS", [128, 8, 16], F32)
        self.S1 = self.sb("S1", [128, 16], F32)
        self.S2 = self.sb("S2", [128, 16], F32)
        self.S3 = self.sb("S3", [128, 16], F32)
        self.LW1 = self.sb("LW1", [64, TB], BF16)
        self.LA1 = self.sb("LA1", [64, TB], BF16)
        self.L2W = self.sb("L2W", [128, D], BF16)
        self.L2A = self.sb("L2A", [64, D], BF16)
        self.ACC = self.sb("ACC", [128, NTT, D], F32)
        self.ATS = [self.sb("ATS%d" % i, [128, 2, TB], BF16) for i in range(2)]
        self.WIN = [self.sb("WIN%d" % i, [128, 8 * 512], BF16) for i in range(2)]
        self.WOUT = [self.sb("WOUT%d" % i, [128, 2 * 1024], BF16) for i in range(2)]
        self.GB = [self.sb("GB%d" % i, [128, D], F32) for i in range(2)]
        self.XS = [self.sb("XS%d" % i, [128, D], BF16) for i in range(2)]
        self.SG = [self.sb("SG%d" % i, [128, 512], BF16) for i in range(2)]
        self.JUNK = self.sb("JUNK", [128, D], BF16)
        self.SS = self.sb("SS", [128, 16], F32)
        self.IDB = self.sb("IDB", [128, 128], BF16)
        self.epsc = {EPS: self.sb("eps0", [128, 1], F32), 4 * EPS: self.sb("eps4", [128, 1], F32), 1e-5: self.sb("eps1", [128, 1], F32),
                     64e-5: self.sb("eps2", [128, 1], F32)}
        self.GS = self.sb("GS", [128, 4, 256], F32)
        self.GSB = self.sb("GSB", [128, 4, 256], BF16)
        self.EBLS = self.sb("EBLS", [128, 4, 8], F32)
        self.ONES = self.sb("ONES", [128, 128], F32)
        self.ONE1 = self.ONES
        self.NBG = self.sb("NBG", [128, 4], F32)
        self.SSG = self.sb("SSG", [128, 4], F32)
        self.CMASK = self.sb("CMASK", [128, 1024], BF16)
        self.COLS = self.sb("COLS", [128, cfg["ncols"]], F32)
        self.QT = self.sb("QT", [128, 8, TB], BF16)
        self.KT = self.sb("KT", [128, 4, TB + 128], BF16)
        self.VA = self.sb("VA", [128, 9, 260], BF16)
        self.KC = [self.sb("KC%d" % i, [128, 4, 128], BF16) for i in range(2)]
        self.VC = [self.sb("VC%d" % i, [128, 260], BF16) for i in range(2)]
        self.PTS = [self.sb("PTS%d" % i, [128, 512], BF16) for i in range(4)]
        self.ESK = self.sb("ESK", [128, 16], F32)
        self.BQ8 = self.sb("BQ8", [128, 8], F32)
        self.DEN = self.sb("DEN", [128, 4], F32)

        self.PG = [self.ps("PG%d" % i, [128, 512], F32) for i in range(2)]
        self.PU = [self.ps("PU%d" % i, [128, 512], F32) for i in range(2)]
        self.PO = [self.ps("PO%d" % i, [128, 512], F32) for i in range(2)]
        self.PT = self.ps("PT", [128, 1024], BF16)
        self.PT2 = self.ps("PT2", [128, 1024], BF16)

        P.op("sp", lambda e: e.dma_start(out=self.IDB[:, :], in_=self.dram["idb"].ap()[:, :]), w=[P.b("IDB")], dma=True)
        P.op("sp", lambda e: e.dma_start(out=self.CMASK[:, :], in_=self.dram["cmask"].ap()[:, :]), w=[P.b("CMASK")], dma=True)
        P.op("sp", lambda e: e.dma_start(out=self.COLS[:, :], in_=self.dram["cols"].ap()[:, :]), w=[P.b("COLS")], dma=True)
        P.op("dve", lambda e: e.memset(self.VA[:, :, :], 1.0), w=[P.b("VA", i) for i in range(-1, 8)])
        P.op("dve", lambda e: e.memset(self.ONES[:, :], 1.0), w=[P.b("ONES")])
        P.op("sp", lambda e: e.dma_start(out=self.RCONST[:, :], in_=self.dram["rconst"].ap()[:, :]),
             w=[P.b("RMASK"), P.b("BLK"), P.b("HSEL")], dma=True)
        for eps, t in self.epsc.items():
            P.op("dve", lambda e, t=t, eps=eps: e.memset(t[:, :], eps), w=[P.b("epsc")])

        xv = x_d.ap().rearrange("(n p) d -> p n d", p=128)
        yv = y_d.ap().rearrange("(n p) d -> p n d", p=128)
        stages = cfg["stages"]
        for half in range(2):
            self.half = half
            for tt in range(NTT):
                P.op("sp", lambda e, tt=tt, half=half: e.dma_start(out=self.H[:, tt, :], in_=xv[:, half * NTT + tt, :]),
                     w=[P.b("H", tt)], dma=True)
            self.norm_done = None
            for si, (l, what) in enumerate(stages):
                nxt = stages[si + 1] if si + 1 < len(stages) else None
                self.next_norm_row = None if nxt is None else nxt[0] * 6 + {"a": 0, "m": 2, "b": 4}[nxt[1]]
                if not hasattr(self, "marks"):
                    self.marks = []
                self.marks.append(("h%d L%d %s" % (half, l, what), len(P.q["pe"])))
                if what == "a":
                    self.ffn(l, 0)
                elif what == "b":
                    self.ffn(l, 1)
                elif what == "m" and l % 3 == 0:
                    self.swa(l)
                elif what == "m" and l % 3 == 1:
                    self.gla(l)
                elif what == "m" and l % 3 == 2:
                    self.rwkv(l)
            outs = []
            for tt in range(NTT):
                outs.append(P.op("sp", lambda e, tt=tt, half=half: e.dma_start(out=yv[:, half * NTT + tt, :], in_=self.H[:, tt, :]),
                                 r=[P.b("H", tt)], dma=True))
        fin = P.op("sp", lambda e: e.nop(), r=[])
        for lst in (P.ndma["sp"][-Prog.NDMA:],):
            for o in lst:
                fin.deps.append(o)
        P.emit()
        return nc


ALL_STAGES = [(l, w) for l in range(DEPTH) for w in ("a", "m", "b")]


def host_layout(inputs):
    f = np.float32
    win = inputs["ffn_w_in"]
    L = win.shape[0]
    w = win.reshape(L, 2, 8, 128, 2, NSLAB, 2, 128).transpose(0, 1, 5, 3, 2, 6, 4, 7)
    ffn_win = np.ascontiguousarray(w).reshape(L, 2, NSLAB, 128, 8 * 512).astype(f, copy=False)
    wout = inputs["ffn_w_out"]
    w = wout.reshape(L, 2, NSLAB, 2, 128, D).transpose(0, 1, 2, 4, 3, 5)
    ffn_wout = np.ascontiguousarray(w).reshape(L, 2, NSLAB, 128, 2 * 1024).astype(f, copy=False)
    rows = [inputs["norm_g"].reshape(DEPTH * 6, D)]
    cols = []
    lay = {}

    def blk(wm):
        n = wm.shape[1] // 512
        return np.ascontiguousarray(wm.reshape(8, 128, n, 512).transpose(2, 1, 0, 3)).reshape(n, 128, 4096)

    def pad_row(v):
        r = np.zeros((1, D), f)
        r[0, :v.size] = v.reshape(-1)
        return r

    def col(v):
        return np.ascontiguousarray(v.reshape(-1, 128).T)

    lay["row_swa"] = sum(r.shape[0] for r in rows)
    lay["col_swa"] = sum(c.shape[1] for c in cols)
    wq, wk, wv, wo = [], [], [], []
    for j in range(2):
        wqkv = inputs["swa_w_qkv"][j]
        b = inputs["swa_b_qkv"][j]
        wq.append(blk(wqkv[:, 0:1024]))
        kd = wqkv[:, 1024:1280].reshape(1024, 4, 1, 64).repeat(2, axis=2).reshape(1024, 512)
        wk.append(blk(kd)[0])
        vd = np.concatenate([wqkv[:, 1280:1536], np.zeros((1024, 256), f)], axis=1)
        wv.append(blk(vd)[0])
        wo.append(blk(inputs["swa_w_o"][j]))
        rows += [pad_row(b[1280:1536]), pad_row(inputs["swa_b_o"][j]), pad_row(inputs["swa_sinks"][j])]
        bkd = b[1024:1280].reshape(4, 1, 64).repeat(2, axis=1).reshape(512)
        cols += [col(b[0:1024]), col(bkd)]
    out = {"swa_wq": np.stack(wq), "swa_wk": np.stack(wk), "swa_wv": np.stack(wv), "swa_wo": np.stack(wo)}
    lay["row_gla"] = sum(r.shape[0] for r in rows)
    lay["col_gla"] = sum(c.shape[1] for c in cols)
    gw = np.concatenate([inputs["gla_w_in"][0], np.zeros((1024, 7 * 512 - 3088), f)], axis=1)
    out["gla_win"] = blk(gw)
    out["gla_wo"] = blk(inputs["gla_w_o"][0])
    out["gla_wg2"] = np.ascontiguousarray(inputs["gla_w_gate2"][0]).astype(f, copy=False)
    rows += [np.tile(inputs["gla_norm_g"][0], 4)[None, :]]
    cols += [col(inputs["gla_b_gate"][0])]
    lay["row_rwkv"] = sum(r.shape[0] for r in rows)
    lay["col_rwkv"] = sum(c.shape[1] for c in cols)
    l1 = np.concatenate([inputs["rwkv_w1"][0], inputs["rwkv_a1"][0], inputs["rwkv_g1"][0],
                         np.zeros((1024, 512 - 288), f)], axis=1)
    out["rwkv_wl1"] = blk(l1)
    out["rwkv_wv"] = blk(inputs["rwkv_w_rkv"][0, 2])
    wrk = inputs["rwkv_w_rkv"][0, 0:2]
    out["rwkv_wrk"] = np.ascontiguousarray(wrk.reshape(2, 8, 128, 8, 128).transpose(3, 2, 0, 1, 4)).reshape(8, 128, 2048)
    out["rwkv_wo"] = blk(inputs["rwkv_w_o"][0])
    out["rwkv_w2"] = np.ascontiguousarray(inputs["rwkv_w2"][0])
    out["rwkv_a2"] = np.ascontiguousarray(inputs["rwkv_a2"][0])
    out["rwkv_g2"] = np.ascontiguousarray(inputs["rwkv_g2"][0])
    rows += [inputs["rwkv_lnx_g"][0][None, :], inputs["rwkv_lnx_b"][0][None, :]]
    cols += [col(inputs["rwkv_mu"][0].reshape(-1)), col(inputs["rwkv_w0"][0]), col(inputs["rwkv_a0"][0]),
             col(inputs["rwkv_k_k"][0]), col(inputs["rwkv_k_a"][0]), col(inputs["rwkv_r_k"][0].reshape(-1))]
    pp = np.arange(128)[:, None]
    ff = np.arange(128)[None, :]
    p6 = pp % 64
    f6 = np.arange(64)[None, :]
    same = (pp // 64 == ff // 64)
    rc = np.concatenate([same & (pp < ff), same & (pp <= ff), same & (pp > ff), same,
                         (pp // 64 == np.arange(2)[None, :]), (p6 == f6)], axis=1)
    out["rconst"] = rc.astype(np.float32).astype(ml_dtypes.bfloat16)
    rows = np.ascontiguousarray(np.concatenate(rows, axis=0)).astype(f, copy=False)
    cols = np.ascontiguousarray(np.concatenate(cols, axis=1)).astype(f, copy=False)
    idb = np.eye(128, dtype=np.float32).astype(ml_dtypes.bfloat16)
    jj = np.arange(128)[:, None]
    ii = np.arange(128)[None, :]
    cm = np.concatenate([np.tile((jj <= ii), (1, 4)), np.tile((jj > ii), (1, 4))], axis=1)
    cmask = cm.astype(np.float32).astype(ml_dtypes.bfloat16)
    out.update({"rows": rows, "cols": cols, "ffn_win": ffn_win, "ffn_wout": ffn_wout, "idb": idb, "cmask": cmask})
    for k in list(out):
        if out[k].dtype == np.float64:
            out[k] = out[k].astype(f)
    return out, lay


_CACHE = {}


def run(inputs, stages, ncores=8, trace=False):
    shared, lay = host_layout(inputs)
    cfg = {"stages": stages, "nrows": shared["rows"].shape[0], "ncols": shared["cols"].shape[1]}
    cfg.update(lay)
    kk = K(cfg)
    with kk.stack:
        nc = kk.build()
    x = np.ascontiguousarray(inputs["x"]).astype(np.float32, copy=False)
    in_maps = []
    for c in range(ncores):
        m = {"x": x[c]}
        for k in kk.dram:
            if k != "x":
                m[k] = shared[k]
        in_maps.append(m)
    res = run_bass_kernel_spmd(nc, in_maps, core_ids=list(range(ncores)), trace=trace)
    out = np.stack([np.asarray(r["y"]) for r in res.results], axis=0)
    return out.astype(np.float32, copy=False), res


def kernel(**inputs):
    out, _ = run(inputs, ALL_STAGES)
    return out
```
